# Optimizing a Trainium2 kernel written in Bass

```python
import jax, jax.numpy as jnp
from jax import lax
import numpy as np

D_MODEL = 1024
BATCH = 32
SEQ = 2048
DEPTH = 2

HEAD_DIM = 64
A_Q_HEADS = 8
A_KV_HEADS = 2
A_WINDOW = 128
B_HEADS = 8
B_PATTERNS = ((128, 1), (512, 4), (2048, 16))
ROPE_THETA = 10000.0
BLOCK = 128
C_HEADS = 16
C_Q_RANK = 384
C_KV_RANK = 256
C_NOPE_DIM = 64
C_ROPE_DIM = 32
C_V_DIM = 64
C_QK_DIM = C_NOPE_DIM + C_ROPE_DIM
D_FF = 2816
N_EXPERTS = 8
TOP_K = 2
D_FF_EXPERT = 3584
MOE_BLOCK = 256
EPS = 1e-6

A_Q_W = A_Q_HEADS * HEAD_DIM
A_KV_W = A_KV_HEADS * HEAD_DIM
B_W = B_HEADS * HEAD_DIM
L0_IN = A_Q_W + 2 * A_KV_W + 3 * B_W
L0_MIX = A_Q_W + B_W
L0_SPLITS = (A_Q_W, A_Q_W + A_KV_W, A_Q_W + 2 * A_KV_W, A_Q_W + 2 * A_KV_W + B_W, A_Q_W + 2 * A_KV_W + 2 * B_W)
L1_IN = C_Q_RANK + C_KV_RANK + C_ROPE_DIM

kernel_name = "hybrid_swa_dilated_mla_moe"


def rms_norm(x, g):
    xf = x.astype(jnp.float32)
    y = xf * lax.rsqrt(jnp.mean(xf * xf, axis=-1, keepdims=True) + EPS)
    return (y * g.astype(jnp.float32)).astype(x.dtype)


def rope_tables(positions, dim):
    inv = ROPE_THETA ** (-jnp.arange(0, dim, 2, dtype=jnp.float32) / dim)
    ang = positions.astype(jnp.float32)[..., None] * inv
    return jnp.cos(ang)[:, :, None, :], jnp.sin(ang)[:, :, None, :]


def apply_rope(t, cos, sin):
    half = t.shape[-1] // 2
    tf = t.astype(jnp.float32)
    t1, t2 = tf[..., :half], tf[..., half:]
    return jnp.concatenate([t1 * cos - t2 * sin, t2 * cos + t1 * sin], axis=-1).astype(t.dtype)


def swiglu(h, w_gate, w_up, w_down):
    return (jax.nn.silu(h @ w_gate) * (h @ w_up)) @ w_down


def banded_attention(q, k, v, max_dist, sinks):
    nb_, L, hq, dh = q.shape
    hkv = k.shape[2]
    g = hq // hkv
    nblk = -(-L // BLOCK)
    lp = nblk * BLOCK
    pad = ((0, 0), (0, lp - L), (0, 0), (0, 0))
    q, k, v = jnp.pad(q, pad), jnp.pad(k, pad), jnp.pad(v, pad)

    def two_blocks(t):
        prev = jnp.pad(t, ((0, 0), (BLOCK, 0), (0, 0), (0, 0)))[:, :lp]
        return jnp.concatenate([prev.reshape(nb_, nblk, BLOCK, hkv, dh),
                                t.reshape(nb_, nblk, BLOCK, hkv, dh)], axis=2)

    kb, vb = two_blocks(k), two_blocks(v)
    qb = q.reshape(nb_, nblk, BLOCK, hkv, g, dh)
    s = jnp.einsum('bnqhgd,bnkhd->bnhgqk', qb, kb, preferred_element_type=jnp.float32) * (dh ** -0.5)
    qi = jnp.arange(BLOCK)[:, None]
    kj = jnp.arange(2 * BLOCK)[None, :]
    dist = BLOCK + qi - kj
    kpos = jnp.arange(nblk)[:, None, None] * BLOCK - BLOCK + kj
    valid = (dist >= 0) & (dist <= max_dist) & (kpos >= 0)
    s = jnp.where(valid[None, :, None, None], s, -jnp.inf)
    m = jnp.max(s, axis=-1)
    if sinks is not None:
        sink = sinks.astype(jnp.float32).reshape(1, 1, hkv, g, 1)
        m = jnp.maximum(m, sink)
    p = jnp.exp(s - m[..., None])
    denom = jnp.sum(p, axis=-1)
    if sinks is not None:
        denom = denom + jnp.exp(sink - m)
    o = jnp.einsum('bnhgqk,bnkhd->bnhgqd', p.astype(v.dtype), vb,
                   preferred_element_type=jnp.float32) / denom[..., None]
    o = o.transpose(0, 1, 4, 2, 3, 5).reshape(nb_, lp, hq, dh)[:, :L]
    lse = (m + jnp.log(denom)).transpose(0, 1, 4, 2, 3).reshape(nb_, lp, hq)[:, :L]
    return o, lse


def dilated_mixture(q, k, v):
    bn, s, h, dh = q.shape
    outs, lses = [], []
    for window, dil in B_PATTERNS:
        def fold(t):
            rest = t.shape[2:]
            return t.reshape(bn, s // dil, dil, *rest).swapaxes(1, 2).reshape(bn * dil, s // dil, *rest)

        def unfold(t):
            rest = t.shape[2:]
            return t.reshape(bn, dil, s // dil, *rest).swapaxes(1, 2).reshape(bn, s, *rest)

        o, lse = banded_attention(fold(q), fold(k), fold(v), window // dil, None)
        outs.append(unfold(o))
        lses.append(unfold(lse))
    w = jax.nn.softmax(jnp.stack(lses), axis=0)
    return jnp.sum(w[..., None] * jnp.stack(outs), axis=0)


def causal_block_attention(q, k, v):
    bn, s, h, dqk = q.shape
    dv = v.shape[-1]
    nq = s // BLOCK
    qb = q.reshape(bn, nq, BLOCK, h, dqk).swapaxes(0, 1)
    key_pos = jnp.arange(s)
    scale = dqk ** -0.5

    def one(args):
        qi, n = args
        sc = jnp.einsum('bqhd,bkhd->bhqk', qi, k, preferred_element_type=jnp.float32) * scale
        qpos = n * BLOCK + jnp.arange(BLOCK)
        sc = jnp.where(qpos[:, None] >= key_pos[None, :], sc, -jnp.inf)
        p = jax.nn.softmax(sc, axis=-1)
        return jnp.einsum('bhqk,bkhd->bqhd', p.astype(v.dtype), v,
                          preferred_element_type=jnp.float32).astype(v.dtype)

    o = lax.map(one, (qb, jnp.arange(nq)))
    return o.swapaxes(0, 1).reshape(bn, s, h * dv)


def moe_swiglu(h, router, we_gate, we_up, we_down):
    bn, s, d = h.shape
    n = bn * s
    xt = h.reshape(n, d)
    logits = jnp.einsum('nd,de->ne', xt, router, preferred_element_type=jnp.float32)
    top_vals, top_idx = lax.top_k(logits, TOP_K)
    gates = jax.nn.softmax(top_vals, axis=-1)
    e_flat = top_idx.reshape(-1).astype(jnp.int32)
    tok_flat = jnp.repeat(jnp.arange(n, dtype=jnp.int32), TOP_K)
    g_flat = gates.reshape(-1)
    order = jnp.argsort(e_flat)
    e_sorted = e_flat[order]
    counts = jnp.zeros((N_EXPERTS,), jnp.int32).at[e_flat].add(1)
    starts = jnp.cumsum(counts) - counts
    padded = (counts + MOE_BLOCK - 1) // MOE_BLOCK * MOE_BLOCK
    pend = jnp.cumsum(padded)
    pstarts = pend - padded
    rank = jnp.arange(n * TOP_K, dtype=jnp.int32) - starts[e_sorted]
    dest = pstarts[e_sorted] + rank
    nb = -(-(n * TOP_K) // MOE_BLOCK) + N_EXPERTS
    p_rows = nb * MOE_BLOCK
    row_tok = jnp.full((p_rows,), n, jnp.int32).at[dest].set(tok_flat[order])
    row_gate = jnp.zeros((p_rows,), jnp.float32).at[dest].set(g_flat[order])
    block_e = jnp.minimum(jnp.searchsorted(pend, jnp.arange(nb, dtype=jnp.int32) * MOE_BLOCK, side='right'),
                          N_EXPERTS - 1)
    x_pad = jnp.concatenate([xt, jnp.zeros((1, d), xt.dtype)], axis=0)
    xs = x_pad[row_tok].reshape(nb, MOE_BLOCK, d)

    def expert_block(args):
        xb, e = args
        return swiglu(xb, we_gate[e], we_up[e], we_down[e])

    ys = lax.map(expert_block, (xs, block_e)).reshape(p_rows, d)
    ys = ys * row_gate[:, None].astype(ys.dtype)
    out = jnp.zeros((n + 1, d), ys.dtype).at[row_tok].add(ys)[:n]
    return out.reshape(bn, s, d)


def ab_layer(x, cos, sin, attn_norm, w_in, a_qn, a_kn, a_sinks, b_qn, b_kn, w_out,
             ffn_norm, w_gate, w_up, w_down):
    bn, s, _ = x.shape
    h = rms_norm(x, attn_norm)
    proj = h @ w_in
    aq, ak, av, bq, bk, bv = jnp.split(proj, L0_SPLITS, axis=-1)

    def heads(t, nh):
        return t.reshape(bn, s, nh, HEAD_DIM)

    aq = apply_rope(rms_norm(heads(aq, A_Q_HEADS), a_qn), cos, sin)
    ak = apply_rope(rms_norm(heads(ak, A_KV_HEADS), a_kn), cos, sin)
    a_out, _ = banded_attention(aq, ak, heads(av, A_KV_HEADS), A_WINDOW - 1, a_sinks)
    bq = apply_rope(rms_norm(heads(bq, B_HEADS), b_qn), cos, sin)
    bk = apply_rope(rms_norm(heads(bk, B_HEADS), b_kn), cos, sin)
    b_out = dilated_mixture(bq, bk, heads(bv, B_HEADS))
    mix = jnp.concatenate([a_out.reshape(bn, s, A_Q_W), b_out.reshape(bn, s, B_W)], axis=-1).astype(x.dtype)
    x = x + mix @ w_out
    return x + swiglu(rms_norm(x, ffn_norm), w_gate, w_up, w_down)


def mla_moe_layer(x, cos, sin, attn_norm, w_in, q_a_norm, w_uq, kv_a_norm, w_ukv, c_qn, c_kn, w_out,
                  ffn_norm, router, we_gate, we_up, we_down):
    bn, s, _ = x.shape
    h = rms_norm(x, attn_norm)
    proj = h @ w_in
    cq, ckv, kpe = jnp.split(proj, (C_Q_RANK, C_Q_RANK + C_KV_RANK), axis=-1)
    q = (rms_norm(cq, q_a_norm) @ w_uq).reshape(bn, s, C_HEADS, C_QK_DIM)
    kv = (rms_norm(ckv, kv_a_norm) @ w_ukv).reshape(bn, s, C_HEADS, C_NOPE_DIM + C_V_DIM)
    k_nope, v = kv[..., :C_NOPE_DIM], kv[..., C_NOPE_DIM:]
    k = jnp.concatenate([k_nope, jnp.broadcast_to(kpe[:, :, None, :], (bn, s, C_HEADS, C_ROPE_DIM))], axis=-1)
    q = rms_norm(q, c_qn)
    k = rms_norm(k, c_kn)
    q = jnp.concatenate([q[..., :C_NOPE_DIM], apply_rope(q[..., C_NOPE_DIM:], cos, sin)], axis=-1)
    k = jnp.concatenate([k[..., :C_NOPE_DIM], apply_rope(k[..., C_NOPE_DIM:], cos, sin)], axis=-1)
    attn = causal_block_attention(q, k, v)
    x = x + attn @ w_out
    return x + moe_swiglu(rms_norm(x, ffn_norm), router, we_gate, we_up, we_down)


def setup_inputs(seed: int = 0) -> dict:
    key = jax.random.key(seed)
    ks = iter(jax.random.split(key, 40))

    def w(shape, fan_in):
        return jax.random.normal(next(ks), shape, jnp.float32) * (fan_in ** -0.5)

    def gain(nd):
        return 1.0 + 0.05 * jax.random.normal(next(ks), (nd,), jnp.float32)

    x = jax.random.normal(next(ks), (BATCH, SEQ, D_MODEL), jnp.float32)
    offsets = jax.random.randint(next(ks), (BATCH, 1), 0, 4096, dtype=jnp.int32)
    positions = (offsets + jnp.arange(SEQ, dtype=jnp.int32)[None, :]).astype(jnp.int32)
    return {
        'x': x,
        'positions': positions,
        'l0_attn_norm': gain(D_MODEL),
        'l0_w_in': w((D_MODEL, L0_IN), D_MODEL),
        'l0_a_q_norm': gain(HEAD_DIM),
        'l0_a_k_norm': gain(HEAD_DIM),
        'l0_a_sinks': 0.5 * jax.random.normal(next(ks), (A_Q_HEADS,), jnp.float32),
        'l0_b_q_norm': gain(HEAD_DIM),
        'l0_b_k_norm': gain(HEAD_DIM),
        'l0_w_out': w((L0_MIX, D_MODEL), L0_MIX),
        'l0_ffn_norm': gain(D_MODEL),
        'l0_w_gate': w((D_MODEL, D_FF), D_MODEL),
        'l0_w_up': w((D_MODEL, D_FF), D_MODEL),
        'l0_w_down': w((D_FF, D_MODEL), D_FF),
        'l1_attn_norm': gain(D_MODEL),
        'l1_w_in': w((D_MODEL, L1_IN), D_MODEL),
        'l1_q_a_norm': gain(C_Q_RANK),
        'l1_w_uq': w((C_Q_RANK, C_HEADS * C_QK_DIM), C_Q_RANK),
        'l1_kv_a_norm': gain(C_KV_RANK),
        'l1_w_ukv': w((C_KV_RANK, C_HEADS * (C_NOPE_DIM + C_V_DIM)), C_KV_RANK),
        'l1_c_q_norm': gain(C_QK_DIM),
        'l1_c_k_norm': gain(C_QK_DIM),
        'l1_w_out': w((C_HEADS * C_V_DIM, D_MODEL), C_HEADS * C_V_DIM),
        'l1_ffn_norm': gain(D_MODEL),
        'l1_router': w((D_MODEL, N_EXPERTS), D_MODEL),
        'l1_we_gate': w((N_EXPERTS, D_MODEL, D_FF_EXPERT), D_MODEL),
        'l1_we_up': w((N_EXPERTS, D_MODEL, D_FF_EXPERT), D_MODEL),
        'l1_we_down': w((N_EXPERTS, D_FF_EXPERT, D_MODEL), D_FF_EXPERT),
    }


def reference(x, positions, l0_attn_norm, l0_w_in, l0_a_q_norm, l0_a_k_norm, l0_a_sinks, l0_b_q_norm,
              l0_b_k_norm, l0_w_out, l0_ffn_norm, l0_w_gate, l0_w_up, l0_w_down, l1_attn_norm, l1_w_in,
              l1_q_a_norm, l1_w_uq, l1_kv_a_norm, l1_w_ukv, l1_c_q_norm, l1_c_k_norm, l1_w_out, l1_ffn_norm,
              l1_router, l1_we_gate, l1_we_up, l1_we_down):
    cos_h, sin_h = rope_tables(positions, HEAD_DIM)
    cos_c, sin_c = rope_tables(positions, C_ROPE_DIM)
    layer_params = (
        (l0_attn_norm, l0_w_in, l0_a_q_norm, l0_a_k_norm, l0_a_sinks, l0_b_q_norm, l0_b_k_norm, l0_w_out,
         l0_ffn_norm, l0_w_gate, l0_w_up, l0_w_down),
        (l1_attn_norm, l1_w_in, l1_q_a_norm, l1_w_uq, l1_kv_a_norm, l1_w_ukv, l1_c_q_norm, l1_c_k_norm,
         l1_w_out, l1_ffn_norm, l1_router, l1_we_gate, l1_we_up, l1_we_down),
    )
    for layer in range(DEPTH):
        if layer % 2 == 0:
            x = ab_layer(x, cos_h, sin_h, *layer_params[layer])
        else:
            x = mla_moe_layer(x, cos_c, sin_c, *layer_params[layer])
    return x
```

```python
import numpy as np
from contextlib import ExitStack
import concourse.bass as bass
import concourse.mybir as mybir
from concourse.bass_utils import run_bass_kernel_spmd

F32 = mybir.dt.float32
BF16 = mybir.dt.bfloat16
I32 = mybir.dt.int32
AF = mybir.ActivationFunctionType
ALU = mybir.AluOpType
AX = mybir.AxisListType

D = 1024
SEQ = 2048
NT = SEQ // 128
L0_IN = 2304
DFF = 2816
L1_IN = 672
NEXP = 8
DFE = 3584
EPS = 1e-6
PI = float(np.pi)
ECAP = 8192
GRP = 512


class Sched:
    def __init__(self, nc, stack, ring=10):
        self.nc = nc
        self.eng = {"pe": nc.tensor, "act": nc.scalar, "dve": nc.vector, "pool": nc.gpsimd, "sp": nc.sync}
        self.sem = {k: stack.enter_context(nc.semaphore("s_" + k)) for k in self.eng}
        self.cnt = {k: 0 for k in self.eng}
        self.seen = {k: {} for k in self.eng}
        self.rings = {}
        for q in ("sp", "pool"):
            self.rings[q] = [[stack.enter_context(nc.semaphore("d_%s%d" % (q, i))), 0] for i in range(ring)]
        self.ring_pos = {q: 0 for q in self.rings}
        self.lastw = {}
        self.readers = {}
        self.prog = {k: [] for k in self.eng}
        self.dma_toks = []
        self.bg_jobs = []

    def bg_step(self, n=1):
        for _ in range(n):
            if self.bg_jobs:
                self.bg_jobs.pop(0)()

    def _wait(self, e, tok):
        sem, val, owner = tok
        if owner == e and e == "pe":
            return
        name = id(sem)
        if self.seen[e].get(name, 0) >= val:
            return
        self.prog[e].append(("w", sem, val))
        self.seen[e][name] = val

    def _deps(self, e, reads, writes):
        for k in reads:
            t = self.lastw.get(k)
            if t is not None:
                self._wait(e, t)
        for k in writes:
            t = self.lastw.get(k)
            if t is not None:
                self._wait(e, t)
            for t in self.readers.get(k, ()):
                self._wait(e, t)

    def _commit(self, tok, reads, writes):
        for k in reads:
            self.readers.setdefault(k, []).append(tok)
        for k in writes:
            self.lastw[k] = tok
            self.readers[k] = []

    def op(self, e, meth, reads=(), writes=(), signal=True, **kw):
        self._deps(e, reads, writes)
        if signal:
            self.cnt[e] += 1
            self.prog[e].append(("i", meth, kw, self.sem[e], 1))
            tok = (self.sem[e], self.cnt[e], e)
        else:
            self.prog[e].append(("i", meth, kw, None, 0))
            tok = (self.sem[e], self.cnt[e] + 1, e)
        self._commit(tok, reads, writes)
        return tok

    def dma(self, q, out, in_, reads=(), writes=(), meth="dma_start", **kw):
        self._deps(q, reads, writes)
        ring = self.rings[q]
        i = self.ring_pos[q]
        self.ring_pos[q] = (i + 1) % len(ring)
        sem, n = ring[i]
        if n > 0:
            self._wait(q, (sem, 16 * n, "dma"))
        self.prog[q].append(("i", meth, dict(out=out, in_=in_, **kw), sem, 16))
        ring[i][1] = n + 1
        tok = (sem, 16 * (n + 1), "dma")
        self._commit(tok, reads, writes)
        self.dma_toks.append(tok)
        return tok

    def reg_load_all(self, ap, reads=()):
        for e in self.eng:
            self._deps(e, reads, ())
            self.prog[e].append(("rl", ap))
            tok = (self.sem[e], self.cnt[e] + 1, e) if False else None

    def cond_begin(self, thresh):
        self.cond = {}
        start = {id(self.sem[o]): self.cnt[o] for o in self.eng}
        for q in self.rings:
            for sem, n in self.rings[q]:
                start[id(sem)] = 16 * n
        self.cond_start = start
        for e in self.eng:
            info = {"thresh": thresh, "pos": len(self.prog[e]), "cnt0": self.cnt[e]}
            if e in self.rings:
                info["ring0"] = [n for (_, n) in self.rings[e]]
            self.prog[e].append(("cb", info))
            self.cond[e] = info

    def cond_end(self):
        for e in self.eng:
            info = self.cond[e]
            body = self.prog[e][info["pos"] + 1:]
            info["waits"] = [(it[1], it[2]) for it in body if it[0] == "w" and it[2] <= self.cond_start[id(it[1])]]
            info["delta"] = self.cnt[e] - info["cnt0"]
            if e in self.rings:
                info["rings"] = [(sem, n0, n - n0) for (sem, n), n0 in zip(self.rings[e], info["ring0"])]
            self.prog[e].append(("ce",))
        self.cond = None

    def barrier(self):
        toks = [(self.sem[o], self.cnt[o], o) for o in self.eng if self.cnt[o] > 0]
        for q in self.rings:
            for sem, n in self.rings[q]:
                if n > 0:
                    toks.append((sem, 16 * n, "dma"))
        for e in self.eng:
            for t in toks:
                if t[2] == e:
                    continue
                self._wait(e, t)
        self.lastw.clear()
        self.readers.clear()
        self.dma_toks = []

    def finish(self):
        for sem, n in self.rings["sp"] + self.rings["pool"]:
            if n > 0:
                self._wait("sp", (sem, 16 * n, "dma"))

    def emit(self):
        nc = self.nc
        with nc.Block() as block:
            def mk(name):
                def body(e):
                    reg = None
                    guard = None
                    bcreg = None
                    if name == "pool":
                        bcreg = e.alloc_register("bc")
                        e.reg_mov(bcreg, NEXP * ECAP - 1)
                    for it in self.prog[name]:
                        if it[0] == "w":
                            e.wait_ge(it[1], it[2])
                        elif it[0] == "rl":
                            if reg is None:
                                reg = e.alloc_register("pred_" + name)
                            e.reg_load(reg, it[1])
                        elif it[0] == "cb":
                            info = it[1]
                            g = e.If_lt(reg, info["thresh"])
                            g.__enter__()
                            for (sem, val) in info["waits"]:
                                e.wait_ge(sem, val)
                            if info["delta"] > 0:
                                if info["cnt0"] > 0:
                                    e.wait_ge(self.sem[name], info["cnt0"])
                                e.sem_inc(self.sem[name], info["delta"])
                            for (sem, n0, dn) in info.get("rings", ()):
                                if dn > 0:
                                    if n0 > 0:
                                        e.wait_ge(sem, 16 * n0)
                                    e.sem_inc(sem, 16 * dn)
                            g.__exit__(None, None, None)
                            guard = e.Else()
                            guard.__enter__()
                        elif it[0] == "ce":
                            guard.__exit__(None, None, None)
                            guard = None
                        else:
                            kw = it[2]
                            if kw.get("bounds_check", None) == "BC":
                                kw = dict(kw)
                                kw["bounds_check"] = bcreg
                            ins = getattr(e, it[1])(**kw)
                            if it[3] is not None:
                                ins.then_inc(it[3], it[4])
                return body
            block.sync(mk("sp"))
            block.scalar(mk("act"))
            block.vector(mk("dve"))
            block.gpsimd(mk("pool"))
            block.tensor(mk("pe"))


class Ctx:
    def __init__(self, nc, S, stack, pfx):
        self.nc, self.S, self.st, self.pfx = nc, S, stack, pfx

    def sb(self, name, shape, dt):
        return self.st.enter_context(self.nc.sbuf_tensor(self.pfx + "s_" + name, shape, dt))

    def ps(self, name, shape, dt):
        return self.st.enter_context(self.nc.psum_tensor(self.pfx + "p_" + name, shape, dt))


def bc_dram(handle, parts, inner):
    return bass.AP(handle, 0, [[0, parts], [1, inner]])


def load_consts(nc, S, cx, T, need_mb=False, need_ma=False):
    C = {}
    C["ident"] = cx.sb("ident", [128, 128], BF16)
    S.dma("pool", C["ident"][:], T["ident"].ap(), writes=["ident"])
    C["eps"] = cx.sb("eps", [128, 1], F32)
    S.op("dve", "memset", writes=["eps"], ap=C["eps"][:], constant=EPS)
    C["pib"] = cx.sb("pib", [128, 1], F32)
    S.op("dve", "memset", writes=["pib"], ap=C["pib"][:], constant=PI)
    if need_ma:
        C["MA"] = cx.sb("MA", [128, 2, 128], BF16)
        S.dma("pool", C["MA"][:], T["maskA"].ap(), writes=["masks"])
    if need_mb:
        C["MB"] = cx.sb("MB", [128, 16, 128], BF16)
        S.dma("pool", C["MB"][:], T["maskB"].ap(), writes=["masks"])
    return C


def rms_rstd(S, ss_ap, out_ap, tmp_ap, scale, eps_ap, keys_r, keys_w, tmpkey):
    S.op("act", "activation", reads=list(keys_r) + ["eps"], writes=[tmpkey], out=tmp_ap, in_=ss_ap, func=AF.Ln,
         scale=scale, bias=eps_ap)
    S.op("act", "activation", reads=[tmpkey], writes=list(keys_w), out=out_ap, in_=tmp_ap, func=AF.Exp, scale=-0.5)


def norm_tile(S, C, xt, xkey, gain, gkey, xn, xnkey, W, sfx):
    S.op("dve", "scalar_tensor_tensor", reads=[xkey], writes=["junk" + sfx, "ss" + sfx], out=W["junk"][:, 0:D], in0=xt,
         scalar=1.0, in1=xt, op0=ALU.mult, op1=ALU.mult, accum_out=W["ss"][:])
    rms_rstd(S, W["ss"][:], W["rstd"][:], W["lnt"][:], 1.0 / D, C["eps"][:], ["ss" + sfx], ["rstd" + sfx],
             "lnt" + sfx)
    S.op("dve", "scalar_tensor_tensor", reads=[xkey, "rstd" + sfx, gkey], writes=[xnkey], out=xn, in0=xt,
         scalar=W["rstd"][:, 0:1], in1=gain, op0=ALU.mult, op1=ALU.mult)


def transposes(S, C, src_aps, srckey, psT, pskey, dst_ap, dstkey, nrows=128, evac="dve"):
    n = len(src_aps)
    for i, a in enumerate(src_aps):
        w = a.shape[-1] if len(a.shape) == 2 else None
        S.op("pe", "transpose", reads=[srckey, "ident"], writes=[pskey], signal=(i == n - 1),
             out=psT[0:nrows, i, :], in_=a, identity=C["ident"][:])
    if evac == "dve":
        S.op("dve", "tensor_copy", reads=[pskey], writes=[dstkey], out=dst_ap, in_=psT[0:nrows, 0:n, :])
    else:
        S.op("act", "copy", reads=[pskey], writes=[dstkey], out=dst_ap, in_=psT[0:nrows, 0:n, :])


def rope_tables(S, C, cx, pos_ap, invt, nfreq, sfx):
    W = C["rope" + sfx]
    AP_ = lambda x: x if isinstance(x, bass.AP) else x[:]
    S.dma("sp", AP_(W["posi"]), pos_ap, writes=["posi" + sfx])
    S.op("dve", "tensor_copy", reads=["posi" + sfx], writes=["posf" + sfx], out=AP_(W["posf"]), in_=AP_(W["posi"]))
    pf = AP_(W["posf"]).unsqueeze(2).to_broadcast([128, NT, nfreq])
    iv = invt[:].unsqueeze(1).to_broadcast([128, NT, nfreq])
    S.op("dve", "tensor_tensor", reads=["posf" + sfx, "inv" + sfx], writes=["ang" + sfx], out=AP_(W["ang"]), in0=pf,
         in1=iv, op=ALU.mult)
    for (nm, shift) in (("sin", 0.0), ("cos", PI / 2)):
        S.op("dve", "tensor_scalar", reads=["ang" + sfx], writes=["rr" + sfx], out=AP_(W["rr"]), in0=AP_(W["ang"]),
             scalar1=shift, scalar2=1.0 / (2 * PI), op0=ALU.add, op1=ALU.mult)
        S.op("dve", "tensor_copy", reads=["rr" + sfx], writes=["ki" + sfx], out=AP_(W["ki"]), in_=AP_(W["rr"]))
        S.op("dve", "tensor_copy", reads=["ki" + sfx], writes=["kf" + sfx], out=AP_(W["kf"]), in_=AP_(W["ki"]))
        S.op("dve", "tensor_scalar", reads=["ang" + sfx], writes=["rr" + sfx], out=AP_(W["rr"]), in0=AP_(W["ang"]),
             scalar1=shift, scalar2=None, op0=ALU.add)
        S.op("dve", "scalar_tensor_tensor", reads=["kf" + sfx, "rr" + sfx], writes=["rr" + sfx], out=AP_(W["rr"]),
             in0=AP_(W["kf"]), scalar=-2 * PI, in1=AP_(W["rr"]), op0=ALU.mult, op1=ALU.add)
        S.op("dve", "tensor_scalar", reads=["rr" + sfx], writes=["kf" + sfx], out=AP_(W["kf"]), in0=AP_(W["rr"]),
             scalar1=PI, scalar2=None, op0=ALU.is_gt)
        S.op("dve", "scalar_tensor_tensor", reads=["kf" + sfx, "rr" + sfx], writes=["rr" + sfx], out=AP_(W["rr"]),
             in0=AP_(W["kf"]), scalar=-2 * PI, in1=AP_(W["rr"]), op0=ALU.mult, op1=ALU.add)
        S.op("dve", "tensor_scalar", reads=["rr" + sfx], writes=["rr" + sfx], out=AP_(W["rr"]), in0=AP_(W["rr"]),
             scalar1=PI, scalar2=-PI, op0=ALU.min, op1=ALU.max)
        S.op("act", "activation", reads=["rr" + sfx], writes=[nm + sfx], out=AP_(W[nm]), in_=AP_(W["rr"]), func=AF.Sin)


def alloc_rope(cx, nfreq, sfx, big=False):
    W = {
        "posi": cx.sb("posi" + sfx, [128, NT], I32),
        "posf": cx.sb("posf" + sfx, [128, NT], F32),
        "ki": cx.sb("ki" + sfx, [128, NT, nfreq], I32),
        "sin": cx.sb("sin" + sfx, [128, NT, nfreq], F32),
        "cos": cx.sb("cos" + sfx, [128, NT, nfreq], F32),
    }
    for nm in ("ang", "rr", "kf"):
        if big:
            full = cx.sb(nm + sfx, [128, NT, 2 * nfreq], F32)
            W[nm + "_full"] = full
            W[nm] = full[:, :, 0:nfreq]
        else:
            W[nm] = cx.sb(nm + sfx, [128, NT, nfreq], F32)
    return W


def make_jobs(head, kbs):
    jobs = []
    nkb = len(kbs)
    for c0 in range(0, nkb, 4):
        jobs.append({"h": head, "c0": c0, "chunk": kbs[c0:c0 + 4], "nkb": nkb})
    return jobs


def job_S(S, bufs, ctr, job):
    h = job["h"]
    psS, pT = bufs["psS"], bufs["pT"]
    i_s = ctr["s"] % len(psS)
    ctr["s"] += 1
    i_p = ctr["p"] % len(pT)
    ctr["p"] += 1
    ps, pskey = psS[i_s], "psS%d" % i_s
    pt, ptkey = pT[i_p], "pT%d" % i_p
    job["pt"], job["ptkey"] = pt, ptkey
    chunk = job["chunk"]
    n = len(chunk)
    madd = h["mask_fn"](job["c0"], chunk)
    lo, hi = (madd[0], madd[1]) if madd is not None else (0, 0)
    order = [i for i in range(n) if not (lo <= i < hi)] + [i for i in range(n) if lo <= i < hi]
    for pos, i in enumerate(order):
        kb = chunk[i]
        masked = lo <= i < hi
        if masked and i == lo:
            S.op("pe", "matmul", reads=["ident", "masks"], writes=[pskey], signal=False,
                 out=ps[:, lo * 128:hi * 128], lhsT=bufs["ident"], rhs=madd[2], start=True, stop=False)
        S.op("pe", "matmul", reads=[h["qkey"], h["kkey"](kb)], writes=[pskey], signal=(pos == n - 1),
             out=ps[:, i * 128:(i + 1) * 128], lhsT=h["kT_fn"](kb), rhs=h["q_ap"], start=(not masked),
             stop=((not masked) or i == hi - 1))
    S.op("act", "activation", reads=[pskey], writes=[ptkey], out=pt[:, 0:n * 128], in_=ps[:, 0:n * 128],
         func=AF.Exp, scale=h["scale"])


def job_PV(S, bufs, ctr, job):
    h = job["h"]
    psO = bufs["psO"]
    if job["c0"] == 0:
        io = ctr["o"] % len(psO)
        ctr["o"] += 1
        h["po"], h["pokey"] = psO[io], "psO%d" % io
    po, pokey = h["po"], h["pokey"]
    chunk = job["chunk"]
    n = len(chunk)
    pt, ptkey = job["pt"], job["ptkey"]
    for i, kb in enumerate(chunk):
        gi = job["c0"] + i
        S.op("pe", "matmul", reads=[ptkey, h["vkey"](kb)], writes=[pokey], signal=(i == n - 1),
             out=po[:, 0:65], lhsT=pt[:, i * 128:(i + 1) * 128], rhs=h["v_fn"](kb), start=(gi == 0),
             stop=(gi == job["nkb"] - 1))
    if job["c0"] + n == job["nkb"]:
        h["fin_fn"](po, pokey)


def run_jobs(S, bufs, ctr, jobs, fillers):
    n = len(jobs)
    nf = len(fillers)
    fi = 0
    LA = len(bufs["psS"]) - 1
    for k in range(n + LA):
        if k < n:
            job_S(S, bufs, ctr, jobs[k])
        if k >= LA:
            job_PV(S, bufs, ctr, jobs[k - LA])
        while fi < nf and (k + 1) * nf >= (fi + 1) * (n + LA):
            fillers[fi]()
            fi += 1
    while fi < nf:
        fillers[fi]()
        fi += 1


def phase_l0_attn(nc, S, T, x_in, x_out, nseq, ntiles=NT):
    with ExitStack() as st:
        cx = Ctx(nc, S, st, "a0")
        C = load_consts(nc, S, cx, T, need_mb=True, need_ma=True)
        w_in = cx.sb("w_in", [128, 8, L0_IN], BF16)
        for c in range(8):
            for hi in range(2):
                S.dma("pool", w_in[:, c, 0:512].rearrange("p (lo hi d) -> p lo hi d", lo=4, hi=2)[:, :, hi, :],
                      T["l0_w_in"].ap()[c * 128:(c + 1) * 128, hi * 256:(hi + 1) * 256].rearrange(
                          "k (lo d) -> k lo d", d=64), writes=["w_in"])
            S.dma("pool", w_in[:, c, 512:L0_IN], T["l0_w_in"].ap()[c * 128:(c + 1) * 128, 512:L0_IN],
                  writes=["w_in"])
        w_out = cx.sb("w_out", [128, 8, D], BF16)
        S.dma("pool", w_out[:], T["l0_w_out"].ap().rearrange("(c p) n -> p c n", p=128), writes=["w_out"])
        gattn = cx.sb("gattn", [128, D], F32)
        S.dma("sp", gattn[:], bc_dram(T["l0_attn_norm"], 128, D), writes=["gattn"])
        gfull = cx.sb("gfull", [128, 26, 64], F32)
        for (h0, nh, nm) in ((0, 8, "l0_a_q_norm"), (8, 2, "l0_a_k_norm"), (10, 8, "l0_b_q_norm"),
                             (18, 8, "l0_b_k_norm")):
            S.dma("sp", gfull[:, h0:h0 + nh, :], bass.AP(T[nm], 0, [[0, 128], [0, nh], [1, 64]]), writes=["gfull"])
        esink = cx.sb("esink", [128, 8], F32)
        S.dma("sp", esink[:], bc_dram(T["l0_a_sinks"], 128, 8), writes=["esink"])
        S.op("act", "activation", reads=["esink"], writes=["esink"], out=esink[:], in_=esink[:], func=AF.Exp)
        invh = cx.sb("invh", [128, 32], F32)
        S.dma("sp", invh[:], T["inv_h"].ap(), writes=["inv_h"])
        C["rope_h"] = alloc_rope(cx, 32, "_h")

        kT = cx.sb("kT", [128, 5, SEQ], BF16)
        Vaug = cx.sb("Vaug", [128, NT, 10, 65], BF16)
        S.op("dve", "memset", writes=[("Vaug", i) for i in range(NT)], ap=Vaug[:], constant=1.0)
        NB = 3
        xt = [cx.sb("xt%d" % i, [128, D], F32) for i in range(NB)]
        qT = [cx.sb("qT%d" % i, [128, 8, 128], BF16) for i in range(2)]
        xn = cx.sb("xn", [128, D], BF16)
        hT = cx.sb("hT", [128, 8, 128], BF16)
        proj = cx.sb("proj", [128, L0_IN], F32)
        W = {"junk": cx.sb("junk", [128, D], BF16), "ss": cx.sb("ss", [128, 1], F32),
             "rstd": cx.sb("rstd", [128, 1], F32), "lnt": cx.sb("lnt", [128, 1], F32)}
        sqb = cx.sb("sqb", [128, 1024], BF16)
        ssh = cx.sb("ssh", [128, 26], F32)
        rsh = cx.sb("rsh", [128, 26], F32)
        lnh = cx.sb("lnh", [128, 26], F32)
        tn = cx.sb("tn", [128, 16, 64], F32)
        tcb = cx.sb("tcb", [128, 16, 64], F32)
        tsb = cx.sb("tsb", [128, 16, 64], F32)
        qk = cx.sb("qk", [128, 26, 64], BF16)
        pT = [cx.sb("pT%d" % i, [128, 512], BF16) for i in range(4)]
        mix = cx.sb("mix", [128, D], BF16)
        mixT = cx.sb("mixT", [128, 8, 128], BF16)
        x1 = cx.sb("x1", [128, D], F32)
        den = cx.sb("den", [128, 32], F32)
        obufs = [cx.sb("obuf%d" % i, [128, 16, 65], F32) for i in range(2)]
        psT = [cx.ps("psT%d" % i, [128, 8, 128], BF16) for i in range(2)]
        psP = [cx.ps("psP%d" % i, [128, 512], F32) for i in range(1)]
        psS = [cx.ps("psS%d" % i, [128, 512], F32) for i in range(3)]
        psO = [cx.ps("psO%d" % i, [128, 512], F32) for i in range(2)]
        bufs = {"psS": psS, "pT": pT, "psO": psO, "ident": C["ident"][:]}
        ctr = {"s": 0, "p": 0, "o": 0, "t": 0, "pp": 0, "d": 0}

        def next_psT():
            i = ctr["t"] % 2
            ctr["t"] += 1
            return psT[i], "psT%d" % i

        def next_psP():
            i = ctr["pp"] % len(psP)
            ctr["pp"] += 1
            return psP[i], "psP%d" % i

        KK = lambda kb: ("kT", kb)
        VK = lambda kb: ("Vaug", kb)
        xin = x_in.ap() if hasattr(x_in, "ap") else x_in
        xo = x_out.ap() if hasattr(x_out, "ap") else x_out

        RW = C["rope_h"]

        def stageA(b, t):
            row0 = b * SEQ + t * 128
            gi = b * ntiles + t
            xtt, xkey = xt[gi % NB], "xt%d" % (gi % NB)
            steps = []

            def s1():
                S.dma("sp", xtt[:], xin[row0:row0 + 128, :], writes=[xkey])
                norm_tile(S, C, xtt[:], xkey, gattn[:], "gattn", xn[:], "xn", W, "")
                p_, pk = next_psT()
                transposes(S, C, [xn[:, c * 128:(c + 1) * 128] for c in range(8)], "xn", p_, pk, hT[:], "hT")
            steps.append(s1)

            def s2(n0):
                nw = min(512, L0_IN - n0)
                pp, ppk = next_psP()
                for c in range(8):
                    S.op("pe", "matmul", reads=["hT", "w_in"], writes=[ppk], signal=(c == 7), out=pp[:, 0:nw],
                         lhsT=hT[:, c, :], rhs=w_in[:, c, n0:n0 + nw], start=(c == 0), stop=(c == 7))
                S.op("act", "copy", reads=[ppk], writes=["proj"], out=proj[:, n0:n0 + nw], in_=pp[:, 0:nw])
            for n0 in range(0, L0_IN, 512):
                steps.append(lambda n0=n0: s2(n0))

            def s3():
                S.op("act", "copy", reads=["proj"], writes=[("Vaug", t)], out=Vaug[:, t, 0:2, 0:64],
                     in_=proj[:, 640:768].rearrange("p (h d) -> p h d", d=64))
                S.op("act", "copy", reads=["proj"], writes=[("Vaug", t)], out=Vaug[:, t, 2:10, 0:64],
                     in_=proj[:, 1792:2304].rearrange("p (h d) -> p h d", d=64))
            steps.append(s3)
            cosb = RW["cos"][:, t, :]
            sinb = RW["sin"][:, t, :]

            def s4a1(c0, nh, h0):
                S.op("act", "activation", reads=["proj"], writes=["sqb"], out=sqb[:, 0:nh * 64],
                     in_=proj[:, c0:c0 + nh * 64], func=AF.Square)

            def s4a2(c0, nh, h0):
                S.op("dve", "tensor_reduce", reads=["sqb"], writes=["ssh"], out=ssh[:, h0:h0 + nh],
                     in_=sqb[:, 0:nh * 64].rearrange("p (h d) -> p h d", d=64), axis=AX.X, op=ALU.add)

            def s4a3(c0, nh, h0):
                rms_rstd(S, ssh[:, h0:h0 + nh], rsh[:, h0:h0 + nh], lnh[:, h0:h0 + nh], 1.0 / 64, C["eps"][:],
                         ["ssh"], ["rsh"], "lnh")

            def s4a4(c0, nh, h0):
                pv = proj[:, c0:c0 + nh * 64].rearrange("p (h d) -> p h d", d=64)
                S.op("dve", "tensor_tensor", reads=["proj", "rsh"], writes=["tn"], out=tn[:, 0:nh, :], in0=pv,
                     in1=rsh[:, h0:h0 + nh].unsqueeze(2).to_broadcast([128, nh, 64]), op=ALU.mult)
                S.op("dve", "tensor_tensor", reads=["tn", "gfull"], writes=["tn"], out=tn[:, 0:nh, :],
                     in0=tn[:, 0:nh, :], in1=gfull[:, h0:h0 + nh, :], op=ALU.mult)

            def s4b(c0, nh, h0):
                t4 = tn[:, 0:nh, :].rearrange("p h (two d) -> p h two d", two=2)
                tc4 = tcb[:, 0:nh, :].rearrange("p h (two d) -> p h two d", two=2)
                ts4 = tsb[:, 0:nh, :].rearrange("p h (two d) -> p h two d", two=2)
                cos4 = cosb.unsqueeze(1).unsqueeze(1).to_broadcast([128, nh, 2, 32])
                sin4 = sinb.unsqueeze(1).unsqueeze(1).to_broadcast([128, nh, 2, 32])
                S.op("dve", "tensor_tensor", reads=["tn", "cos_h"], writes=["tcb"], out=tc4, in0=t4, in1=cos4,
                     op=ALU.mult)
                S.op("dve", "tensor_tensor", reads=["tn", "sin_h"], writes=["tsb"], out=ts4, in0=t4, in1=sin4,
                     op=ALU.mult)
                q4 = qk[:, h0:h0 + nh, :].rearrange("p h (two d) -> p h two d", two=2)
                S.op("dve", "tensor_tensor", reads=["tcb", "tsb"], writes=["qk"], out=q4[:, :, 0, :],
                     in0=tc4[:, :, 0, :], in1=ts4[:, :, 1, :], op=ALU.subtract)
                S.op("dve", "tensor_tensor", reads=["tcb", "tsb"], writes=["qk"], out=q4[:, :, 1, :],
                     in0=tc4[:, :, 1, :], in1=ts4[:, :, 0, :], op=ALU.add)
            for (c0, nh, h0) in ((0, 10, 0), (768, 16, 10)):
                steps.append(lambda c0=c0, nh=nh, h0=h0: s4a1(c0, nh, h0))
                steps.append(lambda c0=c0, nh=nh, h0=h0: s4a2(c0, nh, h0))
                steps.append(lambda c0=c0, nh=nh, h0=h0: s4a3(c0, nh, h0))
                steps.append(lambda c0=c0, nh=nh, h0=h0: s4a4(c0, nh, h0))
                steps.append(lambda c0=c0, nh=nh, h0=h0: s4b(c0, nh, h0))
            qTt, qkey = qT[gi % 2], "qT%d" % (gi % 2)
            qk2 = qk[:].rearrange("p h d -> p (h d)")

            def s5():
                p_, pk = next_psT()
                srcs = [qk2[:, 128 * j:128 * (j + 1)] for j in range(4)] + \
                       [qk2[:, 640 + 128 * j:640 + 128 * (j + 1)] for j in range(4)]
                transposes(S, C, srcs, "qk", p_, pk, qTt[:], qkey, evac="act")

            def s6():
                p_, pk = next_psT()
                srcs = [qk2[:, 512:640]] + [qk2[:, 1152 + 128 * j:1152 + 128 * (j + 1)] for j in range(4)]
                transposes(S, C, srcs, "qk", p_, pk, kT[:, :, t * 128:(t + 1) * 128], ("kT", t))
            steps.append(s5)
            steps.append(s6)
            return steps

        def stageB(b, t, fillers):
            gi = b * ntiles + t
            obuf, obn = obufs[gi % 2], "obuf%d" % (gi % 2)
            qTt, qkey = qT[gi % 2], "qT%d" % (gi % 2)
            jobs = []
            for h in range(8):
                half = slice(0, 64) if h < 4 else slice(64, 128)
                kbs = [t - 1, t] if t >= 1 else [t]

                def maskA(c0, chunk):
                    if len(chunk) == 2:
                        return (0, 2, C["MA"][:].rearrange("p a q -> p (a q)"))
                    return (0, 1, C["MA"][:, 1, :])

                def finA(po, pokey, h=h):
                    S.op("act", "copy", reads=[pokey], writes=[(obn, h)], out=obuf[:, h, :], in_=po[:, 0:65])

                head = {"kT_fn": (lambda kb, half=half: kT[half, 0, kb * 128:(kb + 1) * 128]),
                        "q_ap": qTt[half, h % 4, :], "qkey": qkey, "kkey": KK,
                        "v_fn": (lambda kb, h=h: Vaug[:, kb, h // 4, :]), "vkey": VK, "mask_fn": maskA,
                        "scale": 0.125, "fin_fn": finA}
                jobs += make_jobs(head, kbs)
            for h in range(8):
                half = slice(0, 64) if h % 2 == 0 else slice(64, 128)
                kbs = list(range(t + 1))

                def maskB(c0, chunk, t=t):
                    i0 = 15 - t + chunk[0]
                    return (0, len(chunk), C["MB"][:, i0:i0 + len(chunk), :].rearrange("p a q -> p (a q)"))

                def finB(po, pokey, h=h):
                    S.op("act", "copy", reads=[pokey], writes=[(obn, 8 + h)], out=obuf[:, 8 + h, :],
                         in_=po[:, 0:65])

                head = {"kT_fn": (lambda kb, half=half, h=h: kT[half, 1 + h // 2, kb * 128:(kb + 1) * 128]),
                        "q_ap": qTt[half, 4 + h // 2, :], "qkey": qkey, "kkey": KK,
                        "v_fn": (lambda kb, h=h: Vaug[:, kb, 2 + h, :]), "vkey": VK, "mask_fn": maskB,
                        "scale": 0.125, "fin_fn": finB}
                jobs += make_jobs(head, kbs)
            S.bg_step()
            run_jobs(S, bufs, ctr, jobs, fillers)

        def stageC(b, t):
            row0 = b * SEQ + t * 128
            gi = b * ntiles + t
            xtt, xkey = xt[gi % NB], "xt%d" % (gi % NB)
            obuf, obn = obufs[gi % 2], "obuf%d" % (gi % 2)
            okeys = [(obn, i) for i in range(16)]

            def c1():
                S.op("dve", "tensor_tensor", reads=okeys + ["esink"], writes=["den"], out=den[:, 0:8],
                     in0=obuf[:, 0:8, 64], in1=esink[:], op=ALU.add)
                S.op("dve", "tensor_copy", reads=okeys, writes=["den"], out=den[:, 8:16], in_=obuf[:, 8:16, 64])
                S.op("dve", "reciprocal", reads=["den"], writes=["den"], out=den[:, 16:32], in_=den[:, 0:16])
                S.op("dve", "tensor_tensor", reads=okeys + ["den"], writes=["mix"],
                     out=mix[:].rearrange("p (h d) -> p h d", d=64), in0=obuf[:, :, 0:64],
                     in1=den[:, 16:32].unsqueeze(2).to_broadcast([128, 16, 64]), op=ALU.mult)

            def c2():
                p_, pk = next_psT()
                transposes(S, C, [mix[:, c * 128:(c + 1) * 128] for c in range(8)], "mix", p_, pk, mixT[:], "mixT")

            def c3(n0):
                pp, ppk = next_psP()
                for c in range(8):
                    S.op("pe", "matmul", reads=["mixT", "w_out"], writes=[ppk], signal=(c == 7), out=pp[:],
                         lhsT=mixT[:, c, :], rhs=w_out[:, c, n0:n0 + 512], start=(c == 0), stop=(c == 7))
                S.op("dve", "tensor_tensor", reads=[ppk, xkey], writes=["x1"], out=x1[:, n0:n0 + 512], in0=pp[:],
                     in1=xtt[:, n0:n0 + 512], op=ALU.add)
                if n0 == 512:
                    S.dma("sp", xo[row0:row0 + 128, :], x1[:], reads=["x1"])
            return [c1, c2, (lambda: c3(0)), (lambda: c3(512))]

        order = [(b, t) for b in range(nseq) for t in range(ntiles)]
        for idx, (b, t) in enumerate(order):
            if idx == 0:
                rope_tables(S, C, cx, T["pos_t"].ap()[b], invh, 32, "_h")
                for f in stageA(b, t):
                    f()
            fillers = []
            if idx >= 1:
                fillers += stageC(*order[idx - 1])
            if idx + 1 < len(order):
                nb, nt_ = order[idx + 1]
                if nt_ == 0:
                    fillers.append(lambda nb=nb: rope_tables(S, C, cx, T["pos_t"].ap()[nb], invh, 32, "_h"))
                fillers += stageA(nb, nt_)
            stageB(b, t, fillers)
        for f in stageC(*order[-1]):
            f()
        S.barrier()


def phase_l0_ffn(nc, S, T, x_in, x_out, nrows):
    with ExitStack() as st:
        cx = Ctx(nc, S, st, "f0")
        C = load_consts(nc, S, cx, T)
        G = min(1024, nrows)
        GT = G // 128
        NF = DFF // 128
        wd = cx.sb("wd", [128, NF, D], BF16)
        pre = "l0_w_gate_bf" in T
        wq = "sp" if pre else "pool"
        wdv = T["l0_w_down_bf" if pre else "l0_w_down"].ap().rearrange("(f p) n -> p f n", p=128)
        for f0 in range(0, NF, 6):
            f1 = min(NF, f0 + 6)
            S.dma(wq, wd[:, f0:f1, :], wdv[:, f0:f1, :], writes=["wd"])
        gffn = cx.sb("gffn", [128, D], F32)
        S.dma("sp", gffn[:], bc_dram(T["l0_ffn_norm"], 128, D), writes=["gffn"])
        hT = cx.sb("hT", [128, 8, G], BF16)
        hidT = cx.sb("hidT", [128, NF, G], BF16)
        xs = cx.sb("xs", [128, GT, D], F32)
        wg = [cx.sb("wg%d" % i, [128, 8, 512], BF16) for i in range(2)]
        wu = [cx.sb("wu%d" % i, [128, 8, 512], BF16) for i in range(2)]
        xn = cx.sb("xn", [128, D], BF16)
        W = {"junk": cx.sb("junk", [128, D], BF16), "ss": cx.sb("ss", [128, 1], F32),
             "rstd": cx.sb("rstd", [128, 1], F32), "lnt": cx.sb("lnt", [128, 1], F32)}
        sg = [cx.sb("sg%d" % i, [128, 512], F32) for i in range(2)]
        yo = [cx.sb("yo%d" % i, [128, D], F32) for i in range(2)]
        psT = [cx.ps("psT%d" % i, [128, 8, 128], BF16) for i in range(2)]
        psG = [cx.ps("psG%d" % i, [128, 512], F32) for i in range(2)]
        psU = [cx.ps("psU%d" % i, [128, 512], F32) for i in range(2)]
        psY = [cx.ps("psY%d" % i, [128, 512], F32) for i in range(2)]
        xin, xo = x_in.ap(), x_out.ap()
        wgd, wud = T["l0_w_gate_bf" if pre else "l0_w_gate"].ap(), T["l0_w_up_bf" if pre else "l0_w_up"].ap()
        blocks = [(n0, min(512, DFF - n0)) for n0 in range(0, DFF, 512)]
        ctr = {"t": 0, "g": 0, "y": 0, "w": 0}

        def load_w(bi):
            n0, nw = blocks[bi]
            i = ctr["w"] % 2
            ctr["w"] += 1
            S.dma(wq, wg[i][:, :, 0:nw], wgd[:, n0:n0 + nw].rearrange("(c p) n -> p c n", p=128),
                  writes=["wg%d" % i])
            S.dma(wq, wu[i][:, :, 0:nw], wud[:, n0:n0 + nw].rearrange("(c p) n -> p c n", p=128),
                  writes=["wu%d" % i])
            return i

        for g0 in range(0, nrows, G):
            for i in range(GT):
                r0 = g0 + i * 128
                S.dma("sp", xs[:, i, :], xin[r0:r0 + 128, :], writes=[("xs", i)])
                norm_tile(S, C, xs[:, i, :], ("xs", i), gffn[:], "gffn", xn[:], "xn", W, "")
                k = ctr["t"] % 2
                ctr["t"] += 1
                transposes(S, C, [xn[:, c * 128:(c + 1) * 128] for c in range(8)], "xn", psT[k], "psT%d" % k,
                           hT[:, :, i * 128:(i + 1) * 128], "hT")
            wi = load_w(0)
            for bi, (n0, nw) in enumerate(blocks):
                cur = wi
                if bi + 1 < len(blocks):
                    wi = load_w(bi + 1)
                for j in range(nw // 128):
                    f = n0 // 128 + j
                    for th in range(G // 512):
                        k = ctr["g"] % 2
                        ctr["g"] += 1
                        tok = slice(th * 512, (th + 1) * 512)
                        for c in range(8):
                            S.op("pe", "matmul", reads=["hT", "wg%d" % cur], writes=["psG%d" % k], signal=(c == 7),
                                 out=psG[k][:], lhsT=wg[cur][:, c, j * 128:(j + 1) * 128], rhs=hT[:, c, tok],
                                 start=(c == 0), stop=(c == 7))
                        for c in range(8):
                            S.op("pe", "matmul", reads=["hT", "wu%d" % cur], writes=["psU%d" % k], signal=(c == 7),
                                 out=psU[k][:], lhsT=wu[cur][:, c, j * 128:(j + 1) * 128], rhs=hT[:, c, tok],
                                 start=(c == 0), stop=(c == 7))
                        S.op("act", "activation", reads=["psG%d" % k], writes=["sg%d" % k], out=sg[k][:],
                             in_=psG[k][:], func=AF.Silu)
                        S.op("dve", "tensor_tensor", reads=["sg%d" % k, "psU%d" % k], writes=[("hidT", f)],
                             out=hidT[:, f, tok], in0=sg[k][:], in1=psU[k][:], op=ALU.mult)
            for i in range(GT):
                r0 = g0 + i * 128
                yk = ctr["y"] % 2
                ctr["y"] += 1
                for hh, n0 in enumerate((0, 512)):
                    k = (ctr["g"] + hh) % 2
                    for f in range(NF):
                        S.op("pe", "matmul", reads=[("hidT", f), "wd"], writes=["psY%d" % k], signal=(f == NF - 1),
                             out=psY[k][:], lhsT=hidT[:, f, i * 128:(i + 1) * 128], rhs=wd[:, f, n0:n0 + 512],
                             start=(f == 0), stop=(f == NF - 1))
                    S.op("dve", "tensor_tensor", reads=["psY%d" % k, ("xs", i)], writes=["yo%d" % yk],
                         out=yo[yk][:, n0:n0 + 512], in0=psY[k][:], in1=xs[:, i, n0:n0 + 512], op=ALU.add)
                S.dma("sp", xo[r0:r0 + 128, :], yo[yk][:], reads=["yo%d" % yk])
        S.barrier()


def phase_l1_attn(nc, S, T, x_in, x_out, nseq, ntiles=NT):
    with ExitStack() as st:
        cx = Ctx(nc, S, st, "a1")
        C = load_consts(nc, S, cx, T, need_ma=True)
        w_in = cx.sb("w_in", [128, 8, L1_IN], BF16)
        S.dma("pool", w_in[:], T["l1_w_in"].ap().rearrange("(c p) n -> p c n", p=128), writes=["w_in"])
        w_uq = cx.sb("w_uq", [128, 3, 1536], BF16)
        S.dma("pool", w_uq[:], T["l1_w_uq"].ap().rearrange("(c p) n -> p c n", p=128), writes=["w_uq"])
        w_ukv = cx.sb("w_ukv", [128, 2, 2048], BF16)
        S.dma("pool", w_ukv[:], T["l1_w_ukv"].ap().rearrange("(c p) n -> p c n", p=128), writes=["w_ukv"])
        w_out = cx.sb("w_out", [128, 8, D], BF16)
        S.dma("pool", w_out[:], T["l1_w_out"].ap().rearrange("(c p) n -> p c n", p=128), writes=["w_out"])
        gattn = cx.sb("gattn", [128, D], F32)
        S.dma("sp", gattn[:], bc_dram(T["l1_attn_norm"], 128, D), writes=["gattn"])
        glat = cx.sb("glat", [128, 640], F32)
        S.dma("sp", glat[:, 0:384], bc_dram(T["l1_q_a_norm"], 128, 384), writes=["glat"])
        S.dma("sp", glat[:, 384:640], bc_dram(T["l1_kv_a_norm"], 128, 256), writes=["glat"])
        gq = cx.sb("gq", [128, 96], F32)
        S.dma("sp", gq[:], bc_dram(T["l1_c_q_norm"], 128, 96), writes=["gq"])
        gk = cx.sb("gk", [128, 96], F32)
        S.dma("sp", gk[:], bc_dram(T["l1_c_k_norm"], 128, 96), writes=["gk"])
        invc = cx.sb("invc", [128, 16], F32)
        S.dma("sp", invc[:], T["inv_c"].ap(), writes=["inv_c"])
        rp = alloc_rope(cx, 16, "_c", big=True)
        trq, tcq, tsq = rp["ang_full"], rp["rr_full"], rp["kf_full"]
        C["rope_c"] = rp

        kT = cx.sb("kT", [128, 16, SEQ], BF16)
        Vaug = cx.sb("Vaug", [128, NT, 16, 65], BF16)
        S.op("dve", "memset", writes=[("Vaug", i) for i in range(NT)], ap=Vaug[:], constant=1.0)
        NB = 2
        xt = [cx.sb("xt%d" % i, [128, D], F32) for i in range(NB)]
        qTs = [cx.sb("qT%d" % i, [128, 16, 128], BF16) for i in range(2)]
        xn = cx.sb("xn", [128, D], BF16)
        hT = cx.sb("hT", [128, 8, 128], BF16)
        pj = cx.sb("pj", [128, L1_IN], F32)
        cn = cx.sb("cn", [128, 640], BF16)
        cT = hT
        qf = cx.sb("qf", [128, 16, 96], F32)
        knf = cx.sb("knf", [128, 16, 64], F32)
        W = {"junk": xn, "ss": cx.sb("ss", [128, 1], F32),
             "rstd": cx.sb("rstd", [128, 1], F32), "lnt": cx.sb("lnt", [128, 1], F32)}
        lss = cx.sb("lss", [128, 4], F32)
        lrs = cx.sb("lrs", [128, 4], F32)
        lln = cx.sb("lln", [128, 4], F32)
        ssh = cx.sb("ssh", [128, 32], F32)
        rsh = cx.sb("rsh", [128, 32], F32)
        lnh = cx.sb("lnh", [128, 32], F32)
        kr = cx.sb("kr", [128, 4, 32], F32)
        qb = cx.sb("qb", [128, 16, 96], BF16)
        kb_ = cx.sb("kb", [128, 16, 96], BF16)
        pT = [cx.sb("pT%d" % i, [128, 512], BF16) for i in range(3)]
        obuf = cx.sb("obuf", [128, 16, 65], F32)
        mix = cx.sb("mix", [128, D], BF16)
        mixT = hT
        den = cx.sb("den", [128, 16], F32)
        psT = [cx.ps("psT%d" % i, [128, 8, 128], BF16) for i in range(2)]
        psP = [cx.ps("psP%d" % i, [128, 512], F32) for i in range(1)]
        psS = [cx.ps("psS%d" % i, [128, 512], F32) for i in range(3)]
        psO = [cx.ps("psO%d" % i, [128, 512], F32) for i in range(2)]
        bufs = {"psS": psS, "pT": pT, "psO": psO, "ident": C["ident"][:]}
        ctr = {"s": 0, "p": 0, "o": 0, "t": 0, "pp": 0}

        def next_psT():
            i = ctr["t"] % 2
            ctr["t"] += 1
            return psT[i], "psT%d" % i

        def next_psP():
            i = ctr["pp"] % len(psP)
            ctr["pp"] += 1
            return psP[i], "psP%d" % i

        KK = lambda kb: ("kT", kb)
        VK = lambda kb: ("Vaug", kb)
        xin, xo = x_in.ap(), x_out.ap()
        SC = 96 ** -0.5

        RW = C["rope_c"]
        jk96 = qb
        jk64 = kb_

        def stageA(b, t):
            row0 = b * SEQ + t * 128
            gi = b * ntiles + t
            xtt, xkey = xt[gi % NB], "xt%d" % (gi % NB)
            steps = []

            def s1():
                S.dma("sp", xtt[:], xin[row0:row0 + 128, :], writes=[xkey])
                norm_tile(S, C, xtt[:], xkey, gattn[:], "gattn", xn[:], "xn", W, "")
                p_, pk = next_psT()
                transposes(S, C, [xn[:, c * 128:(c + 1) * 128] for c in range(8)], "xn", p_, pk, hT[:], "hT")
                for n0 in range(0, L1_IN, 512):
                    nw = min(512, L1_IN - n0)
                    pp, ppk = next_psP()
                    for c in range(8):
                        S.op("pe", "matmul", reads=["hT", "w_in"], writes=[ppk], signal=(c == 7), out=pp[:, 0:nw],
                             lhsT=hT[:, c, :], rhs=w_in[:, c, n0:n0 + nw], start=(c == 0), stop=(c == 7))
                    S.op("act", "copy", reads=[ppk], writes=["pj"], out=pj[:, n0:n0 + nw], in_=pp[:, 0:nw])
            steps.append(s1)

            def s2a():
                for (i, c0, c1) in ((0, 0, 384), (1, 384, 640)):
                    S.op("dve", "scalar_tensor_tensor", reads=["pj"], writes=["cn", "lss"], out=cn[:, c0:c1],
                         in0=pj[:, c0:c1], scalar=1.0, in1=pj[:, c0:c1], op0=ALU.mult, op1=ALU.mult,
                         accum_out=lss[:, i:i + 1])

            def s2b():
                for (i, c0, c1) in ((0, 0, 384), (1, 384, 640)):
                    rms_rstd(S, lss[:, i:i + 1], lrs[:, i:i + 1], lln[:, i:i + 1], 1.0 / (c1 - c0), C["eps"][:],
                             ["lss"], ["lrs"], "lln")

            def s2c():
                for (i, c0, c1) in ((0, 0, 384), (1, 384, 640)):
                    S.op("dve", "scalar_tensor_tensor", reads=["pj", "lrs", "glat"], writes=["cn"], out=cn[:, c0:c1],
                         in0=pj[:, c0:c1], scalar=lrs[:, i:i + 1], in1=glat[:, c0:c1], op0=ALU.mult, op1=ALU.mult)
                p_, pk = next_psT()
                transposes(S, C, [cn[:, c * 128:(c + 1) * 128] for c in range(5)], "cn", p_, pk, cT[:, 0:5, :], "hT")
            steps.append(s2a)
            steps.append(s2b)
            steps.append(s2c)
            qf2 = qf[:].rearrange("p h d -> p (h d)")

            def s3(n0):
                pp, ppk = next_psP()
                for c in range(3):
                    S.op("pe", "matmul", reads=["hT", "w_uq"], writes=[ppk], signal=(c == 2), out=pp[:],
                         lhsT=cT[:, c, :], rhs=w_uq[:, c, n0:n0 + 512], start=(c == 0), stop=(c == 2))
                S.op("act", "copy", reads=[ppk], writes=["qf"], out=qf2[:, n0:n0 + 512], in_=pp[:])
            for n0 in range(0, 1536, 512):
                steps.append(lambda n0=n0: s3(n0))

            def s4(j):
                pp, ppk = next_psP()
                for c in range(2):
                    S.op("pe", "matmul", reads=["hT", "w_ukv"], writes=[ppk], signal=(c == 1), out=pp[:],
                         lhsT=cT[:, 3 + c, :], rhs=w_ukv[:, c, j * 512:(j + 1) * 512], start=(c == 0),
                         stop=(c == 1))
                pv = pp[:].rearrange("p (h d) -> p h d", d=128)
                S.op("act", "copy", reads=[ppk], writes=["knf"], out=knf[:, 4 * j:4 * j + 4, :], in_=pv[:, :, 0:64])
                S.op("act", "copy", reads=[ppk], writes=[("Vaug", t)], out=Vaug[:, t, 4 * j:4 * j + 4, 0:64],
                     in_=pv[:, :, 64:128])
            for j in range(4):
                steps.append(lambda j=j: s4(j))
            cosb = RW["cos"][:, t, :]
            sinb = RW["sin"][:, t, :]

            def s5a():
                S.op("act", "activation", reads=["qf"], writes=["qb"], out=jk96[:], in_=qf[:], func=AF.Square)
                S.op("act", "activation", reads=["knf"], writes=["kb"], out=jk64[:, :, 0:64], in_=knf[:],
                     func=AF.Square)

            def s5b():
                S.op("dve", "tensor_reduce", reads=["qb"], writes=["ssh"], out=ssh[:, 0:16], in_=jk96[:], axis=AX.X,
                     op=ALU.add)
                S.op("dve", "tensor_reduce", reads=["kb"], writes=["ssh"], out=ssh[:, 16:32], in_=jk64[:, :, 0:64],
                     axis=AX.X, op=ALU.add)
                S.op("dve", "scalar_tensor_tensor", reads=["pj"], writes=["kr", "lss"], out=kr[:, 1, :],
                     in0=pj[:, 640:672], scalar=1.0, in1=pj[:, 640:672], op0=ALU.mult, op1=ALU.mult,
                     accum_out=lss[:, 2:3])
                S.op("dve", "tensor_scalar", reads=["ssh", "lss"], writes=["ssh"], out=ssh[:, 16:32], in0=ssh[:, 16:32],
                     scalar1=lss[:, 2:3], scalar2=None, op0=ALU.add)

            def s5c():
                rms_rstd(S, ssh[:], rsh[:], lnh[:], 1.0 / 96, C["eps"][:], ["ssh"], ["rsh"], "lnh")
            steps.append(s5a)
            steps.append(s5b)
            steps.append(lambda: None)
            steps.append(s5c)

            def s6():
                S.op("dve", "tensor_tensor", reads=["qf", "rsh"], writes=["qf"], out=qf[:], in0=qf[:],
                     in1=rsh[:, 0:16].unsqueeze(2).to_broadcast([128, 16, 96]), op=ALU.mult)
                S.op("dve", "tensor_tensor", reads=["qf", "gq"], writes=["qb"], out=qb[:, :, 0:64], in0=qf[:, :, 0:64],
                     in1=gq[:, 0:64].unsqueeze(1).to_broadcast([128, 16, 64]), op=ALU.mult)
                S.op("dve", "tensor_tensor", reads=["qf", "gq"], writes=["ang_c"], out=trq[:], in0=qf[:, :, 64:96],
                     in1=gq[:, 64:96].unsqueeze(1).to_broadcast([128, 16, 32]), op=ALU.mult)
                t4 = trq[:].rearrange("p h (two d) -> p h two d", two=2)
                tc4 = tcq[:].rearrange("p h (two d) -> p h two d", two=2)
                ts4 = tsq[:].rearrange("p h (two d) -> p h two d", two=2)
                cos4 = cosb.unsqueeze(1).unsqueeze(1).to_broadcast([128, 16, 2, 16])
                sin4 = sinb.unsqueeze(1).unsqueeze(1).to_broadcast([128, 16, 2, 16])
                S.op("dve", "tensor_tensor", reads=["ang_c", "cos_c"], writes=["rr_c"], out=tc4, in0=t4, in1=cos4,
                     op=ALU.mult)
                S.op("dve", "tensor_tensor", reads=["ang_c", "sin_c"], writes=["kf_c"], out=ts4, in0=t4, in1=sin4,
                     op=ALU.mult)
                S.op("dve", "tensor_tensor", reads=["rr_c", "kf_c"], writes=["qb"], out=qb[:, :, 64:80],
                     in0=tc4[:, :, 0, :], in1=ts4[:, :, 1, :], op=ALU.subtract)
                S.op("dve", "tensor_tensor", reads=["rr_c", "kf_c"], writes=["qb"], out=qb[:, :, 80:96],
                     in0=tc4[:, :, 1, :], in1=ts4[:, :, 0, :], op=ALU.add)
            steps.append(s6)

            def s7():
                S.op("dve", "tensor_tensor", reads=["knf", "rsh"], writes=["knf"], out=knf[:], in0=knf[:],
                     in1=rsh[:, 16:32].unsqueeze(2).to_broadcast([128, 16, 64]), op=ALU.mult)
                S.op("dve", "tensor_tensor", reads=["knf", "gk"], writes=["kb"], out=kb_[:, :, 0:64], in0=knf[:],
                     in1=gk[:, 0:64].unsqueeze(1).to_broadcast([128, 16, 64]), op=ALU.mult)
                S.op("dve", "tensor_tensor", reads=["pj", "gk"], writes=["kr"], out=kr[:, 0, :], in0=pj[:, 640:672],
                     in1=gk[:, 64:96], op=ALU.mult)
                k0 = kr[:, 0, :].rearrange("p (two d) -> p two d", two=2)
                k1 = kr[:, 1, :].rearrange("p (two d) -> p two d", two=2)
                k2 = kr[:, 2, :].rearrange("p (two d) -> p two d", two=2)
                S.op("dve", "tensor_tensor", reads=["kr", "cos_c"], writes=["kr"], out=k1, in0=k0,
                     in1=cosb.unsqueeze(1).to_broadcast([128, 2, 16]), op=ALU.mult)
                S.op("dve", "tensor_tensor", reads=["kr", "sin_c"], writes=["kr"], out=k2, in0=k0,
                     in1=sinb.unsqueeze(1).to_broadcast([128, 2, 16]), op=ALU.mult)
                S.op("dve", "tensor_tensor", reads=["kr"], writes=["kr"], out=kr[:, 3, 0:16], in0=k1[:, 0, :],
                     in1=k2[:, 1, :], op=ALU.subtract)
                S.op("dve", "tensor_tensor", reads=["kr"], writes=["kr"], out=kr[:, 3, 16:32], in0=k1[:, 1, :],
                     in1=k2[:, 0, :], op=ALU.add)
                S.op("dve", "tensor_tensor", reads=["kr", "rsh"], writes=["kb"], out=kb_[:, :, 64:96],
                     in0=kr[:, 3, :].unsqueeze(1).to_broadcast([128, 16, 32]),
                     in1=rsh[:, 16:32].unsqueeze(2).to_broadcast([128, 16, 32]), op=ALU.mult)
            steps.append(s7)
            kb2 = kb_[:].rearrange("p h d -> p (h d)")

            def s8(half):
                p_, pk = next_psT()
                transposes(S, C, [kb2[:, (8 * half + j) * 96:(8 * half + j + 1) * 96] for j in range(8)], "kb", p_,
                           pk, kT[0:96, 8 * half:8 * half + 8, t * 128:(t + 1) * 128], ("kT", t), nrows=96)
            steps.append(lambda: s8(0))
            steps.append(lambda: s8(1))
            return steps

        def q_transposes(b, t, half):
            gi = b * ntiles + t
            qT, qkey = qTs[gi % 2], "qT%d" % (gi % 2)
            qb2 = qb[:].rearrange("p h d -> p (h d)")
            p_, pk = next_psT()
            transposes(S, C, [qb2[:, (8 * half + j) * 96:(8 * half + j + 1) * 96] for j in range(8)], "qb", p_,
                       pk, qT[0:96, 8 * half:8 * half + 8, :], qkey, nrows=96, evac="act")

        def stageB(b, t, fillers):
            row0 = b * SEQ + t * 128
            gi = b * ntiles + t
            qT, qkey = qTs[gi % 2], "qT%d" % (gi % 2)
            kbs = list(range(t + 1))
            jobs = []
            for h in range(16):
                def maskC(c0, chunk, t=t):
                    if chunk[-1] == t:
                        return (len(chunk) - 1, len(chunk), C["MA"][:, 1, :])
                    return None

                def finC(po, pokey, h=h):
                    S.op("act", "copy", reads=[pokey], writes=[("obuf", h)], out=obuf[:, h, :], in_=po[:, 0:65])

                head = {"kT_fn": (lambda kb, h=h: kT[0:96, h, kb * 128:(kb + 1) * 128]), "q_ap": qT[0:96, h, :],
                        "qkey": qkey, "kkey": KK, "v_fn": (lambda kb, h=h: Vaug[:, kb, h, :]), "vkey": VK,
                        "mask_fn": maskC, "scale": SC, "fin_fn": finC}
                jobs += make_jobs(head, kbs)
            S.bg_step()
            run_jobs(S, bufs, ctr, jobs, fillers)
            okeys = [("obuf", i) for i in range(16)]
            S.op("dve", "reciprocal", reads=okeys, writes=["den"], out=den[:, 0:16], in_=obuf[:, :, 64])
            S.op("dve", "tensor_tensor", reads=okeys + ["den"], writes=["mix"],
                 out=mix[:].rearrange("p (h d) -> p h d", d=64), in0=obuf[:, :, 0:64],
                 in1=den[:, 0:16].unsqueeze(2).to_broadcast([128, 16, 64]), op=ALU.mult)

        def stageC(b, t):
            row0 = b * SEQ + t * 128
            gi = b * ntiles + t
            xtt, xkey = xt[gi % NB], "xt%d" % (gi % NB)

            def c2():
                p_, pk = next_psT()
                transposes(S, C, [mix[:, c * 128:(c + 1) * 128] for c in range(8)], "mix", p_, pk, mixT[:], "hT")

            def c3(n0):
                pp, ppk = next_psP()
                for c in range(8):
                    S.op("pe", "matmul", reads=["hT", "w_out"], writes=[ppk], signal=(c == 7), out=pp[:],
                         lhsT=mixT[:, c, :], rhs=w_out[:, c, n0:n0 + 512], start=(c == 0), stop=(c == 7))
                S.op("dve", "tensor_tensor", reads=[ppk, xkey], writes=[xkey], out=xtt[:, n0:n0 + 512], in0=pp[:],
                     in1=xtt[:, n0:n0 + 512], op=ALU.add)
                if n0 == 512:
                    S.dma("sp", xo[row0:row0 + 128, :], xtt[:], reads=[xkey])
            return [c2, (lambda: c3(0)), (lambda: c3(512))]

        order = [(b, t) for b in range(nseq) for t in range(ntiles)]
        for idx, (b, t) in enumerate(order):
            if idx == 0:
                rope_tables(S, C, cx, T["pos_t"].ap()[b], invc, 16, "_c")
                for f in stageA(b, t):
                    f()
                q_transposes(b, t, 0)
                q_transposes(b, t, 1)
            fillers = []
            if idx >= 1:
                fillers += stageC(*order[idx - 1])
            if idx + 1 < len(order):
                nb, nt_ = order[idx + 1]
                if nt_ == 0:
                    fillers.append(lambda nb=nb: rope_tables(S, C, cx, T["pos_t"].ap()[nb], invc, 16, "_c"))
                fillers += stageA(nb, nt_)
                fillers.append(lambda nb=nb, nt_=nt_: q_transposes(nb, nt_, 0))
                fillers.append(lambda nb=nb, nt_=nt_: q_transposes(nb, nt_, 1))
            stageB(b, t, fillers)
        for f in stageC(*order[-1]):
            f()
        S.barrier()


def phase_l1_moe(nc, S, T, x_in, x_out, nrows):
    with ExitStack() as st:
        cx = Ctx(nc, S, st, "m1")
        C = load_consts(nc, S, cx, T)
        G = min(1024, nrows)
        GT = G // 128
        NF = DFE // 128
        gffn = cx.sb("gffn", [128, D], F32)
        S.dma("sp", gffn[:], bc_dram(T["l1_ffn_norm"], 128, D), writes=["gffn"])
        rw = cx.sb("rw", [128, 8, NEXP], BF16)
        S.dma("pool", rw[:], T["l1_router"].ap().rearrange("(c p) n -> p c n", p=128), writes=["rw"])
        hT = cx.sb("hT", [128, 8, G], BF16)
        hidT = cx.sb("hidT", [128, NF, G], BF16)
        xs = cx.sb("xs", [128, GT, D], F32)
        wg = [cx.sb("wg%d" % i, [128, 8, 512], BF16) for i in range(2)]
        wu = [cx.sb("wu%d" % i, [128, 8, 512], BF16) for i in range(2)]
        wd = [cx.sb("wd%d" % i, [128, NF, 512], BF16) for i in range(2)]
        xn = cx.sb("xn", [128, D], BF16)
        W = {"junk": cx.sb("junk", [128, D], BF16), "ss": cx.sb("ss", [128, 1], F32),
             "rstd": cx.sb("rstd", [128, 1], F32), "lnt": cx.sb("lnt", [128, 1], F32)}
        sg = [cx.sb("sg%d" % i, [128, 512], F32) for i in range(2)]
        gates = cx.sb("gates", [128, GT, NEXP], F32)
        lg = cx.sb("lg", [128, NEXP], F32)
        l2 = cx.sb("l2", [128, NEXP], F32)
        eq1 = cx.sb("eq1", [128, NEXP], F32)
        eq2 = cx.sb("eq2", [128, NEXP], F32)
        sm = cx.sb("sm", [128, 8], F32)
        psT = [cx.ps("psT%d" % i, [128, 8, 128], BF16) for i in range(2)]
        psG = [cx.ps("psG%d" % i, [128, 512], F32) for i in range(2)]
        psU = [cx.ps("psU%d" % i, [128, 512], F32) for i in range(2)]
        psY = [cx.ps("psY%d" % i, [128, 512], F32) for i in range(2)]
        xin, xo = x_in.ap(), x_out.ap()
        wgd, wud, wdd = T["l1_we_gate"].ap(), T["l1_we_up"].ap(), T["l1_we_down"].ap()
        blocks = [(n0, 512) for n0 in range(0, DFE, 512)]
        ctr = {"t": 0, "g": 0, "y": 0, "w": 0, "d": 0}

        def load_w(e, bi):
            n0, nw = blocks[bi]
            i = ctr["w"] % 2
            ctr["w"] += 1
            S.dma("pool", wg[i][:], wgd[e, :, n0:n0 + nw].rearrange("(c p) n -> p c n", p=128), writes=["wg%d" % i])
            S.dma("pool", wu[i][:], wud[e, :, n0:n0 + nw].rearrange("(c p) n -> p c n", p=128), writes=["wu%d" % i])
            return i

        def load_wd(e, hh):
            i = ctr["d"] % 2
            ctr["d"] += 1
            src = wdd[e, :, hh * 512:(hh + 1) * 512].rearrange("(f p) n -> p f n", p=128)
            for f0 in range(0, NF, 7):
                S.dma("pool", wd[i][:, f0:f0 + 7, :], src[:, f0:f0 + 7, :], writes=["wd%d" % i])
            return i

        for g0 in range(0, nrows, G):
            for i in range(GT):
                r0 = g0 + i * 128
                S.dma("sp", xs[:, i, :], xin[r0:r0 + 128, :], writes=[("xs", i)])
                norm_tile(S, C, xs[:, i, :], ("xs", i), gffn[:], "gffn", xn[:], "xn", W, "")
                k = ctr["t"] % 2
                ctr["t"] += 1
                transposes(S, C, [xn[:, c * 128:(c + 1) * 128] for c in range(8)], "xn", psT[k], "psT%d" % k,
                           hT[:, :, i * 128:(i + 1) * 128], "hT")
                for c in range(8):
                    S.op("pe", "matmul", reads=["hT", "rw"], writes=["psY0"], signal=(c == 7), out=psY[0][:, 0:NEXP],
                         lhsT=hT[:, c, i * 128:(i + 1) * 128], rhs=rw[:, c, :], start=(c == 0), stop=(c == 7))
                S.op("dve", "tensor_copy", reads=["psY0"], writes=["lg"], out=lg[:], in_=psY[0][:, 0:NEXP])
                S.op("dve", "tensor_reduce", reads=["lg"], writes=["sm"], out=sm[:, 0:1], in_=lg[:], axis=AX.X,
                     op=ALU.max)
                S.op("dve", "tensor_scalar", reads=["lg", "sm"], writes=["eq1"], out=eq1[:], in0=lg[:],
                     scalar1=sm[:, 0:1], scalar2=None, op0=ALU.is_equal)
                S.op("dve", "scalar_tensor_tensor", reads=["eq1", "lg"], writes=["l2"], out=l2[:], in0=eq1[:],
                     scalar=-1e30, in1=lg[:], op0=ALU.mult, op1=ALU.add)
                S.op("dve", "tensor_reduce", reads=["l2"], writes=["sm"], out=sm[:, 1:2], in_=l2[:], axis=AX.X,
                     op=ALU.max)
                S.op("dve", "tensor_scalar", reads=["l2", "sm"], writes=["eq2"], out=eq2[:], in0=l2[:],
                     scalar1=sm[:, 1:2], scalar2=None, op0=ALU.is_equal)
                S.op("dve", "tensor_tensor", reads=["sm"], writes=["sm"], out=sm[:, 2:3], in0=sm[:, 1:2],
                     in1=sm[:, 0:1], op=ALU.subtract)
                S.op("act", "activation", reads=["sm"], writes=["sm"], out=sm[:, 3:4], in_=sm[:, 2:3], func=AF.Exp)
                S.op("dve", "tensor_scalar", reads=["sm"], writes=["sm"], out=sm[:, 4:5], in0=sm[:, 3:4], scalar1=1.0,
                     scalar2=None, op0=ALU.add)
                S.op("dve", "reciprocal", reads=["sm"], writes=["sm"], out=sm[:, 5:6], in_=sm[:, 4:5])
                S.op("dve", "tensor_tensor", reads=["sm"], writes=["sm"], out=sm[:, 6:7], in0=sm[:, 3:4],
                     in1=sm[:, 5:6], op=ALU.mult)
                S.op("dve", "tensor_scalar", reads=["eq1", "sm"], writes=[("gates", i)], out=gates[:, i, :],
                     in0=eq1[:], scalar1=sm[:, 5:6], scalar2=None, op0=ALU.mult)
                S.op("dve", "scalar_tensor_tensor", reads=["eq2", "sm", ("gates", i)], writes=[("gates", i)],
                     out=gates[:, i, :], in0=eq2[:], scalar=sm[:, 6:7], in1=gates[:, i, :], op0=ALU.mult,
                     op1=ALU.add)
            for e in range(NEXP):
                wi = load_w(e, 0)
                dcur = None
                for bi, (n0, nw) in enumerate(blocks):
                    cur = wi
                    if bi + 1 < len(blocks):
                        wi = load_w(e, bi + 1)
                    elif dcur is None:
                        pass
                    if bi == 2:
                        dcur = load_wd(e, 0)
                    for j in range(nw // 128):
                        f = n0 // 128 + j
                        for th in range(G // 512):
                            k = ctr["g"] % 2
                            ctr["g"] += 1
                            tok = slice(th * 512, (th + 1) * 512)
                            for c in range(8):
                                S.op("pe", "matmul", reads=["hT", "wg%d" % cur], writes=["psG%d" % k],
                                     signal=(c == 7), out=psG[k][:], lhsT=wg[cur][:, c, j * 128:(j + 1) * 128],
                                     rhs=hT[:, c, tok], start=(c == 0), stop=(c == 7))
                            for c in range(8):
                                S.op("pe", "matmul", reads=["hT", "wu%d" % cur], writes=["psU%d" % k],
                                     signal=(c == 7), out=psU[k][:], lhsT=wu[cur][:, c, j * 128:(j + 1) * 128],
                                     rhs=hT[:, c, tok], start=(c == 0), stop=(c == 7))
                            S.op("act", "activation", reads=["psG%d" % k], writes=["sg%d" % k], out=sg[k][:],
                                 in_=psG[k][:], func=AF.Silu)
                            S.op("dve", "tensor_tensor", reads=["sg%d" % k, "psU%d" % k], writes=[("hidT", f)],
                                 out=hidT[:, f, tok], in0=sg[k][:], in1=psU[k][:], op=ALU.mult)
                for hh in range(2):
                    dnext = load_wd(e, 1) if hh == 0 else None
                    for i in range(GT):
                        k = ctr["y"] % 2
                        ctr["y"] += 1
                        for f in range(NF):
                            S.op("pe", "matmul", reads=[("hidT", f), "wd%d" % dcur], writes=["psY%d" % k],
                                 signal=(f == NF - 1), out=psY[k][:], lhsT=hidT[:, f, i * 128:(i + 1) * 128],
                                 rhs=wd[dcur][:, f, :], start=(f == 0), stop=(f == NF - 1))
                        S.op("dve", "scalar_tensor_tensor", reads=["psY%d" % k, ("gates", i), ("xs", i)],
                             writes=[("xs", i)], out=xs[:, i, hh * 512:(hh + 1) * 512], in0=psY[k][:],
                             scalar=gates[:, i, e:e + 1], in1=xs[:, i, hh * 512:(hh + 1) * 512], op0=ALU.mult,
                             op1=ALU.add)
                    if dnext is not None:
                        dcur = dnext
            for i in range(GT):
                r0 = g0 + i * 128
                S.dma("sp", xo[r0:r0 + 128, :], xs[:, i, :], reads=[("xs", i)])
        S.barrier()


def phase_l1_moe_sparse(nc, S, T, x_in, x_out, nrows):
    ntile = nrows // 128
    maxgrp = (nrows + GRP - 1) // GRP
    NF = DFE // 128
    hs = nc.dram_tensor("moe_hs", [NEXP * ECAP, D], BF16)
    Y = nc.dram_tensor("moe_y", [NEXP * ECAP, D], F32)
    xin, xo = x_in.ap(), x_out.ap()
    with ExitStack() as st0:
        cx0 = Ctx(nc, S, st0, "m1")
        posb = cx0.sb("posb", [128, ntile, 2], I32)
        gtb = cx0.sb("gtb", [128, ntile, 2], F32)
        ngb = cx0.sb("ngb", [1, NEXP], I32)
        with ExitStack() as st:
            cx = Ctx(nc, S, st, "m1a")
            C = load_consts(nc, S, cx, T)
            gffn = cx.sb("gffn", [128, D], F32)
            S.dma("sp", gffn[:], bc_dram(T["l1_ffn_norm"], 128, D), writes=["gffn"])
            rw = cx.sb("rw", [128, 8, NEXP], BF16)
            S.dma("pool", rw[:], T["l1_router"].ap().rearrange("(c p) n -> p c n", p=128), writes=["rw"])
            triU = cx.sb("triU", [128, 128], BF16)
            S.dma("pool", triU[:], T["triU"].ap(), writes=["triU"])
            onesb = cx.sb("onesb", [128, 128], BF16)
            S.op("dve", "memset", writes=["onesb"], ap=onesb[:], constant=1.0)
            eoff = cx.sb("eoff", [128, NEXP], F32)
            S.dma("sp", eoff[:], T["eoff"].ap(), writes=["eoff"])
            base = cx.sb("base", [128, NEXP], F32)
            S.op("dve", "memset", writes=["base"], ap=base[:], constant=0.0)
            xs = [cx.sb("xs%d" % i, [128, D], F32) for i in range(2)]
            xn = [cx.sb("xn%d" % i, [128, D], BF16) for i in range(3)]
            W = {"junk": cx.sb("junk", [128, D], BF16), "ss": cx.sb("ss", [128, 1], F32),
                 "rstd": cx.sb("rstd", [128, 1], F32), "lnt": cx.sb("lnt", [128, 1], F32)}
            NPB = 3
            small = []
            for k in range(NPB):
                small.append({
                    "lg": cx.sb("lg%d" % k, [128, NEXP], F32), "l2": cx.sb("l2%d" % k, [128, NEXP], F32),
                    "eq1": cx.sb("eq1%d" % k, [128, NEXP], F32), "eq2": cx.sb("eq2%d" % k, [128, NEXP], F32),
                    "selb": cx.sb("selb%d" % k, [128, NEXP], BF16), "dest": cx.sb("dest%d" % k, [128, NEXP], F32),
                    "tmp8": cx.sb("tmp8%d" % k, [128, NEXP], F32), "sm": cx.sb("sm%d" % k, [128, 12], F32),
                    "hT": cx.sb("hTr%d" % k, [128, 8, 128], BF16)})
            psT = [cx.ps("psT%d" % i, [128, 8, 128], BF16) for i in range(2)]
            psRs = [cx.ps("psR%d" % i, [128, 512], F32) for i in range(2)]
            psDs = [cx.ps("psD%d" % i, [128, 512], F32) for i in range(2)]
            psCs = [cx.ps("psC%d" % i, [128, 512], F32) for i in range(2)]
            for i in range(ntile):
                r0 = i * 128
                sb_ = small[i % NPB]
                lg, l2, eq1, eq2, selb, dest, tmp8, sm, hT = (sb_[k] for k in
                                                               ("lg", "l2", "eq1", "eq2", "selb", "dest", "tmp8", "sm",
                                                                "hT"))
                psR, psD, psC = psRs[i % 2], psDs[i % 2], psCs[i % 2]
                kk = lambda nm, i=i: nm + str(i % NPB)
                kp = lambda nm, i=i: nm + str(i % 2)
                xt, xk = xs[i % 2], "xs%d" % (i % 2)
                xnt, xnk = xn[i % 3], "xn%d" % (i % 3)
                S.dma("sp", xt[:], xin[r0:r0 + 128, :], writes=[xk])
                norm_tile(S, C, xt[:], xk, gffn[:], "gffn", xnt[:], xnk, W, "")
                k = i % 2
                transposes(S, C, [xnt[:, c * 128:(c + 1) * 128] for c in range(8)], xnk, psT[k], "psT%d" % k, hT[:],
                           kk("hT"))
                for c in range(8):
                    S.op("pe", "matmul", reads=[kk("hT"), "rw"], writes=[kp("psR")], signal=(c == 7), out=psR[:, 0:NEXP],
                         lhsT=hT[:, c, :], rhs=rw[:, c, :], start=(c == 0), stop=(c == 7))
                S.op("dve", "tensor_copy", reads=[kp("psR")], writes=[kk("lg")], out=lg[:], in_=psR[:, 0:NEXP])
                S.op("dve", "tensor_reduce", reads=[kk("lg")], writes=[kk("sm")], out=sm[:, 0:1], in_=lg[:], axis=AX.X,
                     op=ALU.max)
                S.op("dve", "tensor_scalar", reads=[kk("lg"), kk("sm")], writes=[kk("eq1")], out=eq1[:], in0=lg[:],
                     scalar1=sm[:, 0:1], scalar2=None, op0=ALU.is_equal)
                S.op("dve", "scalar_tensor_tensor", reads=[kk("eq1"), kk("lg")], writes=[kk("l2")], out=l2[:], in0=eq1[:],
                     scalar=-1e30, in1=lg[:], op0=ALU.mult, op1=ALU.add)
                S.op("dve", "tensor_reduce", reads=[kk("l2")], writes=[kk("sm")], out=sm[:, 1:2], in_=l2[:], axis=AX.X,
                     op=ALU.max)
                S.op("dve", "tensor_scalar", reads=[kk("l2"), kk("sm")], writes=[kk("eq2")], out=eq2[:], in0=l2[:],
                     scalar1=sm[:, 1:2], scalar2=None, op0=ALU.is_equal)
                S.op("dve", "tensor_tensor", reads=[kk("sm")], writes=[kk("sm")], out=sm[:, 2:3], in0=sm[:, 1:2],
                     in1=sm[:, 0:1], op=ALU.subtract)
                S.op("act", "activation", reads=[kk("sm")], writes=[kk("sm")], out=sm[:, 3:4], in_=sm[:, 2:3], func=AF.Exp)
                S.op("dve", "tensor_scalar", reads=[kk("sm")], writes=[kk("sm")], out=sm[:, 4:5], in0=sm[:, 3:4], scalar1=1.0,
                     scalar2=None, op0=ALU.add)
                S.op("dve", "reciprocal", reads=[kk("sm")], writes=[("gtb", i)], out=gtb[:, i, 0:1], in_=sm[:, 4:5])
                S.op("dve", "tensor_tensor", reads=[kk("sm"), ("gtb", i)], writes=[("gtb", i)], out=gtb[:, i, 1:2],
                     in0=sm[:, 3:4], in1=gtb[:, i, 0:1], op=ALU.mult)
                S.op("dve", "tensor_tensor", reads=[kk("eq1"), kk("eq2")], writes=[kk("selb")], out=selb[:], in0=eq1[:],
                     in1=eq2[:], op=ALU.add)
                S.op("pe", "matmul", reads=[kk("selb"), "triU"], writes=[kp("psD")], out=psD[:, 0:NEXP], lhsT=triU[:],
                     rhs=selb[:], start=True, stop=True)
                S.op("pe", "matmul", reads=[kk("selb"), "onesb"], writes=[kp("psC")], out=psC[:, 0:NEXP], lhsT=onesb[:],
                     rhs=selb[:], start=True, stop=True)
                S.op("dve", "tensor_tensor", reads=[kp("psD"), "base"], writes=[kk("dest")], out=dest[:], in0=psD[:, 0:NEXP],
                     in1=base[:], op=ALU.add)
                S.op("dve", "tensor_tensor", reads=[kp("psC"), "base"], writes=["base"], out=base[:], in0=psC[:, 0:NEXP],
                     in1=base[:], op=ALU.add)
                S.op("dve", "tensor_tensor", reads=[kk("dest"), "eoff"], writes=[kk("dest")], out=dest[:], in0=dest[:],
                     in1=eoff[:], op=ALU.add)
                for (j, eq, eqk) in ((0, eq1, kk("eq1")), (1, eq2, kk("eq2"))):
                    S.op("dve", "tensor_tensor", reads=[kk("dest"), eqk], writes=[kk("tmp8")], out=tmp8[:], in0=dest[:],
                         in1=eq[:], op=ALU.mult)
                    S.op("dve", "tensor_reduce", reads=[kk("tmp8")], writes=[kk("sm")], out=sm[:, 8 + j:9 + j], in_=tmp8[:],
                         axis=AX.X, op=ALU.add)
                S.op("dve", "tensor_copy", reads=[kk("sm")], writes=[("posb", i)], out=posb[:, i, :], in_=sm[:, 8:10])
                for j in range(2):
                    S.dma("pool", hs.ap(), xnt[:], reads=[xnk, ("posb", i)],
                          meth="indirect_dma_start", out_offset=bass.IndirectOffsetOnAxis(posb[:, i, j:j + 1], 0),
                          in_offset=None, bounds_check="BC", oob_is_err=False)
            sb_ = small[0]
            lg, l2, eq1, eq2, selb, dest, tmp8, sm, hT = (sb_[k] for k in
                                                           ("lg", "l2", "eq1", "eq2", "selb", "dest", "tmp8", "sm", "hT"))
            kk = lambda nm: nm + "0"
            S.op("dve", "tensor_scalar", reads=["base"], writes=[kk("tmp8")], out=tmp8[0:1, :], in0=base[0:1, :],
                 scalar1=1.0 / GRP, scalar2=(GRP - 1.0) / GRP, op0=ALU.mult, op1=ALU.add)
            ngi = cx.sb("ngi", [1, NEXP], I32)
            S.op("dve", "tensor_copy", reads=[kk("tmp8")], writes=["ngi"], out=ngi[:], in_=tmp8[0:1, :])
            S.op("dve", "tensor_copy", reads=["ngi"], writes=[kk("dest")], out=dest[0:1, :], in_=ngi[:])
            S.op("dve", "tensor_tensor", reads=[kk("dest"), kk("tmp8")], writes=[kk("l2")], out=l2[0:1, :], in0=dest[0:1, :],
                 in1=tmp8[0:1, :], op=ALU.is_gt)
            S.op("dve", "tensor_tensor", reads=[kk("dest"), kk("l2")], writes=[kk("dest")], out=dest[0:1, :], in0=dest[0:1, :],
                 in1=l2[0:1, :], op=ALU.subtract)
            S.op("dve", "tensor_copy", reads=[kk("dest")], writes=["ngb"], out=ngb[:], in_=dest[0:1, :])
            S.barrier()
        with ExitStack() as st:
            cx = Ctx(nc, S, st, "m1b")
            C = load_consts(nc, S, cx, T)
            hr = [cx.sb("hr%d" % i, [128, 4, D], BF16) for i in range(2)]
            hTg = cx.sb("hTg", [128, 8, GRP], BF16)
            hidT = cx.sb("hidT", [128, NF, GRP], BF16)
            wg = [cx.sb("wg%d" % i, [128, 8, 512], BF16) for i in range(2)]
            wu = [cx.sb("wu%d" % i, [128, 8, 512], BF16) for i in range(2)]
            wd = [cx.sb("wd%d" % i, [128, NF, 512], BF16) for i in range(2)]
            sg = [cx.sb("sg%d" % i, [128, 512], F32) for i in range(2)]
            yo = [cx.sb("yo%d" % i, [128, D], F32) for i in range(2)]
            psT = [cx.ps("psT%d" % i, [128, 8, 128], BF16) for i in range(2)]
            psG = [cx.ps("psG%d" % i, [128, 512], F32) for i in range(2)]
            psU = [cx.ps("psU%d" % i, [128, 512], F32) for i in range(2)]
            psY = [cx.ps("psY%d" % i, [128, 512], F32) for i in range(2)]
            wgd, wud, wdd = T["l1_we_gate_bf"].ap(), T["l1_we_up_bf"].ap(), T["l1_we_down_bf"].ap()
            ctr = {"t": 0, "g": 0, "y": 0, "w": 0, "h": 0, "o": 0}

            def load_w(e, bi):
                n0 = bi * 512
                i = ctr["w"] % 2
                ctr["w"] += 1
                S.dma("sp", wg[i][:], wgd[e, :, n0:n0 + 512].rearrange("(c p) n -> p c n", p=128),
                      writes=["wg%d" % i])
                S.dma("sp", wu[i][:], wud[e, :, n0:n0 + 512].rearrange("(c p) n -> p c n", p=128),
                      writes=["wu%d" % i])
                return i

            def load_wd(e):
                for hh in range(2):
                    src = wdd[e, :, hh * 512:(hh + 1) * 512].rearrange("(f p) n -> p f n", p=128)
                    for f0 in range(0, NF, 14):
                        S.dma("pool", wd[hh][:, f0:f0 + 14, :], src[:, f0:f0 + 14, :], writes=["wd%d" % hh])

            for e in range(NEXP):
                S.reg_load_all(ngb[0:1, e:e + 1], reads=["ngb"])
                for r in range(maxgrp):
                    S.cond_begin(r + 1)
                    R0 = e * ECAP + r * GRP
                    hb = ctr["h"] % 2
                    ctr["h"] += 1
                    S.dma("sp", hr[hb][:], hs.ap()[R0:R0 + GRP, :].rearrange("(j p) d -> p j d", p=128),
                          writes=["hr%d" % hb])
                    wi = load_w(e, 0)
                    for j in range(4):
                        k = ctr["t"] % 2
                        ctr["t"] += 1
                        transposes(S, C, [hr[hb][:, j, c * 128:(c + 1) * 128] for c in range(8)], "hr%d" % hb, psT[k],
                                   "psT%d" % k, hTg[:, :, j * 128:(j + 1) * 128], "hTg",
                                   evac=("dve" if j % 2 == 0 else "act"))
                    for bi in range(7):
                        cur = wi
                        if bi + 1 < 7:
                            wi = load_w(e, bi + 1)
                        if bi == 1:
                            load_wd(e)
                        for j in range(4):
                            f = bi * 4 + j
                            k = ctr["g"] % 2
                            ctr["g"] += 1
                            for c in range(8):
                                S.op("pe", "matmul", reads=["hTg", "wg%d" % cur], writes=["psG%d" % k],
                                     signal=(c == 7), out=psG[k][:], lhsT=wg[cur][:, c, j * 128:(j + 1) * 128],
                                     rhs=hTg[:, c, :], start=(c == 0), stop=(c == 7))
                            for c in range(8):
                                S.op("pe", "matmul", reads=["hTg", "wu%d" % cur], writes=["psU%d" % k],
                                     signal=(c == 7), out=psU[k][:], lhsT=wu[cur][:, c, j * 128:(j + 1) * 128],
                                     rhs=hTg[:, c, :], start=(c == 0), stop=(c == 7))
                            S.op("act", "activation", reads=["psG%d" % k], writes=["sg%d" % k], out=sg[k][:],
                                 in_=psG[k][:], func=AF.Silu)
                            S.op("dve", "tensor_tensor", reads=["sg%d" % k, "psU%d" % k], writes=[("hidT", f)],
                                 out=hidT[:, f, :], in0=sg[k][:], in1=psU[k][:], op=ALU.mult)
                    for j in range(4):
                        ob = ctr["o"] % 2
                        ctr["o"] += 1
                        for hh in range(2):
                            k = ctr["y"] % 2
                            ctr["y"] += 1
                            for f in range(NF):
                                S.op("pe", "matmul", reads=[("hidT", f), "wd%d" % hh], writes=["psY%d" % k],
                                     signal=(f == NF - 1), out=psY[k][:], lhsT=hidT[:, f, j * 128:(j + 1) * 128],
                                     rhs=wd[hh][:, f, :], start=(f == 0), stop=(f == NF - 1))
                            if hh == 0:
                                S.op("act", "copy", reads=["psY%d" % k], writes=["yo%d" % ob],
                                     out=yo[ob][:, 0:512], in_=psY[k][:])
                            else:
                                S.op("dve", "tensor_copy", reads=["psY%d" % k], writes=["yo%d" % ob],
                                     out=yo[ob][:, 512:1024], in_=psY[k][:])
                        S.dma("pool", Y.ap()[R0 + j * 128:R0 + (j + 1) * 128, :], yo[ob][:], reads=["yo%d" % ob])
                    S.cond_end()
            S.barrier()
        with ExitStack() as st:
            cx = Ctx(nc, S, st, "m1c")
            xs = [cx.sb("xs%d" % i, [128, D], F32) for i in range(2)]
            y1 = [cx.sb("y1_%d" % i, [128, D], F32) for i in range(2)]
            y2 = [cx.sb("y2_%d" % i, [128, D], F32) for i in range(2)]
            for i in range(ntile):
                r0 = i * 128
                k = i % 2
                S.dma("sp", xs[k][:], xin[r0:r0 + 128, :], writes=["xs%d" % k])
                for (j, yb, nm) in ((0, y1[k], "y1_%d" % k), (1, y2[k], "y2_%d" % k)):
                    S.dma("pool", yb[:], Y.ap(), reads=[("posb", i)], writes=[nm], meth="indirect_dma_start",
                          out_offset=None, in_offset=bass.IndirectOffsetOnAxis(posb[:, i, j:j + 1], 0),
                          bounds_check="BC", oob_is_err=False)
                S.op("dve", "scalar_tensor_tensor", reads=["y1_%d" % k, "xs%d" % k], writes=["xs%d" % k],
                     out=xs[k][:], in0=y1[k][:], scalar=gtb[:, i, 0:1], in1=xs[k][:], op0=ALU.mult, op1=ALU.add)
                S.op("dve", "scalar_tensor_tensor", reads=["y2_%d" % k, "xs%d" % k], writes=["xs%d" % k],
                     out=xs[k][:], in0=y2[k][:], scalar=gtb[:, i, 1:2], in1=xs[k][:], op0=ALU.mult, op1=ALU.add)
                S.dma("sp", xo[r0:r0 + 128, :], xs[k][:], reads=["xs%d" % k])
            S.barrier()


WEIGHT_NAMES = [
    ("l0_attn_norm", [D]), ("l0_w_in", [D, L0_IN]), ("l0_a_q_norm", [64]), ("l0_a_k_norm", [64]),
    ("l0_a_sinks", [8]), ("l0_b_q_norm", [64]), ("l0_b_k_norm", [64]), ("l0_w_out", [D, D]),
    ("l0_ffn_norm", [D]), ("l0_w_gate", [D, DFF]), ("l0_w_up", [D, DFF]), ("l0_w_down", [DFF, D]),
    ("l1_attn_norm", [D]), ("l1_w_in", [D, L1_IN]), ("l1_q_a_norm", [384]), ("l1_w_uq", [384, 1536]),
    ("l1_kv_a_norm", [256]), ("l1_w_ukv", [256, 2048]), ("l1_c_q_norm", [96]), ("l1_c_k_norm", [96]),
    ("l1_w_out", [D, D]), ("l1_ffn_norm", [D]), ("l1_router", [D, NEXP]),
    ("l1_we_gate", [NEXP, D, DFE]), ("l1_we_up", [NEXP, D, DFE]), ("l1_we_down", [NEXP, DFE, D]),
]


def host_consts():
    p = np.arange(128)[:, None]
    j = np.arange(128)[None, :]
    NEG = -10000.0
    maskA = np.where(np.stack([(j < p), (j >= p)], axis=1), 0.0, NEG).astype(np.float32)
    mb = np.zeros((128, 16, 128), np.float32)
    for o in range(16):
        d = o * 128 + j - p
        c = ((d >= 0) & (d <= 128)).astype(np.float32) + ((d >= 0) & (d % 4 == 0) & (d <= 512)) + \
            ((d >= 0) & (d % 16 == 0) & (d <= 2048))
        with np.errstate(divide="ignore"):
            mb[:, 15 - o, :] = np.where(c > 0, np.log(np.maximum(c, 1.0)) / 0.125, NEG)
    inv_h = (10000.0 ** (-np.arange(0, 64, 2, dtype=np.float32) / 64)).astype(np.float32)
    inv_c = (10000.0 ** (-np.arange(0, 32, 2, dtype=np.float32) / 32)).astype(np.float32)
    return {
        "ident": np.eye(128, dtype=np.float32),
        "maskA": maskA,
        "maskB": mb,
        "inv_h": np.ascontiguousarray(np.broadcast_to(inv_h[None, :], (128, 32))),
        "inv_c": np.ascontiguousarray(np.broadcast_to(inv_c[None, :], (128, 16))),
        "triU": (p < j).astype(np.float32),
        "eoff": np.ascontiguousarray(np.broadcast_to((np.arange(8, dtype=np.float32) * 8192)[None, :], (128, 8))),
    }


def build_program(nseq, phases, ntiles=NT):
    nc = bass.Bass("TRN2", target_bir_lowering=False)
    T = {}
    rows = nseq * SEQ
    T["x"] = nc.dram_tensor("x", [rows, D], F32, kind="ExternalInput")
    T["pos_t"] = nc.dram_tensor("pos_t", [nseq, 128, NT], I32, kind="ExternalInput")
    for nm, shp in WEIGHT_NAMES:
        T[nm] = nc.dram_tensor(nm, shp, F32, kind="ExternalInput")
    for nm, arr in host_consts().items():
        T[nm] = nc.dram_tensor(nm, list(arr.shape), F32, kind="ExternalInput")
    T["out"] = nc.dram_tensor("out", [rows, D], F32, kind="ExternalOutput")
    nr = ntiles * 128 if ntiles < NT else rows
    with ExitStack() as st:
        S = Sched(nc, st)
        if "f0" in phases and "a0" in phases:
            for nm, shp in (("l0_w_gate", [D, DFF]), ("l0_w_up", [D, DFF]), ("l0_w_down", [DFF, D])):
                T[nm + "_bf"] = nc.dram_tensor(nm + "_bf", shp, BF16)
                wr = shp[0]
                for r0 in range(0, wr, 512):
                    rc = min(512, wr - r0)

                    def job(nm=nm, r0=r0, rc=rc, ncol=shp[1]):
                        src = T[nm].ap()[r0:r0 + rc, :].rearrange("r (a b) -> r a b", b=256)
                        dst = T[nm + "_bf"].ap()[r0:r0 + rc, :].rearrange("r (a b) -> r a b", b=256)
                        S.dma("pool", dst, src)
                    S.bg_jobs.append(job)
        if "m1" in phases:
            for nm, shp in (("l1_we_gate", [NEXP, D, DFE]), ("l1_we_up", [NEXP, D, DFE]),
                            ("l1_we_down", [NEXP, DFE, D])):
                T[nm + "_bf"] = nc.dram_tensor(nm + "_bf", shp, BF16)
            for e in range(NEXP):
                for nm, wrows in (("l1_we_gate", D), ("l1_we_up", D), ("l1_we_down", DFE)):
                    nchunk = 4
                    rc = wrows // nchunk
                    for ci in range(nchunk):
                        def job(nm=nm, e=e, r0=ci * rc, rc=rc):
                            src = T[nm].ap()[e, r0:r0 + rc, :].rearrange("r (a b) -> r a b", b=512)
                            dst = T[nm + "_bf"].ap()[e, r0:r0 + rc, :].rearrange("r (a b) -> r a b", b=512)
                            S.dma("pool", dst, src)
                        S.bg_jobs.append(job)
        cur = T["x"]
        for i, ph in enumerate(phases):
            dst = T["out"] if i == len(phases) - 1 else nc.dram_tensor("xr%d" % i, [rows, D], F32)
            if ph == "a0":
                phase_l0_attn(nc, S, T, cur, dst, nseq, ntiles)
            elif ph == "f0":
                phase_l0_ffn(nc, S, T, cur, dst, nr)
            elif ph == "a1":
                phase_l1_attn(nc, S, T, cur, dst, nseq, ntiles)
            elif ph == "m1":
                S.bg_step(len(S.bg_jobs))
                phase_l1_moe_sparse(nc, S, T, cur, dst, nr)
            elif ph == "m1d":
                phase_l1_moe(nc, S, T, cur, dst, nr)
            cur = dst
        S.finish()
        S.emit()
    return nc


PHASES = ["a0", "f0", "a1", "m1"]
N_CORES = 8
_PROG = {}


def kernel(**inputs):
    x = np.asarray(inputs["x"], dtype=np.float32)
    pos = np.asarray(inputs["positions"]).astype(np.int32)
    B = x.shape[0]
    nseq = B // N_CORES
    if nseq not in _PROG:
        _PROG[nseq] = build_program(nseq, PHASES)
    nc = _PROG[nseq]
    consts = host_consts()
    shared = {nm: np.ascontiguousarray(np.asarray(inputs[nm], dtype=np.float32)) for nm, _ in WEIGHT_NAMES}
    shared.update(consts)
    in_maps = []
    for c in range(N_CORES):
        m = dict(shared)
        m["x"] = np.ascontiguousarray(x[c * nseq:(c + 1) * nseq].reshape(nseq * SEQ, D))
        m["pos_t"] = np.ascontiguousarray(pos[c * nseq:(c + 1) * nseq].reshape(nseq, NT, 128).transpose(0, 2, 1))
        in_maps.append(m)
    res = run_bass_kernel_spmd(nc, in_maps, core_ids=list(range(N_CORES)))
    outs = [np.asarray(r["out"]).reshape(nseq, SEQ, D) for r in res.results]
    return np.concatenate(outs, axis=0).astype(np.float32)
```

```python
import numpy as np
from contextlib import ExitStack
import concourse.bass as bass
import concourse.mybir as mybir
from concourse.bass_utils import run_bass_kernel_spmd

F32 = mybir.dt.float32
BF16 = mybir.dt.bfloat16
I32 = mybir.dt.int32
AF = mybir.ActivationFunctionType
ALU = mybir.AluOpType
AX = mybir.AxisListType

D = 1024
SEQ = 2048
NT = SEQ // 128
L0_IN = 2304
DFF = 2816
L1_IN = 672
NEXP = 8
DFE = 3584
EPS = 1e-6
PI = float(np.pi)
ECAP = 8192
GRP = 512


class Sched:
    def __init__(self, nc, stack, ring=10):
        self.nc = nc
        self.eng = {"pe": nc.tensor, "act": nc.scalar, "dve": nc.vector, "pool": nc.gpsimd, "sp": nc.sync}
        self.sem = {k: stack.enter_context(nc.semaphore("s_" + k)) for k in self.eng}
        self.cnt = {k: 0 for k in self.eng}
        self.seen = {k: {} for k in self.eng}
        self.rings = {}
        for q in ("sp", "pool"):
            self.rings[q] = [[stack.enter_context(nc.semaphore("d_%s%d" % (q, i))), 0] for i in range(ring)]
        self.ring_pos = {q: 0 for q in self.rings}
        self.lastw = {}
        self.readers = {}
        self.prog = {k: [] for k in self.eng}
        self.dma_toks = []
        self.bg_jobs = []

    def bg_step(self, n=1):
        for _ in range(n):
            if self.bg_jobs:
                self.bg_jobs.pop(0)()

    def _wait(self, e, tok):
        sem, val, owner = tok
        if owner == e and e == "pe":
            return
        name = id(sem)
        if self.seen[e].get(name, 0) >= val:
            return
        self.prog[e].append(("w", sem, val))
        self.seen[e][name] = val

    def _deps(self, e, reads, writes):
        for k in reads:
            t = self.lastw.get(k)
            if t is not None:
                self._wait(e, t)
        for k in writes:
            t = self.lastw.get(k)
            if t is not None:
                self._wait(e, t)
            for t in self.readers.get(k, ()):
                self._wait(e, t)

    def _commit(self, tok, reads, writes):
        for k in reads:
            self.readers.setdefault(k, []).append(tok)
        for k in writes:
            self.lastw[k] = tok
            self.readers[k] = []

    def op(self, e, meth, reads=(), writes=(), signal=True, **kw):
        self._deps(e, reads, writes)
        if signal:
            self.cnt[e] += 1
            self.prog[e].append(("i", meth, kw, self.sem[e], 1))
            tok = (self.sem[e], self.cnt[e], e)
        else:
            self.prog[e].append(("i", meth, kw, None, 0))
            tok = (self.sem[e], self.cnt[e] + 1, e)
        self._commit(tok, reads, writes)
        return tok

    def dma(self, q, out, in_, reads=(), writes=(), meth="dma_start", **kw):
        self._deps(q, reads, writes)
        ring = self.rings[q]
        i = self.ring_pos[q]
        self.ring_pos[q] = (i + 1) % len(ring)
        sem, n = ring[i]
        if n > 0:
            self._wait(q, (sem, 16 * n, "dma"))
        self.prog[q].append(("i", meth, dict(out=out, in_=in_, **kw), sem, 16))
        ring[i][1] = n + 1
        tok = (sem, 16 * (n + 1), "dma")
        self._commit(tok, reads, writes)
        self.dma_toks.append(tok)
        return tok

    def reg_load_all(self, ap, reads=()):
        for e in self.eng:
            self._deps(e, reads, ())
            self.prog[e].append(("rl", ap))
            tok = (self.sem[e], self.cnt[e] + 1, e) if False else None

    def cond_begin(self, thresh):
        self.cond = {}
        start = {id(self.sem[o]): self.cnt[o] for o in self.eng}
        for q in self.rings:
            for sem, n in self.rings[q]:
                start[id(sem)] = 16 * n
        self.cond_start = start
        for e in self.eng:
            info = {"thresh": thresh, "pos": len(self.prog[e]), "cnt0": self.cnt[e]}
            if e in self.rings:
                info["ring0"] = [n for (_, n) in self.rings[e]]
            self.prog[e].append(("cb", info))
            self.cond[e] = info

    def cond_end(self):
        for e in self.eng:
            info = self.cond[e]
            body = self.prog[e][info["pos"] + 1:]
            info["waits"] = [(it[1], it[2]) for it in body if it[0] == "w" and it[2] <= self.cond_start[id(it[1])]]
            info["delta"] = self.cnt[e] - info["cnt0"]
            if e in self.rings:
                info["rings"] = [(sem, n0, n - n0) for (sem, n), n0 in zip(self.rings[e], info["ring0"])]
            self.prog[e].append(("ce",))
        self.cond = None

    def barrier(self):
        toks = [(self.sem[o], self.cnt[o], o) for o in self.eng if self.cnt[o] > 0]
        for q in self.rings:
            for sem, n in self.rings[q]:
                if n > 0:
                    toks.append((sem, 16 * n, "dma"))
        for e in self.eng:
            for t in toks:
                if t[2] == e:
                    continue
                self._wait(e, t)
        self.lastw.clear()
        self.readers.clear()
        self.dma_toks = []

    def finish(self):
        for sem, n in self.rings["sp"] + self.rings["pool"]:
            if n > 0:
                self._wait("sp", (sem, 16 * n, "dma"))

    def emit(self):
        nc = self.nc
        with nc.Block() as block:
            def mk(name):
                def body(e):
                    reg = None
                    guard = None
                    bcreg = None
                    if name == "pool":
                        bcreg = e.alloc_register("bc")
                        e.reg_mov(bcreg, NEXP * ECAP - 1)
                    for it in self.prog[name]:
                        if it[0] == "w":
                            e.wait_ge(it[1], it[2])
                        elif it[0] == "rl":
                            if reg is None:
                                reg = e.alloc_register("pred_" + name)
                            e.reg_load(reg, it[1])
                        elif it[0] == "cb":
                            info = it[1]
                            g = e.If_lt(reg, info["thresh"])
                            g.__enter__()
                            for (sem, val) in info["waits"]:
                                e.wait_ge(sem, val)
                            if info["delta"] > 0:
                                if info["cnt0"] > 0:
                                    e.wait_ge(self.sem[name], info["cnt0"])
                                e.sem_inc(self.sem[name], info["delta"])
                            for (sem, n0, dn) in info.get("rings", ()):
                                if dn > 0:
                                    if n0 > 0:
                                        e.wait_ge(sem, 16 * n0)
                                    e.sem_inc(sem, 16 * dn)
                            g.__exit__(None, None, None)
                            guard = e.Else()
                            guard.__enter__()
                        elif it[0] == "ce":
                            guard.__exit__(None, None, None)
                            guard = None
                        else:
                            kw = it[2]
                            if kw.get("bounds_check", None) == "BC":
                                kw = dict(kw)
                                kw["bounds_check"] = bcreg
                            ins = getattr(e, it[1])(**kw)
                            if it[3] is not None:
                                ins.then_inc(it[3], it[4])
                return body
            block.sync(mk("sp"))
            block.scalar(mk("act"))
            block.vector(mk("dve"))
            block.gpsimd(mk("pool"))
            block.tensor(mk("pe"))


class Ctx:
    def __init__(self, nc, S, stack, pfx):
        self.nc, self.S, self.st, self.pfx = nc, S, stack, pfx

    def sb(self, name, shape, dt):
        return self.st.enter_context(self.nc.sbuf_tensor(self.pfx + "s_" + name, shape, dt))

    def ps(self, name, shape, dt):
        return self.st.enter_context(self.nc.psum_tensor(self.pfx + "p_" + name, shape, dt))


def bc_dram(handle, parts, inner):
    return bass.AP(handle, 0, [[0, parts], [1, inner]])


def load_consts(nc, S, cx, T, need_mb=False, need_ma=False):
    C = {}
    C["ident"] = cx.sb("ident", [128, 128], BF16)
    S.dma("pool", C["ident"][:], T["ident"].ap(), writes=["ident"])
    C["eps"] = cx.sb("eps", [128, 1], F32)
    S.op("dve", "memset", writes=["eps"], ap=C["eps"][:], constant=EPS)
    C["pib"] = cx.sb("pib", [128, 1], F32)
    S.op("dve", "memset", writes=["pib"], ap=C["pib"][:], constant=PI)
    if need_ma:
        C["MA"] = cx.sb("MA", [128, 2, 128], BF16)
        S.dma("pool", C["MA"][:], T["maskA"].ap(), writes=["masks"])
    if need_mb:
        C["MB"] = cx.sb("MB", [128, 16, 128], BF16)
        S.dma("pool", C["MB"][:], T["maskB"].ap(), writes=["masks"])
    return C


def rms_rstd(S, ss_ap, out_ap, tmp_ap, scale, eps_ap, keys_r, keys_w, tmpkey):
    S.op("act", "activation", reads=list(keys_r) + ["eps"], writes=[tmpkey], out=tmp_ap, in_=ss_ap, func=AF.Ln,
         scale=scale, bias=eps_ap)
    S.op("act", "activation", reads=[tmpkey], writes=list(keys_w), out=out_ap, in_=tmp_ap, func=AF.Exp, scale=-0.5)


def norm_tile(S, C, xt, xkey, gain, gkey, xn, xnkey, W, sfx):
    S.op("dve", "scalar_tensor_tensor", reads=[xkey], writes=["junk" + sfx, "ss" + sfx], out=W["junk"][:, 0:D], in0=xt,
         scalar=1.0, in1=xt, op0=ALU.mult, op1=ALU.mult, accum_out=W["ss"][:])
    rms_rstd(S, W["ss"][:], W["rstd"][:], W["lnt"][:], 1.0 / D, C["eps"][:], ["ss" + sfx], ["rstd" + sfx],
             "lnt" + sfx)
    S.op("dve", "scalar_tensor_tensor", reads=[xkey, "rstd" + sfx, gkey], writes=[xnkey], out=xn, in0=xt,
         scalar=W["rstd"][:, 0:1], in1=gain, op0=ALU.mult, op1=ALU.mult)


def transposes(S, C, src_aps, srckey, psT, pskey, dst_ap, dstkey, nrows=128, evac="dve"):
    n = len(src_aps)
    for i, a in enumerate(src_aps):
        w = a.shape[-1] if len(a.shape) == 2 else None
        S.op("pe", "transpose", reads=[srckey, "ident"], writes=[pskey], signal=(i == n - 1),
             out=psT[0:nrows, i, :], in_=a, identity=C["ident"][:])
    if evac == "dve":
        S.op("dve", "tensor_copy", reads=[pskey], writes=[dstkey], out=dst_ap, in_=psT[0:nrows, 0:n, :])
    else:
        S.op("act", "copy", reads=[pskey], writes=[dstkey], out=dst_ap, in_=psT[0:nrows, 0:n, :])


def rope_tables(S, C, cx, pos_ap, invt, nfreq, sfx):
    W = C["rope" + sfx]
    AP_ = lambda x: x if isinstance(x, bass.AP) else x[:]
    S.dma("sp", AP_(W["posi"]), pos_ap, writes=["posi" + sfx])
    S.op("dve", "tensor_copy", reads=["posi" + sfx], writes=["posf" + sfx], out=AP_(W["posf"]), in_=AP_(W["posi"]))
    pf = AP_(W["posf"]).unsqueeze(2).to_broadcast([128, NT, nfreq])
    iv = invt[:].unsqueeze(1).to_broadcast([128, NT, nfreq])
    S.op("dve", "tensor_tensor", reads=["posf" + sfx, "inv" + sfx], writes=["ang" + sfx], out=AP_(W["ang"]), in0=pf,
         in1=iv, op=ALU.mult)
    for (nm, shift) in (("sin", 0.0), ("cos", PI / 2)):
        S.op("dve", "tensor_scalar", reads=["ang" + sfx], writes=["rr" + sfx], out=AP_(W["rr"]), in0=AP_(W["ang"]),
             scalar1=shift, scalar2=1.0 / (2 * PI), op0=ALU.add, op1=ALU.mult)
        S.op("dve", "tensor_copy", reads=["rr" + sfx], writes=["ki" + sfx], out=AP_(W["ki"]), in_=AP_(W["rr"]))
        S.op("dve", "tensor_copy", reads=["ki" + sfx], writes=["kf" + sfx], out=AP_(W["kf"]), in_=AP_(W["ki"]))
        S.op("dve", "tensor_scalar", reads=["ang" + sfx], writes=["rr" + sfx], out=AP_(W["rr"]), in0=AP_(W["ang"]),
             scalar1=shift, scalar2=None, op0=ALU.add)
        S.op("dve", "scalar_tensor_tensor", reads=["kf" + sfx, "rr" + sfx], writes=["rr" + sfx], out=AP_(W["rr"]),
             in0=AP_(W["kf"]), scalar=-2 * PI, in1=AP_(W["rr"]), op0=ALU.mult, op1=ALU.add)
        S.op("dve", "tensor_scalar", reads=["rr" + sfx], writes=["kf" + sfx], out=AP_(W["kf"]), in0=AP_(W["rr"]),
             scalar1=PI, scalar2=None, op0=ALU.is_gt)
        S.op("dve", "scalar_tensor_tensor", reads=["kf" + sfx, "rr" + sfx], writes=["rr" + sfx], out=AP_(W["rr"]),
             in0=AP_(W["kf"]), scalar=-2 * PI, in1=AP_(W["rr"]), op0=ALU.mult, op1=ALU.add)
        S.op("dve", "tensor_scalar", reads=["rr" + sfx], writes=["rr" + sfx], out=AP_(W["rr"]), in0=AP_(W["rr"]),
             scalar1=PI, scalar2=-PI, op0=ALU.min, op1=ALU.max)
        S.op("act", "activation", reads=["rr" + sfx], writes=[nm + sfx], out=AP_(W[nm]), in_=AP_(W["rr"]), func=AF.Sin)


def alloc_rope(cx, nfreq, sfx, big=False):
    W = {
        "posi": cx.sb("posi" + sfx, [128, NT], I32),
        "posf": cx.sb("posf" + sfx, [128, NT], F32),
        "ki": cx.sb("ki" + sfx, [128, NT, nfreq], I32),
        "sin": cx.sb("sin" + sfx, [128, NT, nfreq], F32),
        "cos": cx.sb("cos" + sfx, [128, NT, nfreq], F32),
    }
    for nm in ("ang", "rr", "kf"):
        if big:
            full = cx.sb(nm + sfx, [128, NT, 2 * nfreq], F32)
            W[nm + "_full"] = full
            W[nm] = full[:, :, 0:nfreq]
        else:
            W[nm] = cx.sb(nm + sfx, [128, NT, nfreq], F32)
    return W


def make_jobs(head, kbs):
    jobs = []
    nkb = len(kbs)
    for c0 in range(0, nkb, 4):
        jobs.append({"h": head, "c0": c0, "chunk": kbs[c0:c0 + 4], "nkb": nkb})
    return jobs


def job_S(S, bufs, ctr, job):
    h = job["h"]
    psS, pT = bufs["psS"], bufs["pT"]
    i_s = ctr["s"] % len(psS)
    ctr["s"] += 1
    i_p = ctr["p"] % len(pT)
    ctr["p"] += 1
    ps, pskey = psS[i_s], "psS%d" % i_s
    pt, ptkey = pT[i_p], "pT%d" % i_p
    job["pt"], job["ptkey"] = pt, ptkey
    chunk = job["chunk"]
    n = len(chunk)
    madd = h["mask_fn"](job["c0"], chunk)
    lo, hi = (madd[0], madd[1]) if madd is not None else (0, 0)
    order = [i for i in range(n) if not (lo <= i < hi)] + [i for i in range(n) if lo <= i < hi]
    for pos, i in enumerate(order):
        kb = chunk[i]
        masked = lo <= i < hi
        if masked and i == lo:
            S.op("pe", "matmul", reads=["ident", "masks"], writes=[pskey], signal=False,
                 out=ps[:, lo * 128:hi * 128], lhsT=bufs["ident"], rhs=madd[2], start=True, stop=False)
        S.op("pe", "matmul", reads=[h["qkey"], h["kkey"](kb)], writes=[pskey], signal=(pos == n - 1),
             out=ps[:, i * 128:(i + 1) * 128], lhsT=h["kT_fn"](kb), rhs=h["q_ap"], start=(not masked),
             stop=((not masked) or i == hi - 1))
    S.op("act", "activation", reads=[pskey], writes=[ptkey], out=pt[:, 0:n * 128], in_=ps[:, 0:n * 128],
         func=AF.Exp, scale=h["scale"])


def job_PV(S, bufs, ctr, job):
    h = job["h"]
    psO = bufs["psO"]
    if job["c0"] == 0:
        io = ctr["o"] % len(psO)
        ctr["o"] += 1
        h["po"], h["pokey"] = psO[io], "psO%d" % io
    po, pokey = h["po"], h["pokey"]
    chunk = job["chunk"]
    n = len(chunk)
    pt, ptkey = job["pt"], job["ptkey"]
    for i, kb in enumerate(chunk):
        gi = job["c0"] + i
        S.op("pe", "matmul", reads=[ptkey, h["vkey"](kb)], writes=[pokey], signal=(i == n - 1),
             out=po[:, 0:65], lhsT=pt[:, i * 128:(i + 1) * 128], rhs=h["v_fn"](kb), start=(gi == 0),
             stop=(gi == job["nkb"] - 1))
    if job["c0"] + n == job["nkb"]:
        h["fin_fn"](po, pokey)


def run_jobs(S, bufs, ctr, jobs, fillers):
    n = len(jobs)
    nf = len(fillers)
    fi = 0
    LA = len(bufs["psS"]) - 1
    for k in range(n + LA):
        if k < n:
            job_S(S, bufs, ctr, jobs[k])
        if k >= LA:
            job_PV(S, bufs, ctr, jobs[k - LA])
        while fi < nf and (k + 1) * nf >= (fi + 1) * (n + LA):
            fillers[fi]()
            fi += 1
    while fi < nf:
        fillers[fi]()
        fi += 1


def phase_l0_attn(nc, S, T, x_in, x_out, nseq, ntiles=NT):
    with ExitStack() as st:
        cx = Ctx(nc, S, st, "a0")
        C = load_consts(nc, S, cx, T, need_mb=True, need_ma=True)
        w_in = cx.sb("w_in", [128, 8, L0_IN], BF16)
        for c in range(8):
            for hi in range(2):
                S.dma("pool", w_in[:, c, 0:512].rearrange("p (lo hi d) -> p lo hi d", lo=4, hi=2)[:, :, hi, :],
                      T["l0_w_in"].ap()[c * 128:(c + 1) * 128, hi * 256:(hi + 1) * 256].rearrange(
                          "k (lo d) -> k lo d", d=64), writes=["w_in"])
            S.dma("pool", w_in[:, c, 512:L0_IN], T["l0_w_in"].ap()[c * 128:(c + 1) * 128, 512:L0_IN],
                  writes=["w_in"])
        w_out = cx.sb("w_out", [128, 8, D], BF16)
        S.dma("pool", w_out[:], T["l0_w_out"].ap().rearrange("(c p) n -> p c n", p=128), writes=["w_out"])
        gattn = cx.sb("gattn", [128, D], F32)
        S.dma("sp", gattn[:], bc_dram(T["l0_attn_norm"], 128, D), writes=["gattn"])
        gfull = cx.sb("gfull", [128, 26, 64], F32)
        for (h0, nh, nm) in ((0, 8, "l0_a_q_norm"), (8, 2, "l0_a_k_norm"), (10, 8, "l0_b_q_norm"),
                             (18, 8, "l0_b_k_norm")):
            S.dma("sp", gfull[:, h0:h0 + nh, :], bass.AP(T[nm], 0, [[0, 128], [0, nh], [1, 64]]), writes=["gfull"])
        esink = cx.sb("esink", [128, 8], F32)
        S.dma("sp", esink[:], bc_dram(T["l0_a_sinks"], 128, 8), writes=["esink"])
        S.op("act", "activation", reads=["esink"], writes=["esink"], out=esink[:], in_=esink[:], func=AF.Exp)
        invh = cx.sb("invh", [128, 32], F32)
        S.dma("sp", invh[:], T["inv_h"].ap(), writes=["inv_h"])
        C["rope_h"] = alloc_rope(cx, 32, "_h")

        kT = cx.sb("kT", [128, 5, SEQ], BF16)
        Vaug = cx.sb("Vaug", [128, NT, 10, 65], BF16)
        S.op("dve", "memset", writes=[("Vaug", i) for i in range(NT)], ap=Vaug[:], constant=1.0)
        NB = 3
        xt = [cx.sb("xt%d" % i, [128, D], F32) for i in range(NB)]
        qT = [cx.sb("qT%d" % i, [128, 8, 128], BF16) for i in range(2)]
        xn = cx.sb("xn", [128, D], BF16)
        hT = cx.sb("hT", [128, 8, 128], BF16)
        proj = cx.sb("proj", [128, L0_IN], F32)
        W = {"junk": cx.sb("junk", [128, D], BF16), "ss": cx.sb("ss", [128, 1], F32),
             "rstd": cx.sb("rstd", [128, 1], F32), "lnt": cx.sb("lnt", [128, 1], F32)}
        sqb = cx.sb("sqb", [128, 1024], BF16)
        ssh = cx.sb("ssh", [128, 26], F32)
        rsh = cx.sb("rsh", [128, 26], F32)
        lnh = cx.sb("lnh", [128, 26], F32)
        tn = cx.sb("tn", [128, 16, 64], F32)
        tcb = cx.sb("tcb", [128, 16, 64], F32)
        tsb = cx.sb("tsb", [128, 16, 64], F32)
        qk = cx.sb("qk", [128, 26, 64], BF16)
        pT = [cx.sb("pT%d" % i, [128, 512], BF16) for i in range(4)]
        mix = cx.sb("mix", [128, D], BF16)
        mixT = cx.sb("mixT", [128, 8, 128], BF16)
        x1 = cx.sb("x1", [128, D], F32)
        den = cx.sb("den", [128, 32], F32)
        obufs = [cx.sb("obuf%d" % i, [128, 16, 65], F32) for i in range(2)]
        psT = [cx.ps("psT%d" % i, [128, 8, 128], BF16) for i in range(2)]
        psP = [cx.ps("psP%d" % i, [128, 512], F32) for i in range(1)]
        psS = [cx.ps("psS%d" % i, [128, 512], F32) for i in range(3)]
        psO = [cx.ps("psO%d" % i, [128, 512], F32) for i in range(2)]
        bufs = {"psS": psS, "pT": pT, "psO": psO, "ident": C["ident"][:]}
        ctr = {"s": 0, "p": 0, "o": 0, "t": 0, "pp": 0, "d": 0}

        def next_psT():
            i = ctr["t"] % 2
            ctr["t"] += 1
            return psT[i], "psT%d" % i

        def next_psP():
            i = ctr["pp"] % len(psP)
            ctr["pp"] += 1
            return psP[i], "psP%d" % i

        KK = lambda kb: ("kT", kb)
        VK = lambda kb: ("Vaug", kb)
        xin = x_in.ap() if hasattr(x_in, "ap") else x_in
        xo = x_out.ap() if hasattr(x_out, "ap") else x_out

        RW = C["rope_h"]

        def stageA(b, t):
            row0 = b * SEQ + t * 128
            gi = b * ntiles + t
            xtt, xkey = xt[gi % NB], "xt%d" % (gi % NB)
            steps = []

            def s1():
                S.dma("sp", xtt[:], xin[row0:row0 + 128, :], writes=[xkey])
                norm_tile(S, C, xtt[:], xkey, gattn[:], "gattn", xn[:], "xn", W, "")
                p_, pk = next_psT()
                transposes(S, C, [xn[:, c * 128:(c + 1) * 128] for c in range(8)], "xn", p_, pk, hT[:], "hT")
            steps.append(s1)

            def s2(n0):
                nw = min(512, L0_IN - n0)
                pp, ppk = next_psP()
                for c in range(8):
                    S.op("pe", "matmul", reads=["hT", "w_in"], writes=[ppk], signal=(c == 7), out=pp[:, 0:nw],
                         lhsT=hT[:, c, :], rhs=w_in[:, c, n0:n0 + nw], start=(c == 0), stop=(c == 7))
                S.op("act", "copy", reads=[ppk], writes=["proj"], out=proj[:, n0:n0 + nw], in_=pp[:, 0:nw])
            for n0 in range(0, L0_IN, 512):
                steps.append(lambda n0=n0: s2(n0))

            def s3():
                S.op("act", "copy", reads=["proj"], writes=[("Vaug", t)], out=Vaug[:, t, 0:2, 0:64],
                     in_=proj[:, 640:768].rearrange("p (h d) -> p h d", d=64))
                S.op("act", "copy", reads=["proj"], writes=[("Vaug", t)], out=Vaug[:, t, 2:10, 0:64],
                     in_=proj[:, 1792:2304].rearrange("p (h d) -> p h d", d=64))
            steps.append(s3)
            cosb = RW["cos"][:, t, :]
            sinb = RW["sin"][:, t, :]

            def s4a1(c0, nh, h0):
                S.op("act", "activation", reads=["proj"], writes=["sqb"], out=sqb[:, 0:nh * 64],
                     in_=proj[:, c0:c0 + nh * 64], func=AF.Square)

            def s4a2(c0, nh, h0):
                S.op("dve", "tensor_reduce", reads=["sqb"], writes=["ssh"], out=ssh[:, h0:h0 + nh],
                     in_=sqb[:, 0:nh * 64].rearrange("p (h d) -> p h d", d=64), axis=AX.X, op=ALU.add)

            def s4a3(c0, nh, h0):
                rms_rstd(S, ssh[:, h0:h0 + nh], rsh[:, h0:h0 + nh], lnh[:, h0:h0 + nh], 1.0 / 64, C["eps"][:],
                         ["ssh"], ["rsh"], "lnh")

            def s4a4(c0, nh, h0):
                pv = proj[:, c0:c0 + nh * 64].rearrange("p (h d) -> p h d", d=64)
                S.op("dve", "tensor_tensor", reads=["proj", "rsh"], writes=["tn"], out=tn[:, 0:nh, :], in0=pv,
                     in1=rsh[:, h0:h0 + nh].unsqueeze(2).to_broadcast([128, nh, 64]), op=ALU.mult)
                S.op("dve", "tensor_tensor", reads=["tn", "gfull"], writes=["tn"], out=tn[:, 0:nh, :],
                     in0=tn[:, 0:nh, :], in1=gfull[:, h0:h0 + nh, :], op=ALU.mult)

            def s4b(c0, nh, h0):
                t4 = tn[:, 0:nh, :].rearrange("p h (two d) -> p h two d", two=2)
                tc4 = tcb[:, 0:nh, :].rearrange("p h (two d) -> p h two d", two=2)
                ts4 = tsb[:, 0:nh, :].rearrange("p h (two d) -> p h two d", two=2)
                cos4 = cosb.unsqueeze(1).unsqueeze(1).to_broadcast([128, nh, 2, 32])
                sin4 = sinb.unsqueeze(1).unsqueeze(1).to_broadcast([128, nh, 2, 32])
                S.op("dve", "tensor_tensor", reads=["tn", "cos_h"], writes=["tcb"], out=tc4, in0=t4, in1=cos4,
                     op=ALU.mult)
                S.op("dve", "tensor_tensor", reads=["tn", "sin_h"], writes=["tsb"], out=ts4, in0=t4, in1=sin4,
                     op=ALU.mult)
                q4 = qk[:, h0:h0 + nh, :].rearrange("p h (two d) -> p h two d", two=2)
                S.op("dve", "tensor_tensor", reads=["tcb", "tsb"], writes=["qk"], out=q4[:, :, 0, :],
                     in0=tc4[:, :, 0, :], in1=ts4[:, :, 1, :], op=ALU.subtract)
                S.op("dve", "tensor_tensor", reads=["tcb", "tsb"], writes=["qk"], out=q4[:, :, 1, :],
                     in0=tc4[:, :, 1, :], in1=ts4[:, :, 0, :], op=ALU.add)
            for (c0, nh, h0) in ((0, 10, 0), (768, 16, 10)):
                steps.append(lambda c0=c0, nh=nh, h0=h0: s4a1(c0, nh, h0))
                steps.append(lambda c0=c0, nh=nh, h0=h0: s4a2(c0, nh, h0))
                steps.append(lambda c0=c0, nh=nh, h0=h0: s4a3(c0, nh, h0))
                steps.append(lambda c0=c0, nh=nh, h0=h0: s4a4(c0, nh, h0))
                steps.append(lambda c0=c0, nh=nh, h0=h0: s4b(c0, nh, h0))
            qTt, qkey = qT[gi % 2], "qT%d" % (gi % 2)
            qk2 = qk[:].rearrange("p h d -> p (h d)")

            def s5():
                p_, pk = next_psT()
                srcs = [qk2[:, 128 * j:128 * (j + 1)] for j in range(4)] + \
                       [qk2[:, 640 + 128 * j:640 + 128 * (j + 1)] for j in range(4)]
                transposes(S, C, srcs, "qk", p_, pk, qTt[:], qkey, evac="act")

            def s6():
                p_, pk = next_psT()
                srcs = [qk2[:, 512:640]] + [qk2[:, 1152 + 128 * j:1152 + 128 * (j + 1)] for j in range(4)]
                transposes(S, C, srcs, "qk", p_, pk, kT[:, :, t * 128:(t + 1) * 128], ("kT", t))
            steps.append(s5)
            steps.append(s6)
            return steps

        def stageB(b, t, fillers):
            gi = b * ntiles + t
            obuf, obn = obufs[gi % 2], "obuf%d" % (gi % 2)
            qTt, qkey = qT[gi % 2], "qT%d" % (gi % 2)
            jobs = []
            for h in range(8):
                half = slice(0, 64) if h < 4 else slice(64, 128)
                kbs = [t - 1, t] if t >= 1 else [t]

                def maskA(c0, chunk):
                    if len(chunk) == 2:
                        return (0, 2, C["MA"][:].rearrange("p a q -> p (a q)"))
                    return (0, 1, C["MA"][:, 1, :])

                def finA(po, pokey, h=h):
                    S.op("act", "copy", reads=[pokey], writes=[(obn, h)], out=obuf[:, h, :], in_=po[:, 0:65])

                head = {"kT_fn": (lambda kb, half=half: kT[half, 0, kb * 128:(kb + 1) * 128]),
                        "q_ap": qTt[half, h % 4, :], "qkey": qkey, "kkey": KK,
                        "v_fn": (lambda kb, h=h: Vaug[:, kb, h // 4, :]), "vkey": VK, "mask_fn": maskA,
                        "scale": 0.125, "fin_fn": finA}
                jobs += make_jobs(head, kbs)
            for h in range(8):
                half = slice(0, 64) if h % 2 == 0 else slice(64, 128)
                kbs = list(range(t + 1))

                def maskB(c0, chunk, t=t):
                    i0 = 15 - t + chunk[0]
                    return (0, len(chunk), C["MB"][:, i0:i0 + len(chunk), :].rearrange("p a q -> p (a q)"))

                def finB(po, pokey, h=h):
                    S.op("act", "copy", reads=[pokey], writes=[(obn, 8 + h)], out=obuf[:, 8 + h, :],
                         in_=po[:, 0:65])

                head = {"kT_fn": (lambda kb, half=half, h=h: kT[half, 1 + h // 2, kb * 128:(kb + 1) * 128]),
                        "q_ap": qTt[half, 4 + h // 2, :], "qkey": qkey, "kkey": KK,
                        "v_fn": (lambda kb, h=h: Vaug[:, kb, 2 + h, :]), "vkey": VK, "mask_fn": maskB,
                        "scale": 0.125, "fin_fn": finB}
                jobs += make_jobs(head, kbs)
            S.bg_step()
            run_jobs(S, bufs, ctr, jobs, fillers)

        def stageC(b, t):
            row0 = b * SEQ + t * 128
            gi = b * ntiles + t
            xtt, xkey = xt[gi % NB], "xt%d" % (gi % NB)
            obuf, obn = obufs[gi % 2], "obuf%d" % (gi % 2)
            okeys = [(obn, i) for i in range(16)]

            def c1():
                S.op("dve", "tensor_tensor", reads=okeys + ["esink"], writes=["den"], out=den[:, 0:8],
                     in0=obuf[:, 0:8, 64], in1=esink[:], op=ALU.add)
                S.op("dve", "tensor_copy", reads=okeys, writes=["den"], out=den[:, 8:16], in_=obuf[:, 8:16, 64])
                S.op("dve", "reciprocal", reads=["den"], writes=["den"], out=den[:, 16:32], in_=den[:, 0:16])
                S.op("dve", "tensor_tensor", reads=okeys + ["den"], writes=["mix"],
                     out=mix[:].rearrange("p (h d) -> p h d", d=64), in0=obuf[:, :, 0:64],
                     in1=den[:, 16:32].unsqueeze(2).to_broadcast([128, 16, 64]), op=ALU.mult)

            def c2():
                p_, pk = next_psT()
                transposes(S, C, [mix[:, c * 128:(c + 1) * 128] for c in range(8)], "mix", p_, pk, mixT[:], "mixT")

            def c3(n0):
                pp, ppk = next_psP()
                for c in range(8):
                    S.op("pe", "matmul", reads=["mixT", "w_out"], writes=[ppk], signal=(c == 7), out=pp[:],
                         lhsT=mixT[:, c, :], rhs=w_out[:, c, n0:n0 + 512], start=(c == 0), stop=(c == 7))
                S.op("dve", "tensor_tensor", reads=[ppk, xkey], writes=["x1"], out=x1[:, n0:n0 + 512], in0=pp[:],
                     in1=xtt[:, n0:n0 + 512], op=ALU.add)
                if n0 == 512:
                    S.dma("sp", xo[row0:row0 + 128, :], x1[:], reads=["x1"])
            return [c1, c2, (lambda: c3(0)), (lambda: c3(512))]

        order = [(b, t) for b in range(nseq) for t in range(ntiles)]
        for idx, (b, t) in enumerate(order):
            if idx == 0:
                rope_tables(S, C, cx, T["pos_t"].ap()[b], invh, 32, "_h")
                for f in stageA(b, t):
                    f()
            fillers = []
            if idx >= 1:
                fillers += stageC(*order[idx - 1])
            if idx + 1 < len(order):
                nb, nt_ = order[idx + 1]
                if nt_ == 0:
                    fillers.append(lambda nb=nb: rope_tables(S, C, cx, T["pos_t"].ap()[nb], invh, 32, "_h"))
                fillers += stageA(nb, nt_)
            stageB(b, t, fillers)
        for f in stageC(*order[-1]):
            f()
        S.barrier()


def phase_l0_ffn(nc, S, T, x_in, x_out, nrows):
    with ExitStack() as st:
        cx = Ctx(nc, S, st, "f0")
        C = load_consts(nc, S, cx, T)
        G = min(1024, nrows)
        GT = G // 128
        NF = DFF // 128
        wd = cx.sb("wd", [128, NF, D], BF16)
        pre = "l0_w_gate_bf" in T
        wq = "sp" if pre else "pool"
        wdv = T["l0_w_down_bf" if pre else "l0_w_down"].ap().rearrange("(f p) n -> p f n", p=128)
        for f0 in range(0, NF, 6):
            f1 = min(NF, f0 + 6)
            S.dma(wq, wd[:, f0:f1, :], wdv[:, f0:f1, :], writes=["wd"])
        gffn = cx.sb("gffn", [128, D], F32)
        S.dma("sp", gffn[:], bc_dram(T["l0_ffn_norm"], 128, D), writes=["gffn"])
        hT = cx.sb("hT", [128, 8, G], BF16)
        hidT = cx.sb("hidT", [128, NF, G], BF16)
        xs = cx.sb("xs", [128, GT, D], F32)
        wg = [cx.sb("wg%d" % i, [128, 8, 512], BF16) for i in range(2)]
        wu = [cx.sb("wu%d" % i, [128, 8, 512], BF16) for i in range(2)]
        xn = cx.sb("xn", [128, D], BF16)
        W = {"junk": cx.sb("junk", [128, D], BF16), "ss": cx.sb("ss", [128, 1], F32),
             "rstd": cx.sb("rstd", [128, 1], F32), "lnt": cx.sb("lnt", [128, 1], F32)}
        sg = [cx.sb("sg%d" % i, [128, 512], F32) for i in range(2)]
        yo = [cx.sb("yo%d" % i, [128, D], F32) for i in range(2)]
        psT = [cx.ps("psT%d" % i, [128, 8, 128], BF16) for i in range(2)]
        psG = [cx.ps("psG%d" % i, [128, 512], F32) for i in range(2)]
        psU = [cx.ps("psU%d" % i, [128, 512], F32) for i in range(2)]
        psY = [cx.ps("psY%d" % i, [128, 512], F32) for i in range(2)]
        xin, xo = x_in.ap(), x_out.ap()
        wgd, wud = T["l0_w_gate_bf" if pre else "l0_w_gate"].ap(), T["l0_w_up_bf" if pre else "l0_w_up"].ap()
        blocks = [(n0, min(512, DFF - n0)) for n0 in range(0, DFF, 512)]
        ctr = {"t": 0, "g": 0, "y": 0, "w": 0}

        def load_w(bi):
            n0, nw = blocks[bi]
            i = ctr["w"] % 2
            ctr["w"] += 1
            S.dma(wq, wg[i][:, :, 0:nw], wgd[:, n0:n0 + nw].rearrange("(c p) n -> p c n", p=128),
                  writes=["wg%d" % i])
            S.dma(wq, wu[i][:, :, 0:nw], wud[:, n0:n0 + nw].rearrange("(c p) n -> p c n", p=128),
                  writes=["wu%d" % i])
            return i

        for g0 in range(0, nrows, G):
            for i in range(GT):
                r0 = g0 + i * 128
                S.dma("sp", xs[:, i, :], xin[r0:r0 + 128, :], writes=[("xs", i)])
                norm_tile(S, C, xs[:, i, :], ("xs", i), gffn[:], "gffn", xn[:], "xn", W, "")
                k = ctr["t"] % 2
                ctr["t"] += 1
                transposes(S, C, [xn[:, c * 128:(c + 1) * 128] for c in range(8)], "xn", psT[k], "psT%d" % k,
                           hT[:, :, i * 128:(i + 1) * 128], "hT")
            wi = load_w(0)
            for bi, (n0, nw) in enumerate(blocks):
                cur = wi
                if bi + 1 < len(blocks):
                    wi = load_w(bi + 1)
                for j in range(nw // 128):
                    f = n0 // 128 + j
                    for th in range(G // 512):
                        k = ctr["g"] % 2
                        ctr["g"] += 1
                        tok = slice(th * 512, (th + 1) * 512)
                        for c in range(8):
                            S.op("pe", "matmul", reads=["hT", "wg%d" % cur], writes=["psG%d" % k], signal=(c == 7),
                                 out=psG[k][:], lhsT=wg[cur][:, c, j * 128:(j + 1) * 128], rhs=hT[:, c, tok],
                                 start=(c == 0), stop=(c == 7))
                        for c in range(8):
                            S.op("pe", "matmul", reads=["hT", "wu%d" % cur], writes=["psU%d" % k], signal=(c == 7),
                                 out=psU[k][:], lhsT=wu[cur][:, c, j * 128:(j + 1) * 128], rhs=hT[:, c, tok],
                                 start=(c == 0), stop=(c == 7))
                        S.op("act", "activation", reads=["psG%d" % k], writes=["sg%d" % k], out=sg[k][:],
                             in_=psG[k][:], func=AF.Silu)
                        S.op("dve", "tensor_tensor", reads=["sg%d" % k, "psU%d" % k], writes=[("hidT", f)],
                             out=hidT[:, f, tok], in0=sg[k][:], in1=psU[k][:], op=ALU.mult)
            for i in range(GT):
                r0 = g0 + i * 128
                yk = ctr["y"] % 2
                ctr["y"] += 1
                for hh, n0 in enumerate((0, 512)):
                    k = (ctr["g"] + hh) % 2
                    for f in range(NF):
                        S.op("pe", "matmul", reads=[("hidT", f), "wd"], writes=["psY%d" % k], signal=(f == NF - 1),
                             out=psY[k][:], lhsT=hidT[:, f, i * 128:(i + 1) * 128], rhs=wd[:, f, n0:n0 + 512],
                             start=(f == 0), stop=(f == NF - 1))
                    S.op("dve", "tensor_tensor", reads=["psY%d" % k, ("xs", i)], writes=["yo%d" % yk],
                         out=yo[yk][:, n0:n0 + 512], in0=psY[k][:], in1=xs[:, i, n0:n0 + 512], op=ALU.add)
                S.dma("sp", xo[r0:r0 + 128, :], yo[yk][:], reads=["yo%d" % yk])
        S.barrier()


def phase_l1_attn(nc, S, T, x_in, x_out, nseq, ntiles=NT):
    with ExitStack() as st:
        cx = Ctx(nc, S, st, "a1")
        C = load_consts(nc, S, cx, T, need_ma=True)
        w_in = cx.sb("w_in", [128, 8, L1_IN], BF16)
        S.dma("pool", w_in[:], T["l1_w_in"].ap().rearrange("(c p) n -> p c n", p=128), writes=["w_in"])
        w_uq = cx.sb("w_uq", [128, 3, 1536], BF16)
        S.dma("pool", w_uq[:], T["l1_w_uq"].ap().rearrange("(c p) n -> p c n", p=128), writes=["w_uq"])
        w_ukv = cx.sb("w_ukv", [128, 2, 2048], BF16)
        S.dma("pool", w_ukv[:], T["l1_w_ukv"].ap().rearrange("(c p) n -> p c n", p=128), writes=["w_ukv"])
        w_out = cx.sb("w_out", [128, 8, D], BF16)
        S.dma("pool", w_out[:], T["l1_w_out"].ap().rearrange("(c p) n -> p c n", p=128), writes=["w_out"])
        gattn = cx.sb("gattn", [128, D], F32)
        S.dma("sp", gattn[:], bc_dram(T["l1_attn_norm"], 128, D), writes=["gattn"])
        glat = cx.sb("glat", [128, 640], F32)
        S.dma("sp", glat[:, 0:384], bc_dram(T["l1_q_a_norm"], 128, 384), writes=["glat"])
        S.dma("sp", glat[:, 384:640], bc_dram(T["l1_kv_a_norm"], 128, 256), writes=["glat"])
        gq = cx.sb("gq", [128, 96], F32)
        S.dma("sp", gq[:], bc_dram(T["l1_c_q_norm"], 128, 96), writes=["gq"])
        gk = cx.sb("gk", [128, 96], F32)
        S.dma("sp", gk[:], bc_dram(T["l1_c_k_norm"], 128, 96), writes=["gk"])
        invc = cx.sb("invc", [128, 16], F32)
        S.dma("sp", invc[:], T["inv_c"].ap(), writes=["inv_c"])
        rp = alloc_rope(cx, 16, "_c", big=True)
        trq, tcq, tsq = rp["ang_full"], rp["rr_full"], rp["kf_full"]
        C["rope_c"] = rp

        kT = cx.sb("kT", [128, 16, SEQ], BF16)
        Vaug = cx.sb("Vaug", [128, NT, 16, 65], BF16)
        S.op("dve", "memset", writes=[("Vaug", i) for i in range(NT)], ap=Vaug[:], constant=1.0)
        NB = 2
        xt = [cx.sb("xt%d" % i, [128, D], F32) for i in range(NB)]
        qTs = [cx.sb("qT%d" % i, [128, 16, 128], BF16) for i in range(2)]
        xn = cx.sb("xn", [128, D], BF16)
        hT = cx.sb("hT", [128, 8, 128], BF16)
        pj = cx.sb("pj", [128, L1_IN], F32)
        cn = cx.sb("cn", [128, 640], BF16)
        cT = hT
        qf = cx.sb("qf", [128, 16, 96], F32)
        knf = cx.sb("knf", [128, 16, 64], F32)
        W = {"junk": xn, "ss": cx.sb("ss", [128, 1], F32),
             "rstd": cx.sb("rstd", [128, 1], F32), "lnt": cx.sb("lnt", [128, 1], F32)}
        lss = cx.sb("lss", [128, 4], F32)
        lrs = cx.sb("lrs", [128, 4], F32)
        lln = cx.sb("lln", [128, 4], F32)
        ssh = cx.sb("ssh", [128, 32], F32)
        rsh = cx.sb("rsh", [128, 32], F32)
        lnh = cx.sb("lnh", [128, 32], F32)
        kr = cx.sb("kr", [128, 4, 32], F32)
        qb = cx.sb("qb", [128, 16, 96], BF16)
        kb_ = cx.sb("kb", [128, 16, 96], BF16)
        pT = [cx.sb("pT%d" % i, [128, 512], BF16) for i in range(3)]
        obuf = cx.sb("obuf", [128, 16, 65], F32)
        mix = cx.sb("mix", [128, D], BF16)
        mixT = hT
        den = cx.sb("den", [128, 16], F32)
        psT = [cx.ps("psT%d" % i, [128, 8, 128], BF16) for i in range(2)]
        psP = [cx.ps("psP%d" % i, [128, 512], F32) for i in range(1)]
        psS = [cx.ps("psS%d" % i, [128, 512], F32) for i in range(3)]
        psO = [cx.ps("psO%d" % i, [128, 512], F32) for i in range(2)]
        bufs = {"psS": psS, "pT": pT, "psO": psO, "ident": C["ident"][:]}
        ctr = {"s": 0, "p": 0, "o": 0, "t": 0, "pp": 0}

        def next_psT():
            i = ctr["t"] % 2
            ctr["t"] += 1
            return psT[i], "psT%d" % i

        def next_psP():
            i = ctr["pp"] % len(psP)
            ctr["pp"] += 1
            return psP[i], "psP%d" % i

        KK = lambda kb: ("kT", kb)
        VK = lambda kb: ("Vaug", kb)
        xin, xo = x_in.ap(), x_out.ap()
        SC = 96 ** -0.5

        RW = C["rope_c"]
        jk96 = qb
        jk64 = kb_

        def stageA(b, t):
            row0 = b * SEQ + t * 128
            gi = b * ntiles + t
            xtt, xkey = xt[gi % NB], "xt%d" % (gi % NB)
            steps = []

            def s1():
                S.dma("sp", xtt[:], xin[row0:row0 + 128, :], writes=[xkey])
                norm_tile(S, C, xtt[:], xkey, gattn[:], "gattn", xn[:], "xn", W, "")
                p_, pk = next_psT()
                transposes(S, C, [xn[:, c * 128:(c + 1) * 128] for c in range(8)], "xn", p_, pk, hT[:], "hT")
                for n0 in range(0, L1_IN, 512):
                    nw = min(512, L1_IN - n0)
                    pp, ppk = next_psP()
                    for c in range(8):
                        S.op("pe", "matmul", reads=["hT", "w_in"], writes=[ppk], signal=(c == 7), out=pp[:, 0:nw],
                             lhsT=hT[:, c, :], rhs=w_in[:, c, n0:n0 + nw], start=(c == 0), stop=(c == 7))
                    S.op("act", "copy", reads=[ppk], writes=["pj"], out=pj[:, n0:n0 + nw], in_=pp[:, 0:nw])
            steps.append(s1)

            def s2a():
                for (i, c0, c1) in ((0, 0, 384), (1, 384, 640)):
                    S.op("dve", "scalar_tensor_tensor", reads=["pj"], writes=["cn", "lss"], out=cn[:, c0:c1],
                         in0=pj[:, c0:c1], scalar=1.0, in1=pj[:, c0:c1], op0=ALU.mult, op1=ALU.mult,
                         accum_out=lss[:, i:i + 1])

            def s2b():
                for (i, c0, c1) in ((0, 0, 384), (1, 384, 640)):
                    rms_rstd(S, lss[:, i:i + 1], lrs[:, i:i + 1], lln[:, i:i + 1], 1.0 / (c1 - c0), C["eps"][:],
                             ["lss"], ["lrs"], "lln")

            def s2c():
                for (i, c0, c1) in ((0, 0, 384), (1, 384, 640)):
                    S.op("dve", "scalar_tensor_tensor", reads=["pj", "lrs", "glat"], writes=["cn"], out=cn[:, c0:c1],
                         in0=pj[:, c0:c1], scalar=lrs[:, i:i + 1], in1=glat[:, c0:c1], op0=ALU.mult, op1=ALU.mult)
                p_, pk = next_psT()
                transposes(S, C, [cn[:, c * 128:(c + 1) * 128] for c in range(5)], "cn", p_, pk, cT[:, 0:5, :], "hT")
            steps.append(s2a)
            steps.append(s2b)
            steps.append(s2c)
            qf2 = qf[:].rearrange("p h d -> p (h d)")

            def s3(n0):
                pp, ppk = next_psP()
                for c in range(3):
                    S.op("pe", "matmul", reads=["hT", "w_uq"], writes=[ppk], signal=(c == 2), out=pp[:],
                         lhsT=cT[:, c, :], rhs=w_uq[:, c, n0:n0 + 512], start=(c == 0), stop=(c == 2))
                S.op("act", "copy", reads=[ppk], writes=["qf"], out=qf2[:, n0:n0 + 512], in_=pp[:])
            for n0 in range(0, 1536, 512):
                steps.append(lambda n0=n0: s3(n0))

            def s4(j):
                pp, ppk = next_psP()
                for c in range(2):
                    S.op("pe", "matmul", reads=["hT", "w_ukv"], writes=[ppk], signal=(c == 1), out=pp[:],
                         lhsT=cT[:, 3 + c, :], rhs=w_ukv[:, c, j * 512:(j + 1) * 512], start=(c == 0),
                         stop=(c == 1))
                pv = pp[:].rearrange("p (h d) -> p h d", d=128)
                S.op("act", "copy", reads=[ppk], writes=["knf"], out=knf[:, 4 * j:4 * j + 4, :], in_=pv[:, :, 0:64])
                S.op("act", "copy", reads=[ppk], writes=[("Vaug", t)], out=Vaug[:, t, 4 * j:4 * j + 4, 0:64],
                     in_=pv[:, :, 64:128])
            for j in range(4):
                steps.append(lambda j=j: s4(j))
            cosb = RW["cos"][:, t, :]
            sinb = RW["sin"][:, t, :]

            def s5a():
                S.op("act", "activation", reads=["qf"], writes=["qb"], out=jk96[:], in_=qf[:], func=AF.Square)
                S.op("act", "activation", reads=["knf"], writes=["kb"], out=jk64[:, :, 0:64], in_=knf[:],
                     func=AF.Square)

            def s5b():
                S.op("dve", "tensor_reduce", reads=["qb"], writes=["ssh"], out=ssh[:, 0:16], in_=jk96[:], axis=AX.X,
                     op=ALU.add)
                S.op("dve", "tensor_reduce", reads=["kb"], writes=["ssh"], out=ssh[:, 16:32], in_=jk64[:, :, 0:64],
                     axis=AX.X, op=ALU.add)
                S.op("dve", "scalar_tensor_tensor", reads=["pj"], writes=["kr", "lss"], out=kr[:, 1, :],
                     in0=pj[:, 640:672], scalar=1.0, in1=pj[:, 640:672], op0=ALU.mult, op1=ALU.mult,
                     accum_out=lss[:, 2:3])
                S.op("dve", "tensor_scalar", reads=["ssh", "lss"], writes=["ssh"], out=ssh[:, 16:32], in0=ssh[:, 16:32],
                     scalar1=lss[:, 2:3], scalar2=None, op0=ALU.add)

            def s5c():
                rms_rstd(S, ssh[:], rsh[:], lnh[:], 1.0 / 96, C["eps"][:], ["ssh"], ["rsh"], "lnh")
            steps.append(s5a)
            steps.append(s5b)
            steps.append(lambda: None)
            steps.append(s5c)

            def s6():
                S.op("dve", "tensor_tensor", reads=["qf", "rsh"], writes=["qf"], out=qf[:], in0=qf[:],
                     in1=rsh[:, 0:16].unsqueeze(2).to_broadcast([128, 16, 96]), op=ALU.mult)
                S.op("dve", "tensor_tensor", reads=["qf", "gq"], writes=["qb"], out=qb[:, :, 0:64], in0=qf[:, :, 0:64],
                     in1=gq[:, 0:64].unsqueeze(1).to_broadcast([128, 16, 64]), op=ALU.mult)
                S.op("dve", "tensor_tensor", reads=["qf", "gq"], writes=["ang_c"], out=trq[:], in0=qf[:, :, 64:96],
                     in1=gq[:, 64:96].unsqueeze(1).to_broadcast([128, 16, 32]), op=ALU.mult)
                t4 = trq[:].rearrange("p h (two d) -> p h two d", two=2)
                tc4 = tcq[:].rearrange("p h (two d) -> p h two d", two=2)
                ts4 = tsq[:].rearrange("p h (two d) -> p h two d", two=2)
                cos4 = cosb.unsqueeze(1).unsqueeze(1).to_broadcast([128, 16, 2, 16])
                sin4 = sinb.unsqueeze(1).unsqueeze(1).to_broadcast([128, 16, 2, 16])
                S.op("dve", "tensor_tensor", reads=["ang_c", "cos_c"], writes=["rr_c"], out=tc4, in0=t4, in1=cos4,
                     op=ALU.mult)
                S.op("dve", "tensor_tensor", reads=["ang_c", "sin_c"], writes=["kf_c"], out=ts4, in0=t4, in1=sin4,
                     op=ALU.mult)
                S.op("dve", "tensor_tensor", reads=["rr_c", "kf_c"], writes=["qb"], out=qb[:, :, 64:80],
                     in0=tc4[:, :, 0, :], in1=ts4[:, :, 1, :], op=ALU.subtract)
                S.op("dve", "tensor_tensor", reads=["rr_c", "kf_c"], writes=["qb"], out=qb[:, :, 80:96],
                     in0=tc4[:, :, 1, :], in1=ts4[:, :, 0, :], op=ALU.add)
            steps.append(s6)

            def s7():
                S.op("dve", "tensor_tensor", reads=["knf", "rsh"], writes=["knf"], out=knf[:], in0=knf[:],
                     in1=rsh[:, 16:32].unsqueeze(2).to_broadcast([128, 16, 64]), op=ALU.mult)
                S.op("dve", "tensor_tensor", reads=["knf", "gk"], writes=["kb"], out=kb_[:, :, 0:64], in0=knf[:],
                     in1=gk[:, 0:64].unsqueeze(1).to_broadcast([128, 16, 64]), op=ALU.mult)
                S.op("dve", "tensor_tensor", reads=["pj", "gk"], writes=["kr"], out=kr[:, 0, :], in0=pj[:, 640:672],
                     in1=gk[:, 64:96], op=ALU.mult)
                k0 = kr[:, 0, :].rearrange("p (two d) -> p two d", two=2)
                k1 = kr[:, 1, :].rearrange("p (two d) -> p two d", two=2)
                k2 = kr[:, 2, :].rearrange("p (two d) -> p two d", two=2)
                S.op("dve", "tensor_tensor", reads=["kr", "cos_c"], writes=["kr"], out=k1, in0=k0,
                     in1=cosb.unsqueeze(1).to_broadcast([128, 2, 16]), op=ALU.mult)
                S.op("dve", "tensor_tensor", reads=["kr", "sin_c"], writes=["kr"], out=k2, in0=k0,
                     in1=sinb.unsqueeze(1).to_broadcast([128, 2, 16]), op=ALU.mult)
                S.op("dve", "tensor_tensor", reads=["kr"], writes=["kr"], out=kr[:, 3, 0:16], in0=k1[:, 0, :],
                     in1=k2[:, 1, :], op=ALU.subtract)
                S.op("dve", "tensor_tensor", reads=["kr"], writes=["kr"], out=kr[:, 3, 16:32], in0=k1[:, 1, :],
                     in1=k2[:, 0, :], op=ALU.add)
                S.op("dve", "tensor_tensor", reads=["kr", "rsh"], writes=["kb"], out=kb_[:, :, 64:96],
                     in0=kr[:, 3, :].unsqueeze(1).to_broadcast([128, 16, 32]),
                     in1=rsh[:, 16:32].unsqueeze(2).to_broadcast([128, 16, 32]), op=ALU.mult)
            steps.append(s7)
            kb2 = kb_[:].rearrange("p h d -> p (h d)")

            def s8(half):
                p_, pk = next_psT()
                transposes(S, C, [kb2[:, (8 * half + j) * 96:(8 * half + j + 1) * 96] for j in range(8)], "kb", p_,
                           pk, kT[0:96, 8 * half:8 * half + 8, t * 128:(t + 1) * 128], ("kT", t), nrows=96)
            steps.append(lambda: s8(0))
            steps.append(lambda: s8(1))
            return steps

        def q_transposes(b, t, half):
            gi = b * ntiles + t
            qT, qkey = qTs[gi % 2], "qT%d" % (gi % 2)
            qb2 = qb[:].rearrange("p h d -> p (h d)")
            p_, pk = next_psT()
            transposes(S, C, [qb2[:, (8 * half + j) * 96:(8 * half + j + 1) * 96] for j in range(8)], "qb", p_,
                       pk, qT[0:96, 8 * half:8 * half + 8, :], qkey, nrows=96, evac="act")

        def stageB(b, t, fillers):
            row0 = b * SEQ + t * 128
            gi = b * ntiles + t
            qT, qkey = qTs[gi % 2], "qT%d" % (gi % 2)
            kbs = list(range(t + 1))
            jobs = []
            for h in range(16):
                def maskC(c0, chunk, t=t):
                    if chunk[-1] == t:
                        return (len(chunk) - 1, len(chunk), C["MA"][:, 1, :])
                    return None

                def finC(po, pokey, h=h):
                    S.op("act", "copy", reads=[pokey], writes=[("obuf", h)], out=obuf[:, h, :], in_=po[:, 0:65])

                head = {"kT_fn": (lambda kb, h=h: kT[0:96, h, kb * 128:(kb + 1) * 128]), "q_ap": qT[0:96, h, :],
                        "qkey": qkey, "kkey": KK, "v_fn": (lambda kb, h=h: Vaug[:, kb, h, :]), "vkey": VK,
                        "mask_fn": maskC, "scale": SC, "fin_fn": finC}
                jobs += make_jobs(head, kbs)
            S.bg_step()
            run_jobs(S, bufs, ctr, jobs, fillers)
            okeys = [("obuf", i) for i in range(16)]
            S.op("dve", "reciprocal", reads=okeys, writes=["den"], out=den[:, 0:16], in_=obuf[:, :, 64])
            S.op("dve", "tensor_tensor", reads=okeys + ["den"], writes=["mix"],
                 out=mix[:].rearrange("p (h d) -> p h d", d=64), in0=obuf[:, :, 0:64],
                 in1=den[:, 0:16].unsqueeze(2).to_broadcast([128, 16, 64]), op=ALU.mult)

        def stageC(b, t):
            row0 = b * SEQ + t * 128
            gi = b * ntiles + t
            xtt, xkey = xt[gi % NB], "xt%d" % (gi % NB)

            def c2():
                p_, pk = next_psT()
                transposes(S, C, [mix[:, c * 128:(c + 1) * 128] for c in range(8)], "mix", p_, pk, mixT[:], "hT")

            def c3(n0):
                pp, ppk = next_psP()
                for c in range(8):
                    S.op("pe", "matmul", reads=["hT", "w_out"], writes=[ppk], signal=(c == 7), out=pp[:],
                         lhsT=mixT[:, c, :], rhs=w_out[:, c, n0:n0 + 512], start=(c == 0), stop=(c == 7))
                S.op("dve", "tensor_tensor", reads=[ppk, xkey], writes=[xkey], out=xtt[:, n0:n0 + 512], in0=pp[:],
                     in1=xtt[:, n0:n0 + 512], op=ALU.add)
                if n0 == 512:
                    S.dma("sp", xo[row0:row0 + 128, :], xtt[:], reads=[xkey])
            return [c2, (lambda: c3(0)), (lambda: c3(512))]

        order = [(b, t) for b in range(nseq) for t in range(ntiles)]
        for idx, (b, t) in enumerate(order):
            if idx == 0:
                rope_tables(S, C, cx, T["pos_t"].ap()[b], invc, 16, "_c")
                for f in stageA(b, t):
                    f()
                q_transposes(b, t, 0)
                q_transposes(b, t, 1)
            fillers = []
            if idx >= 1:
                fillers += stageC(*order[idx - 1])
            if idx + 1 < len(order):
                nb, nt_ = order[idx + 1]
                if nt_ == 0:
                    fillers.append(lambda nb=nb: rope_tables(S, C, cx, T["pos_t"].ap()[nb], invc, 16, "_c"))
                fillers += stageA(nb, nt_)
                fillers.append(lambda nb=nb, nt_=nt_: q_transposes(nb, nt_, 0))
                fillers.append(lambda nb=nb, nt_=nt_: q_transposes(nb, nt_, 1))
            stageB(b, t, fillers)
        for f in stageC(*order[-1]):
            f()
        S.barrier()


def phase_l1_moe(nc, S, T, x_in, x_out, nrows):
    with ExitStack() as st:
        cx = Ctx(nc, S, st, "m1")
        C = load_consts(nc, S, cx, T)
        G = min(1024, nrows)
        GT = G // 128
        NF = DFE // 128
        gffn = cx.sb("gffn", [128, D], F32)
        S.dma("sp", gffn[:], bc_dram(T["l1_ffn_norm"], 128, D), writes=["gffn"])
        rw = cx.sb("rw", [128, 8, NEXP], BF16)
        S.dma("pool", rw[:], T["l1_router"].ap().rearrange("(c p) n -> p c n", p=128), writes=["rw"])
        hT = cx.sb("hT", [128, 8, G], BF16)
        hidT = cx.sb("hidT", [128, NF, G], BF16)
        xs = cx.sb("xs", [128, GT, D], F32)
        wg = [cx.sb("wg%d" % i, [128, 8, 512], BF16) for i in range(2)]
        wu = [cx.sb("wu%d" % i, [128, 8, 512], BF16) for i in range(2)]
        wd = [cx.sb("wd%d" % i, [128, NF, 512], BF16) for i in range(2)]
        xn = cx.sb("xn", [128, D], BF16)
        W = {"junk": cx.sb("junk", [128, D], BF16), "ss": cx.sb("ss", [128, 1], F32),
             "rstd": cx.sb("rstd", [128, 1], F32), "lnt": cx.sb("lnt", [128, 1], F32)}
        sg = [cx.sb("sg%d" % i, [128, 512], F32) for i in range(2)]
        gates = cx.sb("gates", [128, GT, NEXP], F32)
        lg = cx.sb("lg", [128, NEXP], F32)
        l2 = cx.sb("l2", [128, NEXP], F32)
        eq1 = cx.sb("eq1", [128, NEXP], F32)
        eq2 = cx.sb("eq2", [128, NEXP], F32)
        sm = cx.sb("sm", [128, 8], F32)
        psT = [cx.ps("psT%d" % i, [128, 8, 128], BF16) for i in range(2)]
        psG = [cx.ps("psG%d" % i, [128, 512], F32) for i in range(2)]
        psU = [cx.ps("psU%d" % i, [128, 512], F32) for i in range(2)]
        psY = [cx.ps("psY%d" % i, [128, 512], F32) for i in range(2)]
        xin, xo = x_in.ap(), x_out.ap()
        wgd, wud, wdd = T["l1_we_gate"].ap(), T["l1_we_up"].ap(), T["l1_we_down"].ap()
        blocks = [(n0, 512) for n0 in range(0, DFE, 512)]
        ctr = {"t": 0, "g": 0, "y": 0, "w": 0, "d": 0}

        def load_w(e, bi):
            n0, nw = blocks[bi]
            i = ctr["w"] % 2
            ctr["w"] += 1
            S.dma("pool", wg[i][:], wgd[e, :, n0:n0 + nw].rearrange("(c p) n -> p c n", p=128), writes=["wg%d" % i])
            S.dma("pool", wu[i][:], wud[e, :, n0:n0 + nw].rearrange("(c p) n -> p c n", p=128), writes=["wu%d" % i])
            return i

        def load_wd(e, hh):
            i = ctr["d"] % 2
            ctr["d"] += 1
            src = wdd[e, :, hh * 512:(hh + 1) * 512].rearrange("(f p) n -> p f n", p=128)
            for f0 in range(0, NF, 7):
                S.dma("pool", wd[i][:, f0:f0 + 7, :], src[:, f0:f0 + 7, :], writes=["wd%d" % i])
            return i

        for g0 in range(0, nrows, G):
            for i in range(GT):
                r0 = g0 + i * 128
                S.dma("sp", xs[:, i, :], xin[r0:r0 + 128, :], writes=[("xs", i)])
                norm_tile(S, C, xs[:, i, :], ("xs", i), gffn[:], "gffn", xn[:], "xn", W, "")
                k = ctr["t"] % 2
                ctr["t"] += 1
                transposes(S, C, [xn[:, c * 128:(c + 1) * 128] for c in range(8)], "xn", psT[k], "psT%d" % k,
                           hT[:, :, i * 128:(i + 1) * 128], "hT")
                for c in range(8):
                    S.op("pe", "matmul", reads=["hT", "rw"], writes=["psY0"], signal=(c == 7), out=psY[0][:, 0:NEXP],
                         lhsT=hT[:, c, i * 128:(i + 1) * 128], rhs=rw[:, c, :], start=(c == 0), stop=(c == 7))
                S.op("dve", "tensor_copy", reads=["psY0"], writes=["lg"], out=lg[:], in_=psY[0][:, 0:NEXP])
                S.op("dve", "tensor_reduce", reads=["lg"], writes=["sm"], out=sm[:, 0:1], in_=lg[:], axis=AX.X,
                     op=ALU.max)
                S.op("dve", "tensor_scalar", reads=["lg", "sm"], writes=["eq1"], out=eq1[:], in0=lg[:],
                     scalar1=sm[:, 0:1], scalar2=None, op0=ALU.is_equal)
                S.op("dve", "scalar_tensor_tensor", reads=["eq1", "lg"], writes=["l2"], out=l2[:], in0=eq1[:],
                     scalar=-1e30, in1=lg[:], op0=ALU.mult, op1=ALU.add)
                S.op("dve", "tensor_reduce", reads=["l2"], writes=["sm"], out=sm[:, 1:2], in_=l2[:], axis=AX.X,
                     op=ALU.max)
                S.op("dve", "tensor_scalar", reads=["l2", "sm"], writes=["eq2"], out=eq2[:], in0=l2[:],
                     scalar1=sm[:, 1:2], scalar2=None, op0=ALU.is_equal)
                S.op("dve", "tensor_tensor", reads=["sm"], writes=["sm"], out=sm[:, 2:3], in0=sm[:, 1:2],
                     in1=sm[:, 0:1], op=ALU.subtract)
                S.op("act", "activation", reads=["sm"], writes=["sm"], out=sm[:, 3:4], in_=sm[:, 2:3], func=AF.Exp)
                S.op("dve", "tensor_scalar", reads=["sm"], writes=["sm"], out=sm[:, 4:5], in0=sm[:, 3:4], scalar1=1.0,
                     scalar2=None, op0=ALU.add)
                S.op("dve", "reciprocal", reads=["sm"], writes=["sm"], out=sm[:, 5:6], in_=sm[:, 4:5])
                S.op("dve", "tensor_tensor", reads=["sm"], writes=["sm"], out=sm[:, 6:7], in0=sm[:, 3:4],
                     in1=sm[:, 5:6], op=ALU.mult)
                S.op("dve", "tensor_scalar", reads=["eq1", "sm"], writes=[("gates", i)], out=gates[:, i, :],
                     in0=eq1[:], scalar1=sm[:, 5:6], scalar2=None, op0=ALU.mult)
                S.op("dve", "scalar_tensor_tensor", reads=["eq2", "sm", ("gates", i)], writes=[("gates", i)],
                     out=gates[:, i, :], in0=eq2[:], scalar=sm[:, 6:7], in1=gates[:, i, :], op0=ALU.mult,
                     op1=ALU.add)
            for e in range(NEXP):
                wi = load_w(e, 0)
                dcur = None
                for bi, (n0, nw) in enumerate(blocks):
                    cur = wi
                    if bi + 1 < len(blocks):
                        wi = load_w(e, bi + 1)
                    elif dcur is None:
                        pass
                    if bi == 2:
                        dcur = load_wd(e, 0)
                    for j in range(nw // 128):
                        f = n0 // 128 + j
                        for th in range(G // 512):
                            k = ctr["g"] % 2
                            ctr["g"] += 1
                            tok = slice(th * 512, (th + 1) * 512)
                            for c in range(8):
                                S.op("pe", "matmul", reads=["hT", "wg%d" % cur], writes=["psG%d" % k],
                                     signal=(c == 7), out=psG[k][:], lhsT=wg[cur][:, c, j * 128:(j + 1) * 128],
                                     rhs=hT[:, c, tok], start=(c == 0), stop=(c == 7))
                            for c in range(8):
                                S.op("pe", "matmul", reads=["hT", "wu%d" % cur], writes=["psU%d" % k],
                                     signal=(c == 7), out=psU[k][:], lhsT=wu[cur][:, c, j * 128:(j + 1) * 128],
                                     rhs=hT[:, c, tok], start=(c == 0), stop=(c == 7))
                            S.op("act", "activation", reads=["psG%d" % k], writes=["sg%d" % k], out=sg[k][:],
                                 in_=psG[k][:], func=AF.Silu)
                            S.op("dve", "tensor_tensor", reads=["sg%d" % k, "psU%d" % k], writes=[("hidT", f)],
                                 out=hidT[:, f, tok], in0=sg[k][:], in1=psU[k][:], op=ALU.mult)
                for hh in range(2):
                    dnext = load_wd(e, 1) if hh == 0 else None
                    for i in range(GT):
                        k = ctr["y"] % 2
                        ctr["y"] += 1
                        for f in range(NF):
                            S.op("pe", "matmul", reads=[("hidT", f), "wd%d" % dcur], writes=["psY%d" % k],
                                 signal=(f == NF - 1), out=psY[k][:], lhsT=hidT[:, f, i * 128:(i + 1) * 128],
                                 rhs=wd[dcur][:, f, :], start=(f == 0), stop=(f == NF - 1))
                        S.op("dve", "scalar_tensor_tensor", reads=["psY%d" % k, ("gates", i), ("xs", i)],
                             writes=[("xs", i)], out=xs[:, i, hh * 512:(hh + 1) * 512], in0=psY[k][:],
                             scalar=gates[:, i, e:e + 1], in1=xs[:, i, hh * 512:(hh + 1) * 512], op0=ALU.mult,
                             op1=ALU.add)
                    if dnext is not None:
                        dcur = dnext
            for i in range(GT):
                r0 = g0 + i * 128
                S.dma("sp", xo[r0:r0 + 128, :], xs[:, i, :], reads=[("xs", i)])
        S.barrier()


def phase_l1_moe_sparse(nc, S, T, x_in, x_out, nrows):
    ntile = nrows // 128
    maxgrp = (nrows + GRP - 1) // GRP
    NF = DFE // 128
    hs = nc.dram_tensor("moe_hs", [NEXP * ECAP, D], BF16)
    Y = nc.dram_tensor("moe_y", [NEXP * ECAP, D], F32)
    xin, xo = x_in.ap(), x_out.ap()
    with ExitStack() as st0:
        cx0 = Ctx(nc, S, st0, "m1")
        posb = cx0.sb("posb", [128, ntile, 2], I32)
        gtb = cx0.sb("gtb", [128, ntile, 2], F32)
        ngb = cx0.sb("ngb", [1, NEXP], I32)
        with ExitStack() as st:
            cx = Ctx(nc, S, st, "m1a")
            C = load_consts(nc, S, cx, T)
            gffn = cx.sb("gffn", [128, D], F32)
            S.dma("sp", gffn[:], bc_dram(T["l1_ffn_norm"], 128, D), writes=["gffn"])
            rw = cx.sb("rw", [128, 8, NEXP], BF16)
            S.dma("pool", rw[:], T["l1_router"].ap().rearrange("(c p) n -> p c n", p=128), writes=["rw"])
            triU = cx.sb("triU", [128, 128], BF16)
            S.dma("pool", triU[:], T["triU"].ap(), writes=["triU"])
            onesb = cx.sb("onesb", [128, 128], BF16)
            S.op("dve", "memset", writes=["onesb"], ap=onesb[:], constant=1.0)
            eoff = cx.sb("eoff", [128, NEXP], F32)
            S.dma("sp", eoff[:], T["eoff"].ap(), writes=["eoff"])
            base = cx.sb("base", [128, NEXP], F32)
            S.op("dve", "memset", writes=["base"], ap=base[:], constant=0.0)
            xs = [cx.sb("xs%d" % i, [128, D], F32) for i in range(2)]
            xn = [cx.sb("xn%d" % i, [128, D], BF16) for i in range(3)]
            W = {"junk": cx.sb("junk", [128, D], BF16), "ss": cx.sb("ss", [128, 1], F32),
                 "rstd": cx.sb("rstd", [128, 1], F32), "lnt": cx.sb("lnt", [128, 1], F32)}
            NPB = 3
            small = []
            for k in range(NPB):
                small.append({
                    "lg": cx.sb("lg%d" % k, [128, NEXP], F32), "l2": cx.sb("l2%d" % k, [128, NEXP], F32),
                    "eq1": cx.sb("eq1%d" % k, [128, NEXP], F32), "eq2": cx.sb("eq2%d" % k, [128, NEXP], F32),
                    "selb": cx.sb("selb%d" % k, [128, NEXP], BF16), "dest": cx.sb("dest%d" % k, [128, NEXP], F32),
                    "tmp8": cx.sb("tmp8%d" % k, [128, NEXP], F32), "sm": cx.sb("sm%d" % k, [128, 12], F32),
                    "hT": cx.sb("hTr%d" % k, [128, 8, 128], BF16)})
            psT = [cx.ps("psT%d" % i, [128, 8, 128], BF16) for i in range(2)]
            psRs = [cx.ps("psR%d" % i, [128, 512], F32) for i in range(2)]
            psDs = [cx.ps("psD%d" % i, [128, 512], F32) for i in range(2)]
            psCs = [cx.ps("psC%d" % i, [128, 512], F32) for i in range(2)]
            for i in range(ntile):
                r0 = i * 128
                sb_ = small[i % NPB]
                lg, l2, eq1, eq2, selb, dest, tmp8, sm, hT = (sb_[k] for k in
                                                               ("lg", "l2", "eq1", "eq2", "selb", "dest", "tmp8", "sm",
                                                                "hT"))
                psR, psD, psC = psRs[i % 2], psDs[i % 2], psCs[i % 2]
                kk = lambda nm, i=i: nm + str(i % NPB)
                kp = lambda nm, i=i: nm + str(i % 2)
                xt, xk = xs[i % 2], "xs%d" % (i % 2)
                xnt, xnk = xn[i % 3], "xn%d" % (i % 3)
                S.dma("sp", xt[:], xin[r0:r0 + 128, :], writes=[xk])
                norm_tile(S, C, xt[:], xk, gffn[:], "gffn", xnt[:], xnk, W, "")
                k = i % 2
                transposes(S, C, [xnt[:, c * 128:(c + 1) * 128] for c in range(8)], xnk, psT[k], "psT%d" % k, hT[:],
                           kk("hT"))
                for c in range(8):
                    S.op("pe", "matmul", reads=[kk("hT"), "rw"], writes=[kp("psR")], signal=(c == 7), out=psR[:, 0:NEXP],
                         lhsT=hT[:, c, :], rhs=rw[:, c, :], start=(c == 0), stop=(c == 7))
                S.op("dve", "tensor_copy", reads=[kp("psR")], writes=[kk("lg")], out=lg[:], in_=psR[:, 0:NEXP])
                S.op("dve", "tensor_reduce", reads=[kk("lg")], writes=[kk("sm")], out=sm[:, 0:1], in_=lg[:], axis=AX.X,
                     op=ALU.max)
                S.op("dve", "tensor_scalar", reads=[kk("lg"), kk("sm")], writes=[kk("eq1")], out=eq1[:], in0=lg[:],
                     scalar1=sm[:, 0:1], scalar2=None, op0=ALU.is_equal)
                S.op("dve", "scalar_tensor_tensor", reads=[kk("eq1"), kk("lg")], writes=[kk("l2")], out=l2[:], in0=eq1[:],
                     scalar=-1e30, in1=lg[:], op0=ALU.mult, op1=ALU.add)
                S.op("dve", "tensor_reduce", reads=[kk("l2")], writes=[kk("sm")], out=sm[:, 1:2], in_=l2[:], axis=AX.X,
                     op=ALU.max)
                S.op("dve", "tensor_scalar", reads=[kk("l2"), kk("sm")], writes=[kk("eq2")], out=eq2[:], in0=l2[:],
                     scalar1=sm[:, 1:2], scalar2=None, op0=ALU.is_equal)
                S.op("dve", "tensor_tensor", reads=[kk("sm")], writes=[kk("sm")], out=sm[:, 2:3], in0=sm[:, 1:2],
                     in1=sm[:, 0:1], op=ALU.subtract)
                S.op("act", "activation", reads=[kk("sm")], writes=[kk("sm")], out=sm[:, 3:4], in_=sm[:, 2:3], func=AF.Exp)
                S.op("dve", "tensor_scalar", reads=[kk("sm")], writes=[kk("sm")], out=sm[:, 4:5], in0=sm[:, 3:4], scalar1=1.0,
                     scalar2=None, op0=ALU.add)
                S.op("dve", "reciprocal", reads=[kk("sm")], writes=[("gtb", i)], out=gtb[:, i, 0:1], in_=sm[:, 4:5])
                S.op("dve", "tensor_tensor", reads=[kk("sm"), ("gtb", i)], writes=[("gtb", i)], out=gtb[:, i, 1:2],
                     in0=sm[:, 3:4], in1=gtb[:, i, 0:1], op=ALU.mult)
                S.op("dve", "tensor_tensor", reads=[kk("eq1"), kk("eq2")], writes=[kk("selb")], out=selb[:], in0=eq1[:],
                     in1=eq2[:], op=ALU.add)
                S.op("pe", "matmul", reads=[kk("selb"), "triU"], writes=[kp("psD")], out=psD[:, 0:NEXP], lhsT=triU[:],
                     rhs=selb[:], start=True, stop=True)
                S.op("pe", "matmul", reads=[kk("selb"), "onesb"], writes=[kp("psC")], out=psC[:, 0:NEXP], lhsT=onesb[:],
                     rhs=selb[:], start=True, stop=True)
                S.op("dve", "tensor_tensor", reads=[kp("psD"), "base"], writes=[kk("dest")], out=dest[:], in0=psD[:, 0:NEXP],
                     in1=base[:], op=ALU.add)
                S.op("dve", "tensor_tensor", reads=[kp("psC"), "base"], writes=["base"], out=base[:], in0=psC[:, 0:NEXP],
                     in1=base[:], op=ALU.add)
                S.op("dve", "tensor_tensor", reads=[kk("dest"), "eoff"], writes=[kk("dest")], out=dest[:], in0=dest[:],
                     in1=eoff[:], op=ALU.add)
                for (j, eq, eqk) in ((0, eq1, kk("eq1")), (1, eq2, kk("eq2"))):
                    S.op("dve", "tensor_tensor", reads=[kk("dest"), eqk], writes=[kk("tmp8")], out=tmp8[:], in0=dest[:],
                         in1=eq[:], op=ALU.mult)
                    S.op("dve", "tensor_reduce", reads=[kk("tmp8")], writes=[kk("sm")], out=sm[:, 8 + j:9 + j], in_=tmp8[:],
                         axis=AX.X, op=ALU.add)
                S.op("dve", "tensor_copy", reads=[kk("sm")], writes=[("posb", i)], out=posb[:, i, :], in_=sm[:, 8:10])
                for j in range(2):
                    S.dma("pool", hs.ap(), xnt[:], reads=[xnk, ("posb", i)],
                          meth="indirect_dma_start", out_offset=bass.IndirectOffsetOnAxis(posb[:, i, j:j + 1], 0),
                          in_offset=None, bounds_check="BC", oob_is_err=False)
            sb_ = small[0]
            lg, l2, eq1, eq2, selb, dest, tmp8, sm, hT = (sb_[k] for k in
                                                           ("lg", "l2", "eq1", "eq2", "selb", "dest", "tmp8", "sm", "hT"))
            kk = lambda nm: nm + "0"
            S.op("dve", "tensor_scalar", reads=["base"], writes=[kk("tmp8")], out=tmp8[0:1, :], in0=base[0:1, :],
                 scalar1=1.0 / GRP, scalar2=(GRP - 1.0) / GRP, op0=ALU.mult, op1=ALU.add)
            ngi = cx.sb("ngi", [1, NEXP], I32)
            S.op("dve", "tensor_copy", reads=[kk("tmp8")], writes=["ngi"], out=ngi[:], in_=tmp8[0:1, :])
            S.op("dve", "tensor_copy", reads=["ngi"], writes=[kk("dest")], out=dest[0:1, :], in_=ngi[:])
            S.op("dve", "tensor_tensor", reads=[kk("dest"), kk("tmp8")], writes=[kk("l2")], out=l2[0:1, :], in0=dest[0:1, :],
                 in1=tmp8[0:1, :], op=ALU.is_gt)
            S.op("dve", "tensor_tensor", reads=[kk("dest"), kk("l2")], writes=[kk("dest")], out=dest[0:1, :], in0=dest[0:1, :],
                 in1=l2[0:1, :], op=ALU.subtract)
            S.op("dve", "tensor_copy", reads=[kk("dest")], writes=["ngb"], out=ngb[:], in_=dest[0:1, :])
            S.barrier()
        with ExitStack() as st:
            cx = Ctx(nc, S, st, "m1b")
            C = load_consts(nc, S, cx, T)
            hr = [cx.sb("hr%d" % i, [128, 4, D], BF16) for i in range(2)]
            hTg = cx.sb("hTg", [128, 8, GRP], BF16)
            hidT = cx.sb("hidT", [128, NF, GRP], BF16)
            wg = [cx.sb("wg%d" % i, [128, 8, 512], BF16) for i in range(2)]
            wu = [cx.sb("wu%d" % i, [128, 8, 512], BF16) for i in range(2)]
            wd = [cx.sb("wd%d" % i, [128, NF, 512], BF16) for i in range(2)]
            sg = [cx.sb("sg%d" % i, [128, 512], F32) for i in range(2)]
            yo = [cx.sb("yo%d" % i, [128, D], F32) for i in range(2)]
            psT = [cx.ps("psT%d" % i, [128, 8, 128], BF16) for i in range(2)]
            psG = [cx.ps("psG%d" % i, [128, 512], F32) for i in range(2)]
            psU = [cx.ps("psU%d" % i, [128, 512], F32) for i in range(2)]
            psY = [cx.ps("psY%d" % i, [128, 512], F32) for i in range(2)]
            wgd, wud, wdd = T["l1_we_gate_bf"].ap(), T["l1_we_up_bf"].ap(), T["l1_we_down_bf"].ap()
            ctr = {"t": 0, "g": 0, "y": 0, "w": 0, "h": 0, "o": 0}

            def load_w(e, bi):
                n0 = bi * 512
                i = ctr["w"] % 2
                ctr["w"] += 1
                S.dma("sp", wg[i][:], wgd[e, :, n0:n0 + 512].rearrange("(c p) n -> p c n", p=128),
                      writes=["wg%d" % i])
                S.dma("sp", wu[i][:], wud[e, :, n0:n0 + 512].rearrange("(c p) n -> p c n", p=128),
                      writes=["wu%d" % i])
                return i

            def load_wd(e):
                for hh in range(2):
                    src = wdd[e, :, hh * 512:(hh + 1) * 512].rearrange("(f p) n -> p f n", p=128)
                    for f0 in range(0, NF, 14):
                        S.dma("pool", wd[hh][:, f0:f0 + 14, :], src[:, f0:f0 + 14, :], writes=["wd%d" % hh])

            for e in range(NEXP):
                S.reg_load_all(ngb[0:1, e:e + 1], reads=["ngb"])
                for r in range(maxgrp):
                    S.cond_begin(r + 1)
                    R0 = e * ECAP + r * GRP
                    hb = ctr["h"] % 2
                    ctr["h"] += 1
                    S.dma("sp", hr[hb][:], hs.ap()[R0:R0 + GRP, :].rearrange("(j p) d -> p j d", p=128),
                          writes=["hr%d" % hb])
                    wi = load_w(e, 0)
                    for j in range(4):
                        k = ctr["t"] % 2
                        ctr["t"] += 1
                        transposes(S, C, [hr[hb][:, j, c * 128:(c + 1) * 128] for c in range(8)], "hr%d" % hb, psT[k],
                                   "psT%d" % k, hTg[:, :, j * 128:(j + 1) * 128], "hTg",
                                   evac=("dve" if j % 2 == 0 else "act"))
                    for bi in range(7):
                        cur = wi
                        if bi + 1 < 7:
                            wi = load_w(e, bi + 1)
                        if bi == 1:
                            load_wd(e)
                        for j in range(4):
                            f = bi * 4 + j
                            k = ctr["g"] % 2
                            ctr["g"] += 1
                            for c in range(8):
                                S.op("pe", "matmul", reads=["hTg", "wg%d" % cur], writes=["psG%d" % k],
                                     signal=(c == 7), out=psG[k][:], lhsT=wg[cur][:, c, j * 128:(j + 1) * 128],
                                     rhs=hTg[:, c, :], start=(c == 0), stop=(c == 7))
                            for c in range(8):
                                S.op("pe", "matmul", reads=["hTg", "wu%d" % cur], writes=["psU%d" % k],
                                     signal=(c == 7), out=psU[k][:], lhsT=wu[cur][:, c, j * 128:(j + 1) * 128],
                                     rhs=hTg[:, c, :], start=(c == 0), stop=(c == 7))
                            S.op("act", "activation", reads=["psG%d" % k], writes=["sg%d" % k], out=sg[k][:],
                                 in_=psG[k][:], func=AF.Silu)
                            S.op("dve", "tensor_tensor", reads=["sg%d" % k, "psU%d" % k], writes=[("hidT", f)],
                                 out=hidT[:, f, :], in0=sg[k][:], in1=psU[k][:], op=ALU.mult)
                    for j in range(4):
                        ob = ctr["o"] % 2
                        ctr["o"] += 1
                        for hh in range(2):
                            k = ctr["y"] % 2
                            ctr["y"] += 1
                            for f in range(NF):
                                S.op("pe", "matmul", reads=[("hidT", f), "wd%d" % hh], writes=["psY%d" % k],
                                     signal=(f == NF - 1), out=psY[k][:], lhsT=hidT[:, f, j * 128:(j + 1) * 128],
                                     rhs=wd[hh][:, f, :], start=(f == 0), stop=(f == NF - 1))
                            if hh == 0:
                                S.op("act", "copy", reads=["psY%d" % k], writes=["yo%d" % ob],
                                     out=yo[ob][:, 0:512], in_=psY[k][:])
                            else:
                                S.op("dve", "tensor_copy", reads=["psY%d" % k], writes=["yo%d" % ob],
                                     out=yo[ob][:, 512:1024], in_=psY[k][:])
                        S.dma("pool", Y.ap()[R0 + j * 128:R0 + (j + 1) * 128, :], yo[ob][:], reads=["yo%d" % ob])
                    S.cond_end()
            S.barrier()
        with ExitStack() as st:
            cx = Ctx(nc, S, st, "m1c")
            NB3 = 3
            xs = [cx.sb("xs%d" % i, [128, D], F32) for i in range(NB3)]
            y1 = [cx.sb("y1_%d" % i, [128, D], F32) for i in range(NB3)]
            y2 = [cx.sb("y2_%d" % i, [128, D], F32) for i in range(NB3)]

            def fetch(i):
                k = i % NB3
                S.dma("sp", xs[k][:], xin[i * 128:(i + 1) * 128, :], writes=["xs%d" % k])
                for (j, yb, nm) in ((0, y1[k], "y1_%d" % k), (1, y2[k], "y2_%d" % k)):
                    S.dma("pool", yb[:], Y.ap(), reads=[("posb", i)], writes=[nm], meth="indirect_dma_start",
                          out_offset=None, in_offset=bass.IndirectOffsetOnAxis(posb[:, i, j:j + 1], 0),
                          bounds_check="BC", oob_is_err=False)

            fetch(0)
            if ntile > 1:
                fetch(1)
            for i in range(ntile):
                r0 = i * 128
                k = i % NB3
                S.op("dve", "scalar_tensor_tensor", reads=["y1_%d" % k, "xs%d" % k], writes=["xs%d" % k],
                     out=xs[k][:], in0=y1[k][:], scalar=gtb[:, i, 0:1], in1=xs[k][:], op0=ALU.mult, op1=ALU.add)
                S.op("dve", "scalar_tensor_tensor", reads=["y2_%d" % k, "xs%d" % k], writes=["xs%d" % k],
                     out=xs[k][:], in0=y2[k][:], scalar=gtb[:, i, 1:2], in1=xs[k][:], op0=ALU.mult, op1=ALU.add)
                if i + 2 < ntile:
                    fetch(i + 2)
                S.dma("sp", xo[r0:r0 + 128, :], xs[k][:], reads=["xs%d" % k])
            S.barrier()


WEIGHT_NAMES = [
    ("l0_attn_norm", [D]), ("l0_w_in", [D, L0_IN]), ("l0_a_q_norm", [64]), ("l0_a_k_norm", [64]),
    ("l0_a_sinks", [8]), ("l0_b_q_norm", [64]), ("l0_b_k_norm", [64]), ("l0_w_out", [D, D]),
    ("l0_ffn_norm", [D]), ("l0_w_gate", [D, DFF]), ("l0_w_up", [D, DFF]), ("l0_w_down", [DFF, D]),
    ("l1_attn_norm", [D]), ("l1_w_in", [D, L1_IN]), ("l1_q_a_norm", [384]), ("l1_w_uq", [384, 1536]),
    ("l1_kv_a_norm", [256]), ("l1_w_ukv", [256, 2048]), ("l1_c_q_norm", [96]), ("l1_c_k_norm", [96]),
    ("l1_w_out", [D, D]), ("l1_ffn_norm", [D]), ("l1_router", [D, NEXP]),
    ("l1_we_gate", [NEXP, D, DFE]), ("l1_we_up", [NEXP, D, DFE]), ("l1_we_down", [NEXP, DFE, D]),
]


def host_consts():
    p = np.arange(128)[:, None]
    j = np.arange(128)[None, :]
    NEG = -10000.0
    maskA = np.where(np.stack([(j < p), (j >= p)], axis=1), 0.0, NEG).astype(np.float32)
    mb = np.zeros((128, 16, 128), np.float32)
    for o in range(16):
        d = o * 128 + j - p
        c = ((d >= 0) & (d <= 128)).astype(np.float32) + ((d >= 0) & (d % 4 == 0) & (d <= 512)) + \
            ((d >= 0) & (d % 16 == 0) & (d <= 2048))
        with np.errstate(divide="ignore"):
            mb[:, 15 - o, :] = np.where(c > 0, np.log(np.maximum(c, 1.0)) / 0.125, NEG)
    inv_h = (10000.0 ** (-np.arange(0, 64, 2, dtype=np.float32) / 64)).astype(np.float32)
    inv_c = (10000.0 ** (-np.arange(0, 32, 2, dtype=np.float32) / 32)).astype(np.float32)
    return {
        "ident": np.eye(128, dtype=np.float32),
        "maskA": maskA,
        "maskB": mb,
        "inv_h": np.ascontiguousarray(np.broadcast_to(inv_h[None, :], (128, 32))),
        "inv_c": np.ascontiguousarray(np.broadcast_to(inv_c[None, :], (128, 16))),
        "triU": (p < j).astype(np.float32),
        "eoff": np.ascontiguousarray(np.broadcast_to((np.arange(8, dtype=np.float32) * 8192)[None, :], (128, 8))),
    }


def build_program(nseq, phases, ntiles=NT):
    nc = bass.Bass("TRN2", target_bir_lowering=False)
    T = {}
    rows = nseq * SEQ
    T["x"] = nc.dram_tensor("x", [rows, D], F32, kind="ExternalInput")
    T["pos_t"] = nc.dram_tensor("pos_t", [nseq, 128, NT], I32, kind="ExternalInput")
    for nm, shp in WEIGHT_NAMES:
        T[nm] = nc.dram_tensor(nm, shp, F32, kind="ExternalInput")
    for nm, arr in host_consts().items():
        T[nm] = nc.dram_tensor(nm, list(arr.shape), F32, kind="ExternalInput")
    T["out"] = nc.dram_tensor("out", [rows, D], F32, kind="ExternalOutput")
    nr = ntiles * 128 if ntiles < NT else rows
    with ExitStack() as st:
        S = Sched(nc, st)
        if "f0" in phases and "a0" in phases:
            for nm, shp in (("l0_w_gate", [D, DFF]), ("l0_w_up", [D, DFF]), ("l0_w_down", [DFF, D])):
                T[nm + "_bf"] = nc.dram_tensor(nm + "_bf", shp, BF16)
                wr = shp[0]
                for r0 in range(0, wr, 512):
                    rc = min(512, wr - r0)

                    def job(nm=nm, r0=r0, rc=rc, ncol=shp[1]):
                        src = T[nm].ap()[r0:r0 + rc, :].rearrange("r (a b) -> r a b", b=256)
                        dst = T[nm + "_bf"].ap()[r0:r0 + rc, :].rearrange("r (a b) -> r a b", b=256)
                        S.dma("pool", dst, src)
                    S.bg_jobs.append(job)
        if "m1" in phases:
            for nm, shp in (("l1_we_gate", [NEXP, D, DFE]), ("l1_we_up", [NEXP, D, DFE]),
                            ("l1_we_down", [NEXP, DFE, D])):
                T[nm + "_bf"] = nc.dram_tensor(nm + "_bf", shp, BF16)
            for e in range(NEXP):
                for nm, wrows in (("l1_we_gate", D), ("l1_we_up", D), ("l1_we_down", DFE)):
                    nchunk = 4
                    rc = wrows // nchunk
                    for ci in range(nchunk):
                        def job(nm=nm, e=e, r0=ci * rc, rc=rc):
                            src = T[nm].ap()[e, r0:r0 + rc, :].rearrange("r (a b) -> r a b", b=512)
                            dst = T[nm + "_bf"].ap()[e, r0:r0 + rc, :].rearrange("r (a b) -> r a b", b=512)
                            S.dma("pool", dst, src)
                        S.bg_jobs.append(job)
        cur = T["x"]
        for i, ph in enumerate(phases):
            dst = T["out"] if i == len(phases) - 1 else nc.dram_tensor("xr%d" % i, [rows, D], F32)
            if ph == "a0":
                phase_l0_attn(nc, S, T, cur, dst, nseq, ntiles)
            elif ph == "f0":
                phase_l0_ffn(nc, S, T, cur, dst, nr)
            elif ph == "a1":
                phase_l1_attn(nc, S, T, cur, dst, nseq, ntiles)
            elif ph == "m1":
                S.bg_step(len(S.bg_jobs))
                phase_l1_moe_sparse(nc, S, T, cur, dst, nr)
            elif ph == "m1d":
                phase_l1_moe(nc, S, T, cur, dst, nr)
            cur = dst
        S.finish()
        S.emit()
    return nc


PHASES = ["a0", "f0", "a1", "m1"]
N_CORES = 8
_PROG = {}


def kernel(**inputs):
    x = np.asarray(inputs["x"], dtype=np.float32)
    pos = np.asarray(inputs["positions"]).astype(np.int32)
    B = x.shape[0]
    nseq = B // N_CORES
    if nseq not in _PROG:
        _PROG[nseq] = build_program(nseq, PHASES)
    nc = _PROG[nseq]
    consts = host_consts()
    shared = {nm: np.ascontiguousarray(np.asarray(inputs[nm], dtype=np.float32)) for nm, _ in WEIGHT_NAMES}
    shared.update(consts)
    in_maps = []
    for c in range(N_CORES):
        m = dict(shared)
        m["x"] = np.ascontiguousarray(x[c * nseq:(c + 1) * nseq].reshape(nseq * SEQ, D))
        m["pos_t"] = np.ascontiguousarray(pos[c * nseq:(c + 1) * nseq].reshape(nseq, NT, 128).transpose(0, 2, 1))
        in_maps.append(m)
    res = run_bass_kernel_spmd(nc, in_maps, core_ids=list(range(N_CORES)))
    outs = [np.asarray(r["out"]).reshape(nseq, SEQ, D) for r in res.results]
    return np.concatenate(outs, axis=0).astype(np.float32)
```

```python
import numpy as np
from contextlib import ExitStack
import concourse.bass as bass
import concourse.mybir as mybir
from concourse.bass_utils import run_bass_kernel_spmd

F32 = mybir.dt.float32
BF16 = mybir.dt.bfloat16
I32 = mybir.dt.int32
AF = mybir.ActivationFunctionType
ALU = mybir.AluOpType
AX = mybir.AxisListType

D = 1024
SEQ = 2048
NT = SEQ // 128
L0_IN = 2304
DFF = 2816
L1_IN = 672
NEXP = 8
DFE = 3584
EPS = 1e-6
PI = float(np.pi)
ECAP = 8192
GRP = 512


class Sched:
    def __init__(self, nc, stack, ring=10):
        self.nc = nc
        self.eng = {"pe": nc.tensor, "act": nc.scalar, "dve": nc.vector, "pool": nc.gpsimd, "sp": nc.sync}
        self.sem = {k: stack.enter_context(nc.semaphore("s_" + k)) for k in self.eng}
        self.cnt = {k: 0 for k in self.eng}
        self.seen = {k: {} for k in self.eng}
        self.rings = {}
        for q in ("sp", "pool"):
            self.rings[q] = [[stack.enter_context(nc.semaphore("d_%s%d" % (q, i))), 0] for i in range(ring)]
        self.ring_pos = {q: 0 for q in self.rings}
        self.lastw = {}
        self.readers = {}
        self.prog = {k: [] for k in self.eng}
        self.dma_toks = []
        self.bg_jobs = []

    def bg_step(self, n=1):
        for _ in range(n):
            if self.bg_jobs:
                self.bg_jobs.pop(0)()

    def _wait(self, e, tok):
        sem, val, owner = tok
        if owner == e and e == "pe":
            return
        name = id(sem)
        if self.seen[e].get(name, 0) >= val:
            return
        self.prog[e].append(("w", sem, val))
        self.seen[e][name] = val

    def _deps(self, e, reads, writes):
        for k in reads:
            t = self.lastw.get(k)
            if t is not None:
                self._wait(e, t)
        for k in writes:
            t = self.lastw.get(k)
            if t is not None:
                self._wait(e, t)
            for t in self.readers.get(k, ()):
                self._wait(e, t)

    def _commit(self, tok, reads, writes):
        for k in reads:
            self.readers.setdefault(k, []).append(tok)
        for k in writes:
            self.lastw[k] = tok
            self.readers[k] = []

    def op(self, e, meth, reads=(), writes=(), signal=True, **kw):
        self._deps(e, reads, writes)
        if signal:
            self.cnt[e] += 1
            self.prog[e].append(("i", meth, kw, self.sem[e], 1))
            tok = (self.sem[e], self.cnt[e], e)
        else:
            self.prog[e].append(("i", meth, kw, None, 0))
            tok = (self.sem[e], self.cnt[e] + 1, e)
        self._commit(tok, reads, writes)
        return tok

    def dma(self, q, out, in_, reads=(), writes=(), meth="dma_start", **kw):
        self._deps(q, reads, writes)
        ring = self.rings[q]
        i = self.ring_pos[q]
        self.ring_pos[q] = (i + 1) % len(ring)
        sem, n = ring[i]
        if n > 0:
            self._wait(q, (sem, 16 * n, "dma"))
        self.prog[q].append(("i", meth, dict(out=out, in_=in_, **kw), sem, 16))
        ring[i][1] = n + 1
        tok = (sem, 16 * (n + 1), "dma")
        self._commit(tok, reads, writes)
        self.dma_toks.append(tok)
        return tok

    def reg_load_all(self, ap, reads=()):
        for e in self.eng:
            self._deps(e, reads, ())
            self.prog[e].append(("rl", ap))
            tok = (self.sem[e], self.cnt[e] + 1, e) if False else None

    def cond_begin(self, thresh):
        self.cond = {}
        start = {id(self.sem[o]): self.cnt[o] for o in self.eng}
        for q in self.rings:
            for sem, n in self.rings[q]:
                start[id(sem)] = 16 * n
        self.cond_start = start
        for e in self.eng:
            info = {"thresh": thresh, "pos": len(self.prog[e]), "cnt0": self.cnt[e]}
            if e in self.rings:
                info["ring0"] = [n for (_, n) in self.rings[e]]
            self.prog[e].append(("cb", info))
            self.cond[e] = info

    def cond_end(self):
        for e in self.eng:
            info = self.cond[e]
            body = self.prog[e][info["pos"] + 1:]
            info["waits"] = [(it[1], it[2]) for it in body if it[0] == "w" and it[2] <= self.cond_start[id(it[1])]]
            info["delta"] = self.cnt[e] - info["cnt0"]
            if e in self.rings:
                info["rings"] = [(sem, n0, n - n0) for (sem, n), n0 in zip(self.rings[e], info["ring0"])]
            self.prog[e].append(("ce",))
        self.cond = None

    def barrier(self):
        toks = [(self.sem[o], self.cnt[o], o) for o in self.eng if self.cnt[o] > 0]
        for q in self.rings:
            for sem, n in self.rings[q]:
                if n > 0:
                    toks.append((sem, 16 * n, "dma"))
        for e in self.eng:
            for t in toks:
                if t[2] == e:
                    continue
                self._wait(e, t)
        self.lastw.clear()
        self.readers.clear()
        self.dma_toks = []

    def finish(self):
        for sem, n in self.rings["sp"] + self.rings["pool"]:
            if n > 0:
                self._wait("sp", (sem, 16 * n, "dma"))

    def emit(self):
        nc = self.nc
        with nc.Block() as block:
            def mk(name):
                def body(e):
                    reg = None
                    guard = None
                    bcreg = None
                    if name == "pool":
                        bcreg = e.alloc_register("bc")
                        e.reg_mov(bcreg, NEXP * ECAP - 1)
                    for it in self.prog[name]:
                        if it[0] == "w":
                            e.wait_ge(it[1], it[2])
                        elif it[0] == "rl":
                            if reg is None:
                                reg = e.alloc_register("pred_" + name)
                            e.reg_load(reg, it[1])
                        elif it[0] == "cb":
                            info = it[1]
                            g = e.If_lt(reg, info["thresh"])
                            g.__enter__()
                            for (sem, val) in info["waits"]:
                                e.wait_ge(sem, val)
                            if info["delta"] > 0:
                                if info["cnt0"] > 0:
                                    e.wait_ge(self.sem[name], info["cnt0"])
                                e.sem_inc(self.sem[name], info["delta"])
                            for (sem, n0, dn) in info.get("rings", ()):
                                if dn > 0:
                                    if n0 > 0:
                                        e.wait_ge(sem, 16 * n0)
                                    e.sem_inc(sem, 16 * dn)
                            g.__exit__(None, None, None)
                            guard = e.Else()
                            guard.__enter__()
                        elif it[0] == "ce":
                            guard.__exit__(None, None, None)
                            guard = None
                        else:
                            kw = it[2]
                            if kw.get("bounds_check", None) == "BC":
                                kw = dict(kw)
                                kw["bounds_check"] = bcreg
                            ins = getattr(e, it[1])(**kw)
                            if it[3] is not None:
                                ins.then_inc(it[3], it[4])
                return body
            block.sync(mk("sp"))
            block.scalar(mk("act"))
            block.vector(mk("dve"))
            block.gpsimd(mk("pool"))
            block.tensor(mk("pe"))


class Ctx:
    def __init__(self, nc, S, stack, pfx):
        self.nc, self.S, self.st, self.pfx = nc, S, stack, pfx

    def sb(self, name, shape, dt):
        return self.st.enter_context(self.nc.sbuf_tensor(self.pfx + "s_" + name, shape, dt))

    def ps(self, name, shape, dt):
        return self.st.enter_context(self.nc.psum_tensor(self.pfx + "p_" + name, shape, dt))


def bc_dram(handle, parts, inner):
    return bass.AP(handle, 0, [[0, parts], [1, inner]])


def load_consts(nc, S, cx, T, need_mb=False, need_ma=False):
    C = {}
    C["ident"] = cx.sb("ident", [128, 128], BF16)
    S.dma("pool", C["ident"][:], T["ident"].ap(), writes=["ident"])
    C["eps"] = cx.sb("eps", [128, 1], F32)
    S.op("dve", "memset", writes=["eps"], ap=C["eps"][:], constant=EPS)
    C["pib"] = cx.sb("pib", [128, 1], F32)
    S.op("dve", "memset", writes=["pib"], ap=C["pib"][:], constant=PI)
    if need_ma:
        C["MA"] = cx.sb("MA", [128, 2, 128], BF16)
        S.dma("pool", C["MA"][:], T["maskA"].ap(), writes=["masks"])
    if need_mb:
        C["MB"] = cx.sb("MB", [128, 16, 128], BF16)
        S.dma("pool", C["MB"][:], T["maskB"].ap(), writes=["masks"])
    return C


def rms_rstd(S, ss_ap, out_ap, tmp_ap, scale, eps_ap, keys_r, keys_w, tmpkey):
    S.op("act", "activation", reads=list(keys_r) + ["eps"], writes=[tmpkey], out=tmp_ap, in_=ss_ap, func=AF.Ln,
         scale=scale, bias=eps_ap)
    S.op("act", "activation", reads=[tmpkey], writes=list(keys_w), out=out_ap, in_=tmp_ap, func=AF.Exp, scale=-0.5)


def norm_tile(S, C, xt, xkey, gain, gkey, xn, xnkey, W, sfx):
    S.op("dve", "scalar_tensor_tensor", reads=[xkey], writes=["junk" + sfx, "ss" + sfx], out=W["junk"][:, 0:D], in0=xt,
         scalar=1.0, in1=xt, op0=ALU.mult, op1=ALU.mult, accum_out=W["ss"][:])
    rms_rstd(S, W["ss"][:], W["rstd"][:], W["lnt"][:], 1.0 / D, C["eps"][:], ["ss" + sfx], ["rstd" + sfx],
             "lnt" + sfx)
    S.op("dve", "scalar_tensor_tensor", reads=[xkey, "rstd" + sfx, gkey], writes=[xnkey], out=xn, in0=xt,
         scalar=W["rstd"][:, 0:1], in1=gain, op0=ALU.mult, op1=ALU.mult)


def transposes(S, C, src_aps, srckey, psT, pskey, dst_ap, dstkey, nrows=128, evac="dve"):
    n = len(src_aps)
    for i, a in enumerate(src_aps):
        w = a.shape[-1] if len(a.shape) == 2 else None
        S.op("pe", "transpose", reads=[srckey, "ident"], writes=[pskey], signal=(i == n - 1),
             out=psT[0:nrows, i, :], in_=a, identity=C["ident"][:])
    if evac == "dve":
        S.op("dve", "tensor_copy", reads=[pskey], writes=[dstkey], out=dst_ap, in_=psT[0:nrows, 0:n, :])
    else:
        S.op("act", "copy", reads=[pskey], writes=[dstkey], out=dst_ap, in_=psT[0:nrows, 0:n, :])


def rope_tables(S, C, cx, pos_ap, invt, nfreq, sfx):
    W = C["rope" + sfx]
    AP_ = lambda x: x if isinstance(x, bass.AP) else x[:]
    S.dma("sp", AP_(W["posi"]), pos_ap, writes=["posi" + sfx])
    S.op("dve", "tensor_copy", reads=["posi" + sfx], writes=["posf" + sfx], out=AP_(W["posf"]), in_=AP_(W["posi"]))
    pf = AP_(W["posf"]).unsqueeze(2).to_broadcast([128, NT, nfreq])
    iv = invt[:].unsqueeze(1).to_broadcast([128, NT, nfreq])
    S.op("dve", "tensor_tensor", reads=["posf" + sfx, "inv" + sfx], writes=["ang" + sfx], out=AP_(W["ang"]), in0=pf,
         in1=iv, op=ALU.mult)
    for (nm, shift) in (("sin", 0.0), ("cos", PI / 2)):
        S.op("dve", "tensor_scalar", reads=["ang" + sfx], writes=["rr" + sfx], out=AP_(W["rr"]), in0=AP_(W["ang"]),
             scalar1=shift, scalar2=1.0 / (2 * PI), op0=ALU.add, op1=ALU.mult)
        S.op("dve", "tensor_copy", reads=["rr" + sfx], writes=["ki" + sfx], out=AP_(W["ki"]), in_=AP_(W["rr"]))
        S.op("dve", "tensor_copy", reads=["ki" + sfx], writes=["kf" + sfx], out=AP_(W["kf"]), in_=AP_(W["ki"]))
        S.op("dve", "tensor_scalar", reads=["ang" + sfx], writes=["rr" + sfx], out=AP_(W["rr"]), in0=AP_(W["ang"]),
             scalar1=shift, scalar2=None, op0=ALU.add)
        S.op("dve", "scalar_tensor_tensor", reads=["kf" + sfx, "rr" + sfx], writes=["rr" + sfx], out=AP_(W["rr"]),
             in0=AP_(W["kf"]), scalar=-2 * PI, in1=AP_(W["rr"]), op0=ALU.mult, op1=ALU.add)
        S.op("dve", "tensor_scalar", reads=["rr" + sfx], writes=["kf" + sfx], out=AP_(W["kf"]), in0=AP_(W["rr"]),
             scalar1=PI, scalar2=None, op0=ALU.is_gt)
        S.op("dve", "scalar_tensor_tensor", reads=["kf" + sfx, "rr" + sfx], writes=["rr" + sfx], out=AP_(W["rr"]),
             in0=AP_(W["kf"]), scalar=-2 * PI, in1=AP_(W["rr"]), op0=ALU.mult, op1=ALU.add)
        S.op("dve", "tensor_scalar", reads=["rr" + sfx], writes=["rr" + sfx], out=AP_(W["rr"]), in0=AP_(W["rr"]),
             scalar1=PI, scalar2=-PI, op0=ALU.min, op1=ALU.max)
        S.op("act", "activation", reads=["rr" + sfx], writes=[nm + sfx], out=AP_(W[nm]), in_=AP_(W["rr"]), func=AF.Sin)


def alloc_rope(cx, nfreq, sfx, big=False):
    W = {
        "posi": cx.sb("posi" + sfx, [128, NT], I32),
        "posf": cx.sb("posf" + sfx, [128, NT], F32),
        "ki": cx.sb("ki" + sfx, [128, NT, nfreq], I32),
        "sin": cx.sb("sin" + sfx, [128, NT, nfreq], F32),
        "cos": cx.sb("cos" + sfx, [128, NT, nfreq], F32),
    }
    for nm in ("ang", "rr", "kf"):
        if big:
            full = cx.sb(nm + sfx, [128, NT, 2 * nfreq], F32)
            W[nm + "_full"] = full
            W[nm] = full[:, :, 0:nfreq]
        else:
            W[nm] = cx.sb(nm + sfx, [128, NT, nfreq], F32)
    return W


def make_jobs(head, kbs):
    jobs = []
    nkb = len(kbs)
    for c0 in range(0, nkb, 4):
        jobs.append({"h": head, "c0": c0, "chunk": kbs[c0:c0 + 4], "nkb": nkb})
    return jobs


def job_S(S, bufs, ctr, job):
    h = job["h"]
    psS, pT = bufs["psS"], bufs["pT"]
    i_s = ctr["s"] % len(psS)
    ctr["s"] += 1
    i_p = ctr["p"] % len(pT)
    ctr["p"] += 1
    ps, pskey = psS[i_s], "psS%d" % i_s
    pt, ptkey = pT[i_p], "pT%d" % i_p
    job["pt"], job["ptkey"] = pt, ptkey
    chunk = job["chunk"]
    n = len(chunk)
    madd = h["mask_fn"](job["c0"], chunk)
    lo, hi = (madd[0], madd[1]) if madd is not None else (0, 0)
    order = [i for i in range(n) if not (lo <= i < hi)] + [i for i in range(n) if lo <= i < hi]
    for pos, i in enumerate(order):
        kb = chunk[i]
        masked = lo <= i < hi
        if masked and i == lo:
            S.op("pe", "matmul", reads=["ident", "masks"], writes=[pskey], signal=False,
                 out=ps[:, lo * 128:hi * 128], lhsT=bufs["ident"], rhs=madd[2], start=True, stop=False)
        S.op("pe", "matmul", reads=[h["qkey"], h["kkey"](kb)], writes=[pskey], signal=(pos == n - 1),
             out=ps[:, i * 128:(i + 1) * 128], lhsT=h["kT_fn"](kb), rhs=h["q_ap"], start=(not masked),
             stop=((not masked) or i == hi - 1))
    S.op("act", "activation", reads=[pskey], writes=[ptkey], out=pt[:, 0:n * 128], in_=ps[:, 0:n * 128],
         func=AF.Exp, scale=h["scale"])


def job_PV(S, bufs, ctr, job):
    h = job["h"]
    psO = bufs["psO"]
    if job["c0"] == 0:
        io = ctr["o"] % len(psO)
        ctr["o"] += 1
        h["po"], h["pokey"] = psO[io], "psO%d" % io
    po, pokey = h["po"], h["pokey"]
    chunk = job["chunk"]
    n = len(chunk)
    pt, ptkey = job["pt"], job["ptkey"]
    for i, kb in enumerate(chunk):
        gi = job["c0"] + i
        S.op("pe", "matmul", reads=[ptkey, h["vkey"](kb)], writes=[pokey], signal=(i == n - 1),
             out=po[:, 0:65], lhsT=pt[:, i * 128:(i + 1) * 128], rhs=h["v_fn"](kb), start=(gi == 0),
             stop=(gi == job["nkb"] - 1))
    if job["c0"] + n == job["nkb"]:
        h["fin_fn"](po, pokey)


def run_jobs(S, bufs, ctr, jobs, fillers):
    n = len(jobs)
    nf = len(fillers)
    fi = 0
    LA = len(bufs["psS"]) - 1
    for k in range(n + LA):
        if k < n:
            job_S(S, bufs, ctr, jobs[k])
        if k >= LA:
            job_PV(S, bufs, ctr, jobs[k - LA])
        while fi < nf and (k + 1) * nf >= (fi + 1) * (n + LA):
            fillers[fi]()
            fi += 1
    while fi < nf:
        fillers[fi]()
        fi += 1


def phase_l0_attn(nc, S, T, x_in, x_out, nseq, ntiles=NT):
    with ExitStack() as st:
        cx = Ctx(nc, S, st, "a0")
        C = load_consts(nc, S, cx, T, need_mb=True, need_ma=True)
        w_in = cx.sb("w_in", [128, 8, L0_IN], BF16)
        for c in range(8):
            for hi in range(2):
                S.dma("pool", w_in[:, c, 0:512].rearrange("p (lo hi d) -> p lo hi d", lo=4, hi=2)[:, :, hi, :],
                      T["l0_w_in"].ap()[c * 128:(c + 1) * 128, hi * 256:(hi + 1) * 256].rearrange(
                          "k (lo d) -> k lo d", d=64), writes=["w_in"])
            S.dma("pool", w_in[:, c, 512:L0_IN], T["l0_w_in"].ap()[c * 128:(c + 1) * 128, 512:L0_IN],
                  writes=["w_in"])
        w_out = cx.sb("w_out", [128, 8, D], BF16)
        S.dma("pool", w_out[:], T["l0_w_out"].ap().rearrange("(c p) n -> p c n", p=128), writes=["w_out"])
        gattn = cx.sb("gattn", [128, D], F32)
        S.dma("sp", gattn[:], bc_dram(T["l0_attn_norm"], 128, D), writes=["gattn"])
        gfull = cx.sb("gfull", [128, 26, 64], F32)
        for (h0, nh, nm) in ((0, 8, "l0_a_q_norm"), (8, 2, "l0_a_k_norm"), (10, 8, "l0_b_q_norm"),
                             (18, 8, "l0_b_k_norm")):
            S.dma("sp", gfull[:, h0:h0 + nh, :], bass.AP(T[nm], 0, [[0, 128], [0, nh], [1, 64]]), writes=["gfull"])
        esink = cx.sb("esink", [128, 8], F32)
        S.dma("sp", esink[:], bc_dram(T["l0_a_sinks"], 128, 8), writes=["esink"])
        S.op("act", "activation", reads=["esink"], writes=["esink"], out=esink[:], in_=esink[:], func=AF.Exp)
        invh = cx.sb("invh", [128, 32], F32)
        S.dma("sp", invh[:], T["inv_h"].ap(), writes=["inv_h"])
        C["rope_h"] = alloc_rope(cx, 32, "_h")

        kT = cx.sb("kT", [128, 5, SEQ], BF16)
        Vaug = cx.sb("Vaug", [128, NT, 10, 65], BF16)
        S.op("dve", "memset", writes=[("Vaug", i) for i in range(NT)], ap=Vaug[:], constant=1.0)
        NB = 3
        xt = [cx.sb("xt%d" % i, [128, D], F32) for i in range(NB)]
        qT = [cx.sb("qT%d" % i, [128, 8, 128], BF16) for i in range(2)]
        xn = cx.sb("xn", [128, D], BF16)
        hT = cx.sb("hT", [128, 8, 128], BF16)
        proj = cx.sb("proj", [128, L0_IN], F32)
        W = {"junk": cx.sb("junk", [128, D], BF16), "ss": cx.sb("ss", [128, 1], F32),
             "rstd": cx.sb("rstd", [128, 1], F32), "lnt": cx.sb("lnt", [128, 1], F32)}
        sqb = cx.sb("sqb", [128, 1024], BF16)
        ssh = cx.sb("ssh", [128, 26], F32)
        rsh = cx.sb("rsh", [128, 26], F32)
        lnh = cx.sb("lnh", [128, 26], F32)
        tn = cx.sb("tn", [128, 16, 64], F32)
        tcb = cx.sb("tcb", [128, 16, 64], F32)
        tsb = cx.sb("tsb", [128, 16, 64], F32)
        qk = cx.sb("qk", [128, 26, 64], BF16)
        pT = [cx.sb("pT%d" % i, [128, 512], BF16) for i in range(4)]
        mix = cx.sb("mix", [128, D], BF16)
        mixT = cx.sb("mixT", [128, 8, 128], BF16)
        x1 = cx.sb("x1", [128, D], F32)
        den = cx.sb("den", [128, 32], F32)
        obufs = [cx.sb("obuf%d" % i, [128, 16, 65], F32) for i in range(2)]
        psT = [cx.ps("psT%d" % i, [128, 8, 128], BF16) for i in range(2)]
        psP = [cx.ps("psP%d" % i, [128, 512], F32) for i in range(1)]
        psS = [cx.ps("psS%d" % i, [128, 512], F32) for i in range(3)]
        psO = [cx.ps("psO%d" % i, [128, 512], F32) for i in range(2)]
        bufs = {"psS": psS, "pT": pT, "psO": psO, "ident": C["ident"][:]}
        ctr = {"s": 0, "p": 0, "o": 0, "t": 0, "pp": 0, "d": 0}

        def next_psT():
            i = ctr["t"] % 2
            ctr["t"] += 1
            return psT[i], "psT%d" % i

        def next_psP():
            i = ctr["pp"] % len(psP)
            ctr["pp"] += 1
            return psP[i], "psP%d" % i

        KK = lambda kb: ("kT", kb)
        VK = lambda kb: ("Vaug", kb)
        xin = x_in.ap() if hasattr(x_in, "ap") else x_in
        xo = x_out.ap() if hasattr(x_out, "ap") else x_out

        RW = C["rope_h"]

        def stageA(b, t):
            row0 = b * SEQ + t * 128
            gi = b * ntiles + t
            xtt, xkey = xt[gi % NB], "xt%d" % (gi % NB)
            steps = []

            def s1a():
                S.dma("sp", xtt[:], xin[row0:row0 + 128, :], writes=[xkey])
                norm_tile(S, C, xtt[:], xkey, gattn[:], "gattn", xn[:], "xn", W, "")

            def s1b():
                p_, pk = next_psT()
                transposes(S, C, [xn[:, c * 128:(c + 1) * 128] for c in range(8)], "xn", p_, pk, hT[:], "hT")
            steps.append(s1a)
            steps.append(s1b)

            def s2(n0):
                nw = min(512, L0_IN - n0)
                pp, ppk = next_psP()
                for c in range(8):
                    S.op("pe", "matmul", reads=["hT", "w_in"], writes=[ppk], signal=(c == 7), out=pp[:, 0:nw],
                         lhsT=hT[:, c, :], rhs=w_in[:, c, n0:n0 + nw], start=(c == 0), stop=(c == 7))
                S.op("act", "copy", reads=[ppk], writes=["proj"], out=proj[:, n0:n0 + nw], in_=pp[:, 0:nw])
            for n0 in range(0, L0_IN, 512):
                steps.append(lambda n0=n0: s2(n0))

            def s3():
                S.op("act", "copy", reads=["proj"], writes=[("Vaug", t)], out=Vaug[:, t, 0:2, 0:64],
                     in_=proj[:, 640:768].rearrange("p (h d) -> p h d", d=64))
                S.op("act", "copy", reads=["proj"], writes=[("Vaug", t)], out=Vaug[:, t, 2:10, 0:64],
                     in_=proj[:, 1792:2304].rearrange("p (h d) -> p h d", d=64))
            steps.append(s3)
            cosb = RW["cos"][:, t, :]
            sinb = RW["sin"][:, t, :]

            def s4a1(c0, nh, h0):
                S.op("act", "activation", reads=["proj"], writes=["sqb"], out=sqb[:, 0:nh * 64],
                     in_=proj[:, c0:c0 + nh * 64], func=AF.Square)

            def s4a2(c0, nh, h0):
                S.op("dve", "tensor_reduce", reads=["sqb"], writes=["ssh"], out=ssh[:, h0:h0 + nh],
                     in_=sqb[:, 0:nh * 64].rearrange("p (h d) -> p h d", d=64), axis=AX.X, op=ALU.add)

            def s4a3(c0, nh, h0):
                rms_rstd(S, ssh[:, h0:h0 + nh], rsh[:, h0:h0 + nh], lnh[:, h0:h0 + nh], 1.0 / 64, C["eps"][:],
                         ["ssh"], ["rsh"], "lnh")

            def s4a4(c0, nh, h0):
                pv = proj[:, c0:c0 + nh * 64].rearrange("p (h d) -> p h d", d=64)
                S.op("dve", "tensor_tensor", reads=["proj", "rsh"], writes=["tn"], out=tn[:, 0:nh, :], in0=pv,
                     in1=rsh[:, h0:h0 + nh].unsqueeze(2).to_broadcast([128, nh, 64]), op=ALU.mult)
                S.op("dve", "tensor_tensor", reads=["tn", "gfull"], writes=["tn"], out=tn[:, 0:nh, :],
                     in0=tn[:, 0:nh, :], in1=gfull[:, h0:h0 + nh, :], op=ALU.mult)

            def s4b(c0, nh, h0):
                t4 = tn[:, 0:nh, :].rearrange("p h (two d) -> p h two d", two=2)
                tc4 = tcb[:, 0:nh, :].rearrange("p h (two d) -> p h two d", two=2)
                ts4 = tsb[:, 0:nh, :].rearrange("p h (two d) -> p h two d", two=2)
                cos4 = cosb.unsqueeze(1).unsqueeze(1).to_broadcast([128, nh, 2, 32])
                sin4 = sinb.unsqueeze(1).unsqueeze(1).to_broadcast([128, nh, 2, 32])
                S.op("dve", "tensor_tensor", reads=["tn", "cos_h"], writes=["tcb"], out=tc4, in0=t4, in1=cos4,
                     op=ALU.mult)
                S.op("dve", "tensor_tensor", reads=["tn", "sin_h"], writes=["tsb"], out=ts4, in0=t4, in1=sin4,
                     op=ALU.mult)
                q4 = qk[:, h0:h0 + nh, :].rearrange("p h (two d) -> p h two d", two=2)
                S.op("dve", "tensor_tensor", reads=["tcb", "tsb"], writes=["qk"], out=q4[:, :, 0, :],
                     in0=tc4[:, :, 0, :], in1=ts4[:, :, 1, :], op=ALU.subtract)
                S.op("dve", "tensor_tensor", reads=["tcb", "tsb"], writes=["qk"], out=q4[:, :, 1, :],
                     in0=tc4[:, :, 1, :], in1=ts4[:, :, 0, :], op=ALU.add)
            for (c0, nh, h0) in ((0, 10, 0), (768, 16, 10)):
                steps.append(lambda c0=c0, nh=nh, h0=h0: s4a1(c0, nh, h0))
                steps.append(lambda c0=c0, nh=nh, h0=h0: s4a2(c0, nh, h0))
                steps.append(lambda c0=c0, nh=nh, h0=h0: s4a3(c0, nh, h0))
                steps.append(lambda c0=c0, nh=nh, h0=h0: s4a4(c0, nh, h0))
                steps.append(lambda c0=c0, nh=nh, h0=h0: s4b(c0, nh, h0))
            qTt, qkey = qT[gi % 2], "qT%d" % (gi % 2)
            qk2 = qk[:].rearrange("p h d -> p (h d)")

            def s5():
                p_, pk = next_psT()
                srcs = [qk2[:, 128 * j:128 * (j + 1)] for j in range(4)] + \
                       [qk2[:, 640 + 128 * j:640 + 128 * (j + 1)] for j in range(4)]
                transposes(S, C, srcs, "qk", p_, pk, qTt[:], qkey, evac="act")

            def s6():
                p_, pk = next_psT()
                srcs = [qk2[:, 512:640]] + [qk2[:, 1152 + 128 * j:1152 + 128 * (j + 1)] for j in range(4)]
                transposes(S, C, srcs, "qk", p_, pk, kT[:, :, t * 128:(t + 1) * 128], ("kT", t))
            steps.append(s5)
            steps.append(s6)
            return steps

        def stageB(b, t, fillers):
            gi = b * ntiles + t
            obuf, obn = obufs[gi % 2], "obuf%d" % (gi % 2)
            qTt, qkey = qT[gi % 2], "qT%d" % (gi % 2)
            jobs = []
            for h in range(8):
                half = slice(0, 64) if h < 4 else slice(64, 128)
                kbs = [t - 1, t] if t >= 1 else [t]

                def maskA(c0, chunk):
                    if len(chunk) == 2:
                        return (0, 2, C["MA"][:].rearrange("p a q -> p (a q)"))
                    return (0, 1, C["MA"][:, 1, :])

                def finA(po, pokey, h=h):
                    S.op("act", "copy", reads=[pokey], writes=[(obn, h)], out=obuf[:, h, :], in_=po[:, 0:65])

                head = {"kT_fn": (lambda kb, half=half: kT[half, 0, kb * 128:(kb + 1) * 128]),
                        "q_ap": qTt[half, h % 4, :], "qkey": qkey, "kkey": KK,
                        "v_fn": (lambda kb, h=h: Vaug[:, kb, h // 4, :]), "vkey": VK, "mask_fn": maskA,
                        "scale": 0.125, "fin_fn": finA}
                jobs += make_jobs(head, kbs)
            for h in range(8):
                half = slice(0, 64) if h % 2 == 0 else slice(64, 128)
                kbs = list(range(t + 1))

                def maskB(c0, chunk, t=t):
                    i0 = 15 - t + chunk[0]
                    return (0, len(chunk), C["MB"][:, i0:i0 + len(chunk), :].rearrange("p a q -> p (a q)"))

                def finB(po, pokey, h=h):
                    S.op("act", "copy", reads=[pokey], writes=[(obn, 8 + h)], out=obuf[:, 8 + h, :],
                         in_=po[:, 0:65])

                head = {"kT_fn": (lambda kb, half=half, h=h: kT[half, 1 + h // 2, kb * 128:(kb + 1) * 128]),
                        "q_ap": qTt[half, 4 + h // 2, :], "qkey": qkey, "kkey": KK,
                        "v_fn": (lambda kb, h=h: Vaug[:, kb, 2 + h, :]), "vkey": VK, "mask_fn": maskB,
                        "scale": 0.125, "fin_fn": finB}
                jobs += make_jobs(head, kbs)
            S.bg_step()
            run_jobs(S, bufs, ctr, jobs, fillers)

        def stageC(b, t):
            row0 = b * SEQ + t * 128
            gi = b * ntiles + t
            xtt, xkey = xt[gi % NB], "xt%d" % (gi % NB)
            obuf, obn = obufs[gi % 2], "obuf%d" % (gi % 2)
            okeys = [(obn, i) for i in range(16)]

            def c1():
                S.op("dve", "tensor_tensor", reads=okeys + ["esink"], writes=["den"], out=den[:, 0:8],
                     in0=obuf[:, 0:8, 64], in1=esink[:], op=ALU.add)
                S.op("dve", "tensor_copy", reads=okeys, writes=["den"], out=den[:, 8:16], in_=obuf[:, 8:16, 64])
                S.op("dve", "reciprocal", reads=["den"], writes=["den"], out=den[:, 16:32], in_=den[:, 0:16])
                S.op("dve", "tensor_tensor", reads=okeys + ["den"], writes=["mix"],
                     out=mix[:].rearrange("p (h d) -> p h d", d=64), in0=obuf[:, :, 0:64],
                     in1=den[:, 16:32].unsqueeze(2).to_broadcast([128, 16, 64]), op=ALU.mult)

            def c2():
                p_, pk = next_psT()
                transposes(S, C, [mix[:, c * 128:(c + 1) * 128] for c in range(8)], "mix", p_, pk, mixT[:], "mixT")

            def c3(n0):
                pp, ppk = next_psP()
                for c in range(8):
                    S.op("pe", "matmul", reads=["mixT", "w_out"], writes=[ppk], signal=(c == 7), out=pp[:],
                         lhsT=mixT[:, c, :], rhs=w_out[:, c, n0:n0 + 512], start=(c == 0), stop=(c == 7))
                S.op("dve", "tensor_tensor", reads=[ppk, xkey], writes=["x1"], out=x1[:, n0:n0 + 512], in0=pp[:],
                     in1=xtt[:, n0:n0 + 512], op=ALU.add)
                if n0 == 512:
                    S.dma("sp", xo[row0:row0 + 128, :], x1[:], reads=["x1"])
            return [c1, c2, (lambda: c3(0)), (lambda: c3(512))]

        order = [(b, t) for b in range(nseq) for t in range(ntiles)]
        for idx, (b, t) in enumerate(order):
            if idx == 0:
                rope_tables(S, C, cx, T["pos_t"].ap()[b], invh, 32, "_h")
                for f in stageA(b, t):
                    f()
            cs = stageC(*order[idx - 1]) if idx >= 1 else []
            as_ = []
            pre = []
            if idx + 1 < len(order):
                nb, nt_ = order[idx + 1]
                if nt_ == 0:
                    pre.append(lambda nb=nb: rope_tables(S, C, cx, T["pos_t"].ap()[nb], invh, 32, "_h"))
                as_ = stageA(nb, nt_)
            nop = lambda: None
            fillers = list(pre)
            if as_:
                c = cs + [nop] * (4 - len(cs))
                fillers += [c[0], as_[0], nop, c[1], as_[1], nop, c[2], as_[2], c[3], as_[3], as_[4], as_[5], as_[6],
                            as_[7], as_[8], as_[9], as_[10], as_[11], as_[13], as_[12], as_[14], as_[15], as_[16],
                            nop, as_[17], nop, nop, as_[18], as_[19]]
            else:
                fillers += cs
            stageB(b, t, fillers)
        for f in stageC(*order[-1]):
            f()
        S.barrier()


def phase_l0_ffn(nc, S, T, x_in, x_out, nrows):
    with ExitStack() as st:
        cx = Ctx(nc, S, st, "f0")
        C = load_consts(nc, S, cx, T)
        G = min(1024, nrows)
        GT = G // 128
        NF = DFF // 128
        wd = cx.sb("wd", [128, NF, D], BF16)
        pre = "l0_w_gate_bf" in T
        wq = "sp" if pre else "pool"
        wdv = T["l0_w_down_bf" if pre else "l0_w_down"].ap().rearrange("(f p) n -> p f n", p=128)
        for f0 in range(0, NF, 6):
            f1 = min(NF, f0 + 6)
            S.dma(wq, wd[:, f0:f1, :], wdv[:, f0:f1, :], writes=["wd"])
        gffn = cx.sb("gffn", [128, D], F32)
        S.dma("sp", gffn[:], bc_dram(T["l0_ffn_norm"], 128, D), writes=["gffn"])
        hT = cx.sb("hT", [128, 8, G], BF16)
        hidT = cx.sb("hidT", [128, NF, G], BF16)
        xs = cx.sb("xs", [128, GT, D], F32)
        wg = [cx.sb("wg%d" % i, [128, 8, 512], BF16) for i in range(2)]
        wu = [cx.sb("wu%d" % i, [128, 8, 512], BF16) for i in range(2)]
        xn = cx.sb("xn", [128, D], BF16)
        W = {"junk": cx.sb("junk", [128, D], BF16), "ss": cx.sb("ss", [128, 1], F32),
             "rstd": cx.sb("rstd", [128, 1], F32), "lnt": cx.sb("lnt", [128, 1], F32)}
        sg = [cx.sb("sg%d" % i, [128, 512], F32) for i in range(2)]
        yo = [cx.sb("yo%d" % i, [128, D], F32) for i in range(2)]
        psT = [cx.ps("psT%d" % i, [128, 8, 128], BF16) for i in range(2)]
        psG = [cx.ps("psG%d" % i, [128, 512], F32) for i in range(2)]
        psU = [cx.ps("psU%d" % i, [128, 512], F32) for i in range(2)]
        psY = [cx.ps("psY%d" % i, [128, 512], F32) for i in range(2)]
        xin, xo = x_in.ap(), x_out.ap()
        wgd, wud = T["l0_w_gate_bf" if pre else "l0_w_gate"].ap(), T["l0_w_up_bf" if pre else "l0_w_up"].ap()
        blocks = [(n0, min(512, DFF - n0)) for n0 in range(0, DFF, 512)]
        ctr = {"t": 0, "g": 0, "y": 0, "w": 0}

        def load_w(bi):
            n0, nw = blocks[bi]
            i = ctr["w"] % 2
            ctr["w"] += 1
            S.dma(wq, wg[i][:, :, 0:nw], wgd[:, n0:n0 + nw].rearrange("(c p) n -> p c n", p=128),
                  writes=["wg%d" % i])
            S.dma(wq, wu[i][:, :, 0:nw], wud[:, n0:n0 + nw].rearrange("(c p) n -> p c n", p=128),
                  writes=["wu%d" % i])
            return i

        for g0 in range(0, nrows, G):
            for i in range(GT):
                r0 = g0 + i * 128
                S.dma("sp", xs[:, i, :], xin[r0:r0 + 128, :], writes=[("xs", i)])
                norm_tile(S, C, xs[:, i, :], ("xs", i), gffn[:], "gffn", xn[:], "xn", W, "")
                k = ctr["t"] % 2
                ctr["t"] += 1
                transposes(S, C, [xn[:, c * 128:(c + 1) * 128] for c in range(8)], "xn", psT[k], "psT%d" % k,
                           hT[:, :, i * 128:(i + 1) * 128], "hT")
            wi = load_w(0)
            for bi, (n0, nw) in enumerate(blocks):
                cur = wi
                if bi + 1 < len(blocks):
                    wi = load_w(bi + 1)
                for j in range(nw // 128):
                    f = n0 // 128 + j
                    for th in range(G // 512):
                        k = ctr["g"] % 2
                        ctr["g"] += 1
                        tok = slice(th * 512, (th + 1) * 512)
                        for c in range(8):
                            S.op("pe", "matmul", reads=["hT", "wg%d" % cur], writes=["psG%d" % k], signal=(c == 7),
                                 out=psG[k][:], lhsT=wg[cur][:, c, j * 128:(j + 1) * 128], rhs=hT[:, c, tok],
                                 start=(c == 0), stop=(c == 7))
                        for c in range(8):
                            S.op("pe", "matmul", reads=["hT", "wu%d" % cur], writes=["psU%d" % k], signal=(c == 7),
                                 out=psU[k][:], lhsT=wu[cur][:, c, j * 128:(j + 1) * 128], rhs=hT[:, c, tok],
                                 start=(c == 0), stop=(c == 7))
                        S.op("act", "activation", reads=["psG%d" % k], writes=["sg%d" % k], out=sg[k][:],
                             in_=psG[k][:], func=AF.Silu)
                        S.op("dve", "tensor_tensor", reads=["sg%d" % k, "psU%d" % k], writes=[("hidT", f)],
                             out=hidT[:, f, tok], in0=sg[k][:], in1=psU[k][:], op=ALU.mult)
            for i in range(GT):
                r0 = g0 + i * 128
                yk = ctr["y"] % 2
                ctr["y"] += 1
                for hh, n0 in enumerate((0, 512)):
                    k = (ctr["g"] + hh) % 2
                    for f in range(NF):
                        S.op("pe", "matmul", reads=[("hidT", f), "wd"], writes=["psY%d" % k], signal=(f == NF - 1),
                             out=psY[k][:], lhsT=hidT[:, f, i * 128:(i + 1) * 128], rhs=wd[:, f, n0:n0 + 512],
                             start=(f == 0), stop=(f == NF - 1))
                    S.op("dve", "tensor_tensor", reads=["psY%d" % k, ("xs", i)], writes=["yo%d" % yk],
                         out=yo[yk][:, n0:n0 + 512], in0=psY[k][:], in1=xs[:, i, n0:n0 + 512], op=ALU.add)
                S.dma("sp", xo[r0:r0 + 128, :], yo[yk][:], reads=["yo%d" % yk])
        S.barrier()


def phase_l1_attn(nc, S, T, x_in, x_out, nseq, ntiles=NT):
    with ExitStack() as st:
        cx = Ctx(nc, S, st, "a1")
        C = load_consts(nc, S, cx, T, need_ma=True)
        w_in = cx.sb("w_in", [128, 8, L1_IN], BF16)
        S.dma("pool", w_in[:], T["l1_w_in"].ap().rearrange("(c p) n -> p c n", p=128), writes=["w_in"])
        w_uq = cx.sb("w_uq", [128, 3, 1536], BF16)
        S.dma("pool", w_uq[:], T["l1_w_uq"].ap().rearrange("(c p) n -> p c n", p=128), writes=["w_uq"])
        w_ukv = cx.sb("w_ukv", [128, 2, 2048], BF16)
        S.dma("pool", w_ukv[:], T["l1_w_ukv"].ap().rearrange("(c p) n -> p c n", p=128), writes=["w_ukv"])
        w_out = cx.sb("w_out", [128, 8, D], BF16)
        S.dma("pool", w_out[:], T["l1_w_out"].ap().rearrange("(c p) n -> p c n", p=128), writes=["w_out"])
        gattn = cx.sb("gattn", [128, D], F32)
        S.dma("sp", gattn[:], bc_dram(T["l1_attn_norm"], 128, D), writes=["gattn"])
        glat = cx.sb("glat", [128, 640], F32)
        S.dma("sp", glat[:, 0:384], bc_dram(T["l1_q_a_norm"], 128, 384), writes=["glat"])
        S.dma("sp", glat[:, 384:640], bc_dram(T["l1_kv_a_norm"], 128, 256), writes=["glat"])
        gq = cx.sb("gq", [128, 96], F32)
        S.dma("sp", gq[:], bc_dram(T["l1_c_q_norm"], 128, 96), writes=["gq"])
        gk = cx.sb("gk", [128, 96], F32)
        S.dma("sp", gk[:], bc_dram(T["l1_c_k_norm"], 128, 96), writes=["gk"])
        invc = cx.sb("invc", [128, 16], F32)
        S.dma("sp", invc[:], T["inv_c"].ap(), writes=["inv_c"])
        rp = alloc_rope(cx, 16, "_c", big=True)
        trq, tcq, tsq = rp["ang_full"], rp["rr_full"], rp["kf_full"]
        C["rope_c"] = rp

        kT = cx.sb("kT", [128, 16, SEQ], BF16)
        Vaug = cx.sb("Vaug", [128, NT, 16, 65], BF16)
        S.op("dve", "memset", writes=[("Vaug", i) for i in range(NT)], ap=Vaug[:], constant=1.0)
        NB = 2
        xt = [cx.sb("xt%d" % i, [128, D], F32) for i in range(NB)]
        qTs = [cx.sb("qT%d" % i, [128, 16, 128], BF16) for i in range(2)]
        xn = cx.sb("xn", [128, D], BF16)
        hT = cx.sb("hT", [128, 8, 128], BF16)
        pj = cx.sb("pj", [128, L1_IN], F32)
        cn = cx.sb("cn", [128, 640], BF16)
        cT = hT
        qf = cx.sb("qf", [128, 16, 96], F32)
        knf = cx.sb("knf", [128, 16, 64], F32)
        W = {"junk": xn, "ss": cx.sb("ss", [128, 1], F32),
             "rstd": cx.sb("rstd", [128, 1], F32), "lnt": cx.sb("lnt", [128, 1], F32)}
        lss = cx.sb("lss", [128, 4], F32)
        lrs = cx.sb("lrs", [128, 4], F32)
        lln = cx.sb("lln", [128, 4], F32)
        ssh = cx.sb("ssh", [128, 32], F32)
        rsh = cx.sb("rsh", [128, 32], F32)
        lnh = cx.sb("lnh", [128, 32], F32)
        kr = cx.sb("kr", [128, 4, 32], F32)
        qb = cx.sb("qb", [128, 16, 96], BF16)
        kb_ = cx.sb("kb", [128, 16, 96], BF16)
        pT = [cx.sb("pT%d" % i, [128, 512], BF16) for i in range(3)]
        obuf = cx.sb("obuf", [128, 16, 65], F32)
        mix = cx.sb("mix", [128, D], BF16)
        mixT = hT
        den = cx.sb("den", [128, 16], F32)
        psT = [cx.ps("psT%d" % i, [128, 8, 128], BF16) for i in range(2)]
        psP = [cx.ps("psP%d" % i, [128, 512], F32) for i in range(1)]
        psS = [cx.ps("psS%d" % i, [128, 512], F32) for i in range(3)]
        psO = [cx.ps("psO%d" % i, [128, 512], F32) for i in range(2)]
        bufs = {"psS": psS, "pT": pT, "psO": psO, "ident": C["ident"][:]}
        ctr = {"s": 0, "p": 0, "o": 0, "t": 0, "pp": 0}

        def next_psT():
            i = ctr["t"] % 2
            ctr["t"] += 1
            return psT[i], "psT%d" % i

        def next_psP():
            i = ctr["pp"] % len(psP)
            ctr["pp"] += 1
            return psP[i], "psP%d" % i

        KK = lambda kb: ("kT", kb)
        VK = lambda kb: ("Vaug", kb)
        xin, xo = x_in.ap(), x_out.ap()
        SC = 96 ** -0.5

        RW = C["rope_c"]
        jk96 = qb
        jk64 = kb_

        def stageA(b, t):
            row0 = b * SEQ + t * 128
            gi = b * ntiles + t
            xtt, xkey = xt[gi % NB], "xt%d" % (gi % NB)
            steps = []

            def s1a():
                S.dma("sp", xtt[:], xin[row0:row0 + 128, :], writes=[xkey])
                norm_tile(S, C, xtt[:], xkey, gattn[:], "gattn", xn[:], "xn", W, "")

            def s1b():
                p_, pk = next_psT()
                transposes(S, C, [xn[:, c * 128:(c + 1) * 128] for c in range(8)], "xn", p_, pk, hT[:], "hT")

            def s1c():
                for n0 in range(0, L1_IN, 512):
                    nw = min(512, L1_IN - n0)
                    pp, ppk = next_psP()
                    for c in range(8):
                        S.op("pe", "matmul", reads=["hT", "w_in"], writes=[ppk], signal=(c == 7), out=pp[:, 0:nw],
                             lhsT=hT[:, c, :], rhs=w_in[:, c, n0:n0 + nw], start=(c == 0), stop=(c == 7))
                    S.op("act", "copy", reads=[ppk], writes=["pj"], out=pj[:, n0:n0 + nw], in_=pp[:, 0:nw])
            steps.append(s1a)
            steps.append(s1b)
            steps.append(s1c)

            def s2a():
                for (i, c0, c1) in ((0, 0, 384), (1, 384, 640)):
                    S.op("dve", "scalar_tensor_tensor", reads=["pj"], writes=["cn", "lss"], out=cn[:, c0:c1],
                         in0=pj[:, c0:c1], scalar=1.0, in1=pj[:, c0:c1], op0=ALU.mult, op1=ALU.mult,
                         accum_out=lss[:, i:i + 1])

            def s2b():
                for (i, c0, c1) in ((0, 0, 384), (1, 384, 640)):
                    rms_rstd(S, lss[:, i:i + 1], lrs[:, i:i + 1], lln[:, i:i + 1], 1.0 / (c1 - c0), C["eps"][:],
                             ["lss"], ["lrs"], "lln")

            def s2c():
                for (i, c0, c1) in ((0, 0, 384), (1, 384, 640)):
                    S.op("dve", "scalar_tensor_tensor", reads=["pj", "lrs", "glat"], writes=["cn"], out=cn[:, c0:c1],
                         in0=pj[:, c0:c1], scalar=lrs[:, i:i + 1], in1=glat[:, c0:c1], op0=ALU.mult, op1=ALU.mult)

            def s2d():
                p_, pk = next_psT()
                transposes(S, C, [cn[:, c * 128:(c + 1) * 128] for c in range(5)], "cn", p_, pk, cT[:, 0:5, :], "hT")
            steps.append(s2a)
            steps.append(s2b)
            steps.append(s2c)
            steps.append(s2d)
            qf2 = qf[:].rearrange("p h d -> p (h d)")

            def s3(n0):
                pp, ppk = next_psP()
                for c in range(3):
                    S.op("pe", "matmul", reads=["hT", "w_uq"], writes=[ppk], signal=(c == 2), out=pp[:],
                         lhsT=cT[:, c, :], rhs=w_uq[:, c, n0:n0 + 512], start=(c == 0), stop=(c == 2))
                S.op("act", "copy", reads=[ppk], writes=["qf"], out=qf2[:, n0:n0 + 512], in_=pp[:])
            for n0 in range(0, 1536, 512):
                steps.append(lambda n0=n0: s3(n0))

            def s4(j):
                pp, ppk = next_psP()
                for c in range(2):
                    S.op("pe", "matmul", reads=["hT", "w_ukv"], writes=[ppk], signal=(c == 1), out=pp[:],
                         lhsT=cT[:, 3 + c, :], rhs=w_ukv[:, c, j * 512:(j + 1) * 512], start=(c == 0),
                         stop=(c == 1))
                pv = pp[:].rearrange("p (h d) -> p h d", d=128)
                S.op("act", "copy", reads=[ppk], writes=["knf"], out=knf[:, 4 * j:4 * j + 4, :], in_=pv[:, :, 0:64])
                S.op("act", "copy", reads=[ppk], writes=[("Vaug", t)], out=Vaug[:, t, 4 * j:4 * j + 4, 0:64],
                     in_=pv[:, :, 64:128])
            for j in range(4):
                steps.append(lambda j=j: s4(j))
            cosb = RW["cos"][:, t, :]
            sinb = RW["sin"][:, t, :]

            def s5a():
                S.op("act", "activation", reads=["qf"], writes=["qb"], out=jk96[:], in_=qf[:], func=AF.Square)
                S.op("act", "activation", reads=["knf"], writes=["kb"], out=jk64[:, :, 0:64], in_=knf[:],
                     func=AF.Square)

            def s5b():
                S.op("dve", "tensor_reduce", reads=["qb"], writes=["ssh"], out=ssh[:, 0:16], in_=jk96[:], axis=AX.X,
                     op=ALU.add)
                S.op("dve", "tensor_reduce", reads=["kb"], writes=["ssh"], out=ssh[:, 16:32], in_=jk64[:, :, 0:64],
                     axis=AX.X, op=ALU.add)
                S.op("dve", "scalar_tensor_tensor", reads=["pj"], writes=["kr", "lss"], out=kr[:, 1, :],
                     in0=pj[:, 640:672], scalar=1.0, in1=pj[:, 640:672], op0=ALU.mult, op1=ALU.mult,
                     accum_out=lss[:, 2:3])
                S.op("dve", "tensor_scalar", reads=["ssh", "lss"], writes=["ssh"], out=ssh[:, 16:32], in0=ssh[:, 16:32],
                     scalar1=lss[:, 2:3], scalar2=None, op0=ALU.add)

            def s5c():
                rms_rstd(S, ssh[:], rsh[:], lnh[:], 1.0 / 96, C["eps"][:], ["ssh"], ["rsh"], "lnh")
            steps.append(s5a)
            steps.append(s5b)
            steps.append(lambda: None)
            steps.append(s5c)

            def s6():
                S.op("dve", "tensor_tensor", reads=["qf", "rsh"], writes=["qf"], out=qf[:], in0=qf[:],
                     in1=rsh[:, 0:16].unsqueeze(2).to_broadcast([128, 16, 96]), op=ALU.mult)
                S.op("dve", "tensor_tensor", reads=["qf", "gq"], writes=["qb"], out=qb[:, :, 0:64], in0=qf[:, :, 0:64],
                     in1=gq[:, 0:64].unsqueeze(1).to_broadcast([128, 16, 64]), op=ALU.mult)
                S.op("dve", "tensor_tensor", reads=["qf", "gq"], writes=["ang_c"], out=trq[:], in0=qf[:, :, 64:96],
                     in1=gq[:, 64:96].unsqueeze(1).to_broadcast([128, 16, 32]), op=ALU.mult)
                t4 = trq[:].rearrange("p h (two d) -> p h two d", two=2)
                tc4 = tcq[:].rearrange("p h (two d) -> p h two d", two=2)
                ts4 = tsq[:].rearrange("p h (two d) -> p h two d", two=2)
                cos4 = cosb.unsqueeze(1).unsqueeze(1).to_broadcast([128, 16, 2, 16])
                sin4 = sinb.unsqueeze(1).unsqueeze(1).to_broadcast([128, 16, 2, 16])
                S.op("dve", "tensor_tensor", reads=["ang_c", "cos_c"], writes=["rr_c"], out=tc4, in0=t4, in1=cos4,
                     op=ALU.mult)
                S.op("dve", "tensor_tensor", reads=["ang_c", "sin_c"], writes=["kf_c"], out=ts4, in0=t4, in1=sin4,
                     op=ALU.mult)
                S.op("dve", "tensor_tensor", reads=["rr_c", "kf_c"], writes=["qb"], out=qb[:, :, 64:80],
                     in0=tc4[:, :, 0, :], in1=ts4[:, :, 1, :], op=ALU.subtract)
                S.op("dve", "tensor_tensor", reads=["rr_c", "kf_c"], writes=["qb"], out=qb[:, :, 80:96],
                     in0=tc4[:, :, 1, :], in1=ts4[:, :, 0, :], op=ALU.add)
            steps.append(s6)

            def s7():
                S.op("dve", "tensor_tensor", reads=["knf", "rsh"], writes=["knf"], out=knf[:], in0=knf[:],
                     in1=rsh[:, 16:32].unsqueeze(2).to_broadcast([128, 16, 64]), op=ALU.mult)
                S.op("dve", "tensor_tensor", reads=["knf", "gk"], writes=["kb"], out=kb_[:, :, 0:64], in0=knf[:],
                     in1=gk[:, 0:64].unsqueeze(1).to_broadcast([128, 16, 64]), op=ALU.mult)
                S.op("dve", "tensor_tensor", reads=["pj", "gk"], writes=["kr"], out=kr[:, 0, :], in0=pj[:, 640:672],
                     in1=gk[:, 64:96], op=ALU.mult)
                k0 = kr[:, 0, :].rearrange("p (two d) -> p two d", two=2)
                k1 = kr[:, 1, :].rearrange("p (two d) -> p two d", two=2)
                k2 = kr[:, 2, :].rearrange("p (two d) -> p two d", two=2)
                S.op("dve", "tensor_tensor", reads=["kr", "cos_c"], writes=["kr"], out=k1, in0=k0,
                     in1=cosb.unsqueeze(1).to_broadcast([128, 2, 16]), op=ALU.mult)
                S.op("dve", "tensor_tensor", reads=["kr", "sin_c"], writes=["kr"], out=k2, in0=k0,
                     in1=sinb.unsqueeze(1).to_broadcast([128, 2, 16]), op=ALU.mult)
                S.op("dve", "tensor_tensor", reads=["kr"], writes=["kr"], out=kr[:, 3, 0:16], in0=k1[:, 0, :],
                     in1=k2[:, 1, :], op=ALU.subtract)
                S.op("dve", "tensor_tensor", reads=["kr"], writes=["kr"], out=kr[:, 3, 16:32], in0=k1[:, 1, :],
                     in1=k2[:, 0, :], op=ALU.add)
                S.op("dve", "tensor_tensor", reads=["kr", "rsh"], writes=["kb"], out=kb_[:, :, 64:96],
                     in0=kr[:, 3, :].unsqueeze(1).to_broadcast([128, 16, 32]),
                     in1=rsh[:, 16:32].unsqueeze(2).to_broadcast([128, 16, 32]), op=ALU.mult)
            steps.append(s7)
            kb2 = kb_[:].rearrange("p h d -> p (h d)")

            def s8(half):
                p_, pk = next_psT()
                transposes(S, C, [kb2[:, (8 * half + j) * 96:(8 * half + j + 1) * 96] for j in range(8)], "kb", p_,
                           pk, kT[0:96, 8 * half:8 * half + 8, t * 128:(t + 1) * 128], ("kT", t), nrows=96)
            steps.append(lambda: s8(0))
            steps.append(lambda: s8(1))
            return steps

        def q_transposes(b, t, half):
            gi = b * ntiles + t
            qT, qkey = qTs[gi % 2], "qT%d" % (gi % 2)
            qb2 = qb[:].rearrange("p h d -> p (h d)")
            p_, pk = next_psT()
            transposes(S, C, [qb2[:, (8 * half + j) * 96:(8 * half + j + 1) * 96] for j in range(8)], "qb", p_,
                       pk, qT[0:96, 8 * half:8 * half + 8, :], qkey, nrows=96, evac="act")

        def stageB(b, t, fillers):
            row0 = b * SEQ + t * 128
            gi = b * ntiles + t
            qT, qkey = qTs[gi % 2], "qT%d" % (gi % 2)
            kbs = list(range(t + 1))
            jobs = []
            for h in range(16):
                def maskC(c0, chunk, t=t):
                    if chunk[-1] == t:
                        return (len(chunk) - 1, len(chunk), C["MA"][:, 1, :])
                    return None

                def finC(po, pokey, h=h):
                    S.op("act", "copy", reads=[pokey], writes=[("obuf", h)], out=obuf[:, h, :], in_=po[:, 0:65])

                head = {"kT_fn": (lambda kb, h=h: kT[0:96, h, kb * 128:(kb + 1) * 128]), "q_ap": qT[0:96, h, :],
                        "qkey": qkey, "kkey": KK, "v_fn": (lambda kb, h=h: Vaug[:, kb, h, :]), "vkey": VK,
                        "mask_fn": maskC, "scale": SC, "fin_fn": finC}
                jobs += make_jobs(head, kbs)
            S.bg_step()
            run_jobs(S, bufs, ctr, jobs, fillers)
            okeys = [("obuf", i) for i in range(16)]
            S.op("dve", "reciprocal", reads=okeys, writes=["den"], out=den[:, 0:16], in_=obuf[:, :, 64])
            S.op("dve", "tensor_tensor", reads=okeys + ["den"], writes=["mix"],
                 out=mix[:].rearrange("p (h d) -> p h d", d=64), in0=obuf[:, :, 0:64],
                 in1=den[:, 0:16].unsqueeze(2).to_broadcast([128, 16, 64]), op=ALU.mult)

        def stageC(b, t):
            row0 = b * SEQ + t * 128
            gi = b * ntiles + t
            xtt, xkey = xt[gi % NB], "xt%d" % (gi % NB)

            def c2():
                p_, pk = next_psT()
                transposes(S, C, [mix[:, c * 128:(c + 1) * 128] for c in range(8)], "mix", p_, pk, mixT[:], "hT")

            def c3(n0):
                pp, ppk = next_psP()
                for c in range(8):
                    S.op("pe", "matmul", reads=["hT", "w_out"], writes=[ppk], signal=(c == 7), out=pp[:],
                         lhsT=mixT[:, c, :], rhs=w_out[:, c, n0:n0 + 512], start=(c == 0), stop=(c == 7))
                S.op("dve", "tensor_tensor", reads=[ppk, xkey], writes=[xkey], out=xtt[:, n0:n0 + 512], in0=pp[:],
                     in1=xtt[:, n0:n0 + 512], op=ALU.add)
                if n0 == 512:
                    S.dma("sp", xo[row0:row0 + 128, :], xtt[:], reads=[xkey])
            return [c2, (lambda: c3(0)), (lambda: c3(512))]

        order = [(b, t) for b in range(nseq) for t in range(ntiles)]
        for idx, (b, t) in enumerate(order):
            if idx == 0:
                rope_tables(S, C, cx, T["pos_t"].ap()[b], invc, 16, "_c")
                for f in stageA(b, t):
                    f()
                q_transposes(b, t, 0)
                q_transposes(b, t, 1)
            cs = stageC(*order[idx - 1]) if idx >= 1 else []
            nop = lambda: None
            fillers = []
            if idx + 1 < len(order):
                nb, nt_ = order[idx + 1]
                if nt_ == 0:
                    fillers.append(lambda nb=nb: rope_tables(S, C, cx, T["pos_t"].ap()[nb], invc, 16, "_c"))
                A_ = stageA(nb, nt_)
                c = cs + [nop] * (3 - len(cs))
                q0 = lambda nb=nb, nt_=nt_: q_transposes(nb, nt_, 0)
                q1 = lambda nb=nb, nt_=nt_: q_transposes(nb, nt_, 1)
                fillers += [c[0], nop, c[1], nop, c[2], A_[0], nop, nop, A_[1], nop, nop, A_[2], nop, A_[3], nop, A_[4],
                            nop, A_[5], nop, A_[6], nop] + A_[7:14] + [A_[14], nop, A_[15], nop, A_[17], nop, A_[18], nop, nop, A_[19],
                                                      q0, q1, nop, A_[20], A_[21]]
            else:
                fillers += cs
            stageB(b, t, fillers)
        for f in stageC(*order[-1]):
            f()
        S.barrier()


def phase_l1_moe(nc, S, T, x_in, x_out, nrows):
    with ExitStack() as st:
        cx = Ctx(nc, S, st, "m1")
        C = load_consts(nc, S, cx, T)
        G = min(1024, nrows)
        GT = G // 128
        NF = DFE // 128
        gffn = cx.sb("gffn", [128, D], F32)
        S.dma("sp", gffn[:], bc_dram(T["l1_ffn_norm"], 128, D), writes=["gffn"])
        rw = cx.sb("rw", [128, 8, NEXP], BF16)
        S.dma("pool", rw[:], T["l1_router"].ap().rearrange("(c p) n -> p c n", p=128), writes=["rw"])
        hT = cx.sb("hT", [128, 8, G], BF16)
        hidT = cx.sb("hidT", [128, NF, G], BF16)
        xs = cx.sb("xs", [128, GT, D], F32)
        wg = [cx.sb("wg%d" % i, [128, 8, 512], BF16) for i in range(2)]
        wu = [cx.sb("wu%d" % i, [128, 8, 512], BF16) for i in range(2)]
        wd = [cx.sb("wd%d" % i, [128, NF, 512], BF16) for i in range(2)]
        xn = cx.sb("xn", [128, D], BF16)
        W = {"junk": cx.sb("junk", [128, D], BF16), "ss": cx.sb("ss", [128, 1], F32),
             "rstd": cx.sb("rstd", [128, 1], F32), "lnt": cx.sb("lnt", [128, 1], F32)}
        sg = [cx.sb("sg%d" % i, [128, 512], F32) for i in range(2)]
        gates = cx.sb("gates", [128, GT, NEXP], F32)
        lg = cx.sb("lg", [128, NEXP], F32)
        l2 = cx.sb("l2", [128, NEXP], F32)
        eq1 = cx.sb("eq1", [128, NEXP], F32)
        eq2 = cx.sb("eq2", [128, NEXP], F32)
        sm = cx.sb("sm", [128, 8], F32)
        psT = [cx.ps("psT%d" % i, [128, 8, 128], BF16) for i in range(2)]
        psG = [cx.ps("psG%d" % i, [128, 512], F32) for i in range(2)]
        psU = [cx.ps("psU%d" % i, [128, 512], F32) for i in range(2)]
        psY = [cx.ps("psY%d" % i, [128, 512], F32) for i in range(2)]
        xin, xo = x_in.ap(), x_out.ap()
        wgd, wud, wdd = T["l1_we_gate"].ap(), T["l1_we_up"].ap(), T["l1_we_down"].ap()
        blocks = [(n0, 512) for n0 in range(0, DFE, 512)]
        ctr = {"t": 0, "g": 0, "y": 0, "w": 0, "d": 0}

        def load_w(e, bi):
            n0, nw = blocks[bi]
            i = ctr["w"] % 2
            ctr["w"] += 1
            S.dma("pool", wg[i][:], wgd[e, :, n0:n0 + nw].rearrange("(c p) n -> p c n", p=128), writes=["wg%d" % i])
            S.dma("pool", wu[i][:], wud[e, :, n0:n0 + nw].rearrange("(c p) n -> p c n", p=128), writes=["wu%d" % i])
            return i

        def load_wd(e, hh):
            i = ctr["d"] % 2
            ctr["d"] += 1
            src = wdd[e, :, hh * 512:(hh + 1) * 512].rearrange("(f p) n -> p f n", p=128)
            for f0 in range(0, NF, 7):
                S.dma("pool", wd[i][:, f0:f0 + 7, :], src[:, f0:f0 + 7, :], writes=["wd%d" % i])
            return i

        for g0 in range(0, nrows, G):
            for i in range(GT):
                r0 = g0 + i * 128
                S.dma("sp", xs[:, i, :], xin[r0:r0 + 128, :], writes=[("xs", i)])
                norm_tile(S, C, xs[:, i, :], ("xs", i), gffn[:], "gffn", xn[:], "xn", W, "")
                k = ctr["t"] % 2
                ctr["t"] += 1
                transposes(S, C, [xn[:, c * 128:(c + 1) * 128] for c in range(8)], "xn", psT[k], "psT%d" % k,
                           hT[:, :, i * 128:(i + 1) * 128], "hT")
                for c in range(8):
                    S.op("pe", "matmul", reads=["hT", "rw"], writes=["psY0"], signal=(c == 7), out=psY[0][:, 0:NEXP],
                         lhsT=hT[:, c, i * 128:(i + 1) * 128], rhs=rw[:, c, :], start=(c == 0), stop=(c == 7))
                S.op("dve", "tensor_copy", reads=["psY0"], writes=["lg"], out=lg[:], in_=psY[0][:, 0:NEXP])
                S.op("dve", "tensor_reduce", reads=["lg"], writes=["sm"], out=sm[:, 0:1], in_=lg[:], axis=AX.X,
                     op=ALU.max)
                S.op("dve", "tensor_scalar", reads=["lg", "sm"], writes=["eq1"], out=eq1[:], in0=lg[:],
                     scalar1=sm[:, 0:1], scalar2=None, op0=ALU.is_equal)
                S.op("dve", "scalar_tensor_tensor", reads=["eq1", "lg"], writes=["l2"], out=l2[:], in0=eq1[:],
                     scalar=-1e30, in1=lg[:], op0=ALU.mult, op1=ALU.add)
                S.op("dve", "tensor_reduce", reads=["l2"], writes=["sm"], out=sm[:, 1:2], in_=l2[:], axis=AX.X,
                     op=ALU.max)
                S.op("dve", "tensor_scalar", reads=["l2", "sm"], writes=["eq2"], out=eq2[:], in0=l2[:],
                     scalar1=sm[:, 1:2], scalar2=None, op0=ALU.is_equal)
                S.op("dve", "tensor_tensor", reads=["sm"], writes=["sm"], out=sm[:, 2:3], in0=sm[:, 1:2],
                     in1=sm[:, 0:1], op=ALU.subtract)
                S.op("act", "activation", reads=["sm"], writes=["sm"], out=sm[:, 3:4], in_=sm[:, 2:3], func=AF.Exp)
                S.op("dve", "tensor_scalar", reads=["sm"], writes=["sm"], out=sm[:, 4:5], in0=sm[:, 3:4], scalar1=1.0,
                     scalar2=None, op0=ALU.add)
                S.op("dve", "reciprocal", reads=["sm"], writes=["sm"], out=sm[:, 5:6], in_=sm[:, 4:5])
                S.op("dve", "tensor_tensor", reads=["sm"], writes=["sm"], out=sm[:, 6:7], in0=sm[:, 3:4],
                     in1=sm[:, 5:6], op=ALU.mult)
                S.op("dve", "tensor_scalar", reads=["eq1", "sm"], writes=[("gates", i)], out=gates[:, i, :],
                     in0=eq1[:], scalar1=sm[:, 5:6], scalar2=None, op0=ALU.mult)
                S.op("dve", "scalar_tensor_tensor", reads=["eq2", "sm", ("gates", i)], writes=[("gates", i)],
                     out=gates[:, i, :], in0=eq2[:], scalar=sm[:, 6:7], in1=gates[:, i, :], op0=ALU.mult,
                     op1=ALU.add)
            for e in range(NEXP):
                wi = load_w(e, 0)
                dcur = None
                for bi, (n0, nw) in enumerate(blocks):
                    cur = wi
                    if bi + 1 < len(blocks):
                        wi = load_w(e, bi + 1)
                    elif dcur is None:
                        pass
                    if bi == 2:
                        dcur = load_wd(e, 0)
                    for j in range(nw // 128):
                        f = n0 // 128 + j
                        for th in range(G // 512):
                            k = ctr["g"] % 2
                            ctr["g"] += 1
                            tok = slice(th * 512, (th + 1) * 512)
                            for c in range(8):
                                S.op("pe", "matmul", reads=["hT", "wg%d" % cur], writes=["psG%d" % k],
                                     signal=(c == 7), out=psG[k][:], lhsT=wg[cur][:, c, j * 128:(j + 1) * 128],
                                     rhs=hT[:, c, tok], start=(c == 0), stop=(c == 7))
                            for c in range(8):
                                S.op("pe", "matmul", reads=["hT", "wu%d" % cur], writes=["psU%d" % k],
                                     signal=(c == 7), out=psU[k][:], lhsT=wu[cur][:, c, j * 128:(j + 1) * 128],
                                     rhs=hT[:, c, tok], start=(c == 0), stop=(c == 7))
                            S.op("act", "activation", reads=["psG%d" % k], writes=["sg%d" % k], out=sg[k][:],
                                 in_=psG[k][:], func=AF.Silu)
                            S.op("dve", "tensor_tensor", reads=["sg%d" % k, "psU%d" % k], writes=[("hidT", f)],
                                 out=hidT[:, f, tok], in0=sg[k][:], in1=psU[k][:], op=ALU.mult)
                for hh in range(2):
                    dnext = load_wd(e, 1) if hh == 0 else None
                    for i in range(GT):
                        k = ctr["y"] % 2
                        ctr["y"] += 1
                        for f in range(NF):
                            S.op("pe", "matmul", reads=[("hidT", f), "wd%d" % dcur], writes=["psY%d" % k],
                                 signal=(f == NF - 1), out=psY[k][:], lhsT=hidT[:, f, i * 128:(i + 1) * 128],
                                 rhs=wd[dcur][:, f, :], start=(f == 0), stop=(f == NF - 1))
                        S.op("dve", "scalar_tensor_tensor", reads=["psY%d" % k, ("gates", i), ("xs", i)],
                             writes=[("xs", i)], out=xs[:, i, hh * 512:(hh + 1) * 512], in0=psY[k][:],
                             scalar=gates[:, i, e:e + 1], in1=xs[:, i, hh * 512:(hh + 1) * 512], op0=ALU.mult,
                             op1=ALU.add)
                    if dnext is not None:
                        dcur = dnext
            for i in range(GT):
                r0 = g0 + i * 128
                S.dma("sp", xo[r0:r0 + 128, :], xs[:, i, :], reads=[("xs", i)])
        S.barrier()


def phase_l1_moe_sparse(nc, S, T, x_in, x_out, nrows):
    ntile = nrows // 128
    maxgrp = (nrows + GRP - 1) // GRP
    NF = DFE // 128
    hs = nc.dram_tensor("moe_hs", [NEXP * ECAP, D], BF16)
    Y = nc.dram_tensor("moe_y", [NEXP * ECAP, D], F32)
    xin, xo = x_in.ap(), x_out.ap()
    with ExitStack() as st0:
        cx0 = Ctx(nc, S, st0, "m1")
        posb = cx0.sb("posb", [128, ntile, 2], I32)
        gtb = cx0.sb("gtb", [128, ntile, 2], F32)
        ngb = cx0.sb("ngb", [1, NEXP], I32)
        with ExitStack() as st:
            cx = Ctx(nc, S, st, "m1a")
            C = load_consts(nc, S, cx, T)
            gffn = cx.sb("gffn", [128, D], F32)
            S.dma("sp", gffn[:], bc_dram(T["l1_ffn_norm"], 128, D), writes=["gffn"])
            rw = cx.sb("rw", [128, 8, NEXP], BF16)
            S.dma("pool", rw[:], T["l1_router"].ap().rearrange("(c p) n -> p c n", p=128), writes=["rw"])
            triU = cx.sb("triU", [128, 128], BF16)
            S.dma("pool", triU[:], T["triU"].ap(), writes=["triU"])
            onesb = cx.sb("onesb", [128, 128], BF16)
            S.op("dve", "memset", writes=["onesb"], ap=onesb[:], constant=1.0)
            eoff = cx.sb("eoff", [128, NEXP], F32)
            S.dma("sp", eoff[:], T["eoff"].ap(), writes=["eoff"])
            base = cx.sb("base", [128, NEXP], F32)
            S.op("dve", "memset", writes=["base"], ap=base[:], constant=0.0)
            xs = [cx.sb("xs%d" % i, [128, D], F32) for i in range(2)]
            xn = [cx.sb("xn%d" % i, [128, D], BF16) for i in range(3)]
            W = {"junk": cx.sb("junk", [128, D], BF16), "ss": cx.sb("ss", [128, 1], F32),
                 "rstd": cx.sb("rstd", [128, 1], F32), "lnt": cx.sb("lnt", [128, 1], F32)}
            NPB = 3
            small = []
            for k in range(NPB):
                small.append({
                    "lg": cx.sb("lg%d" % k, [128, NEXP], F32), "l2": cx.sb("l2%d" % k, [128, NEXP], F32),
                    "eq1": cx.sb("eq1%d" % k, [128, NEXP], F32), "eq2": cx.sb("eq2%d" % k, [128, NEXP], F32),
                    "selb": cx.sb("selb%d" % k, [128, NEXP], BF16), "dest": cx.sb("dest%d" % k, [128, NEXP], F32),
                    "tmp8": cx.sb("tmp8%d" % k, [128, NEXP], F32), "sm": cx.sb("sm%d" % k, [128, 12], F32),
                    "hT": cx.sb("hTr%d" % k, [128, 8, 128], BF16)})
            psT = [cx.ps("psT%d" % i, [128, 8, 128], BF16) for i in range(2)]
            psRs = [cx.ps("psR%d" % i, [128, 512], F32) for i in range(2)]
            psDs = [cx.ps("psD%d" % i, [128, 512], F32) for i in range(2)]
            psCs = [cx.ps("psC%d" % i, [128, 512], F32) for i in range(2)]
            for i in range(ntile):
                r0 = i * 128
                sb_ = small[i % NPB]
                lg, l2, eq1, eq2, selb, dest, tmp8, sm, hT = (sb_[k] for k in
                                                               ("lg", "l2", "eq1", "eq2", "selb", "dest", "tmp8", "sm",
                                                                "hT"))
                psR, psD, psC = psRs[i % 2], psDs[i % 2], psCs[i % 2]
                kk = lambda nm, i=i: nm + str(i % NPB)
                kp = lambda nm, i=i: nm + str(i % 2)
                xt, xk = xs[i % 2], "xs%d" % (i % 2)
                xnt, xnk = xn[i % 3], "xn%d" % (i % 3)
                S.dma("sp", xt[:], xin[r0:r0 + 128, :], writes=[xk])
                norm_tile(S, C, xt[:], xk, gffn[:], "gffn", xnt[:], xnk, W, "")
                k = i % 2
                transposes(S, C, [xnt[:, c * 128:(c + 1) * 128] for c in range(8)], xnk, psT[k], "psT%d" % k, hT[:],
                           kk("hT"))
                for c in range(8):
                    S.op("pe", "matmul", reads=[kk("hT"), "rw"], writes=[kp("psR")], signal=(c == 7), out=psR[:, 0:NEXP],
                         lhsT=hT[:, c, :], rhs=rw[:, c, :], start=(c == 0), stop=(c == 7))
                S.op("dve", "tensor_copy", reads=[kp("psR")], writes=[kk("lg")], out=lg[:], in_=psR[:, 0:NEXP])
                S.op("dve", "tensor_reduce", reads=[kk("lg")], writes=[kk("sm")], out=sm[:, 0:1], in_=lg[:], axis=AX.X,
                     op=ALU.max)
                S.op("dve", "tensor_scalar", reads=[kk("lg"), kk("sm")], writes=[kk("eq1")], out=eq1[:], in0=lg[:],
                     scalar1=sm[:, 0:1], scalar2=None, op0=ALU.is_equal)
                S.op("dve", "scalar_tensor_tensor", reads=[kk("eq1"), kk("lg")], writes=[kk("l2")], out=l2[:], in0=eq1[:],
                     scalar=-1e30, in1=lg[:], op0=ALU.mult, op1=ALU.add)
                S.op("dve", "tensor_reduce", reads=[kk("l2")], writes=[kk("sm")], out=sm[:, 1:2], in_=l2[:], axis=AX.X,
                     op=ALU.max)
                S.op("dve", "tensor_scalar", reads=[kk("l2"), kk("sm")], writes=[kk("eq2")], out=eq2[:], in0=l2[:],
                     scalar1=sm[:, 1:2], scalar2=None, op0=ALU.is_equal)
                S.op("dve", "tensor_tensor", reads=[kk("sm")], writes=[kk("sm")], out=sm[:, 2:3], in0=sm[:, 1:2],
                     in1=sm[:, 0:1], op=ALU.subtract)
                S.op("act", "activation", reads=[kk("sm")], writes=[kk("sm")], out=sm[:, 3:4], in_=sm[:, 2:3], func=AF.Exp)
                S.op("dve", "tensor_scalar", reads=[kk("sm")], writes=[kk("sm")], out=sm[:, 4:5], in0=sm[:, 3:4], scalar1=1.0,
                     scalar2=None, op0=ALU.add)
                S.op("dve", "reciprocal", reads=[kk("sm")], writes=[("gtb", i)], out=gtb[:, i, 0:1], in_=sm[:, 4:5])
                S.op("dve", "tensor_tensor", reads=[kk("sm"), ("gtb", i)], writes=[("gtb", i)], out=gtb[:, i, 1:2],
                     in0=sm[:, 3:4], in1=gtb[:, i, 0:1], op=ALU.mult)
                S.op("dve", "tensor_tensor", reads=[kk("eq1"), kk("eq2")], writes=[kk("selb")], out=selb[:], in0=eq1[:],
                     in1=eq2[:], op=ALU.add)
                S.op("pe", "matmul", reads=[kk("selb"), "triU"], writes=[kp("psD")], out=psD[:, 0:NEXP], lhsT=triU[:],
                     rhs=selb[:], start=True, stop=True)
                S.op("pe", "matmul", reads=[kk("selb"), "onesb"], writes=[kp("psC")], out=psC[:, 0:NEXP], lhsT=onesb[:],
                     rhs=selb[:], start=True, stop=True)
                S.op("dve", "tensor_tensor", reads=[kp("psD"), "base"], writes=[kk("dest")], out=dest[:], in0=psD[:, 0:NEXP],
                     in1=base[:], op=ALU.add)
                S.op("dve", "tensor_tensor", reads=[kp("psC"), "base"], writes=["base"], out=base[:], in0=psC[:, 0:NEXP],
                     in1=base[:], op=ALU.add)
                S.op("dve", "tensor_tensor", reads=[kk("dest"), "eoff"], writes=[kk("dest")], out=dest[:], in0=dest[:],
                     in1=eoff[:], op=ALU.add)
                for (j, eq, eqk) in ((0, eq1, kk("eq1")), (1, eq2, kk("eq2"))):
                    S.op("dve", "tensor_tensor", reads=[kk("dest"), eqk], writes=[kk("tmp8")], out=tmp8[:], in0=dest[:],
                         in1=eq[:], op=ALU.mult)
                    S.op("dve", "tensor_reduce", reads=[kk("tmp8")], writes=[kk("sm")], out=sm[:, 8 + j:9 + j], in_=tmp8[:],
                         axis=AX.X, op=ALU.add)
                S.op("dve", "tensor_copy", reads=[kk("sm")], writes=[("posb", i)], out=posb[:, i, :], in_=sm[:, 8:10])
                for j in range(2):
                    S.dma("pool", hs.ap(), xnt[:], reads=[xnk, ("posb", i)],
                          meth="indirect_dma_start", out_offset=bass.IndirectOffsetOnAxis(posb[:, i, j:j + 1], 0),
                          in_offset=None, bounds_check="BC", oob_is_err=False)
            sb_ = small[0]
            lg, l2, eq1, eq2, selb, dest, tmp8, sm, hT = (sb_[k] for k in
                                                           ("lg", "l2", "eq1", "eq2", "selb", "dest", "tmp8", "sm", "hT"))
            kk = lambda nm: nm + "0"
            S.op("dve", "tensor_scalar", reads=["base"], writes=[kk("tmp8")], out=tmp8[0:1, :], in0=base[0:1, :],
                 scalar1=1.0 / GRP, scalar2=(GRP - 1.0) / GRP, op0=ALU.mult, op1=ALU.add)
            ngi = cx.sb("ngi", [1, NEXP], I32)
            S.op("dve", "tensor_copy", reads=[kk("tmp8")], writes=["ngi"], out=ngi[:], in_=tmp8[0:1, :])
            S.op("dve", "tensor_copy", reads=["ngi"], writes=[kk("dest")], out=dest[0:1, :], in_=ngi[:])
            S.op("dve", "tensor_tensor", reads=[kk("dest"), kk("tmp8")], writes=[kk("l2")], out=l2[0:1, :], in0=dest[0:1, :],
                 in1=tmp8[0:1, :], op=ALU.is_gt)
            S.op("dve", "tensor_tensor", reads=[kk("dest"), kk("l2")], writes=[kk("dest")], out=dest[0:1, :], in0=dest[0:1, :],
                 in1=l2[0:1, :], op=ALU.subtract)
            S.op("dve", "tensor_copy", reads=[kk("dest")], writes=["ngb"], out=ngb[:], in_=dest[0:1, :])
            S.barrier()
        with ExitStack() as st:
            cx = Ctx(nc, S, st, "m1b")
            C = load_consts(nc, S, cx, T)
            hr = [cx.sb("hr%d" % i, [128, 4, D], BF16) for i in range(2)]
            hTg = cx.sb("hTg", [128, 8, GRP], BF16)
            hidT = cx.sb("hidT", [128, NF, GRP], BF16)
            wg = [cx.sb("wg%d" % i, [128, 8, 512], BF16) for i in range(2)]
            wu = [cx.sb("wu%d" % i, [128, 8, 512], BF16) for i in range(2)]
            wd = [cx.sb("wd%d" % i, [128, NF, 512], BF16) for i in range(2)]
            sg = [cx.sb("sg%d" % i, [128, 512], F32) for i in range(2)]
            yo = [cx.sb("yo%d" % i, [128, D], F32) for i in range(2)]
            psT = [cx.ps("psT%d" % i, [128, 8, 128], BF16) for i in range(2)]
            psG = [cx.ps("psG%d" % i, [128, 512], F32) for i in range(2)]
            psU = [cx.ps("psU%d" % i, [128, 512], F32) for i in range(2)]
            psY = [cx.ps("psY%d" % i, [128, 512], F32) for i in range(2)]
            wgd, wud, wdd = T["l1_we_gate_bf"].ap(), T["l1_we_up_bf"].ap(), T["l1_we_down_bf"].ap()
            ctr = {"t": 0, "g": 0, "y": 0, "w": 0, "h": 0, "o": 0}

            def load_w(e, bi):
                n0 = bi * 512
                i = ctr["w"] % 2
                ctr["w"] += 1
                S.dma("sp", wg[i][:], wgd[e, :, n0:n0 + 512].rearrange("(c p) n -> p c n", p=128),
                      writes=["wg%d" % i])
                S.dma("sp", wu[i][:], wud[e, :, n0:n0 + 512].rearrange("(c p) n -> p c n", p=128),
                      writes=["wu%d" % i])
                return i

            def load_wd(e):
                for hh in range(2):
                    src = wdd[e, :, hh * 512:(hh + 1) * 512].rearrange("(f p) n -> p f n", p=128)
                    for f0 in range(0, NF, 14):
                        S.dma("pool", wd[hh][:, f0:f0 + 14, :], src[:, f0:f0 + 14, :], writes=["wd%d" % hh])

            for e in range(NEXP):
                S.reg_load_all(ngb[0:1, e:e + 1], reads=["ngb"])
                for r in range(maxgrp):
                    S.cond_begin(r + 1)
                    R0 = e * ECAP + r * GRP
                    hb = ctr["h"] % 2
                    ctr["h"] += 1
                    S.dma("sp", hr[hb][:], hs.ap()[R0:R0 + GRP, :].rearrange("(j p) d -> p j d", p=128),
                          writes=["hr%d" % hb])
                    wi = load_w(e, 0)
                    for j in range(4):
                        k = ctr["t"] % 2
                        ctr["t"] += 1
                        transposes(S, C, [hr[hb][:, j, c * 128:(c + 1) * 128] for c in range(8)], "hr%d" % hb, psT[k],
                                   "psT%d" % k, hTg[:, :, j * 128:(j + 1) * 128], "hTg",
                                   evac=("dve" if j % 2 == 0 else "act"))
                    for bi in range(7):
                        cur = wi
                        if bi + 1 < 7:
                            wi = load_w(e, bi + 1)
                        if bi == 1:
                            load_wd(e)
                        for j in range(4):
                            f = bi * 4 + j
                            k = ctr["g"] % 2
                            ctr["g"] += 1
                            for c in range(8):
                                S.op("pe", "matmul", reads=["hTg", "wg%d" % cur], writes=["psG%d" % k],
                                     signal=(c == 7), out=psG[k][:], lhsT=wg[cur][:, c, j * 128:(j + 1) * 128],
                                     rhs=hTg[:, c, :], start=(c == 0), stop=(c == 7))
                            for c in range(8):
                                S.op("pe", "matmul", reads=["hTg", "wu%d" % cur], writes=["psU%d" % k],
                                     signal=(c == 7), out=psU[k][:], lhsT=wu[cur][:, c, j * 128:(j + 1) * 128],
                                     rhs=hTg[:, c, :], start=(c == 0), stop=(c == 7))
                            S.op("act", "activation", reads=["psG%d" % k], writes=["sg%d" % k], out=sg[k][:],
                                 in_=psG[k][:], func=AF.Silu)
                            S.op("dve", "tensor_tensor", reads=["sg%d" % k, "psU%d" % k], writes=[("hidT", f)],
                                 out=hidT[:, f, :], in0=sg[k][:], in1=psU[k][:], op=ALU.mult)
                    for j in range(4):
                        ob = ctr["o"] % 2
                        ctr["o"] += 1
                        for hh in range(2):
                            k = ctr["y"] % 2
                            ctr["y"] += 1
                            for f in range(NF):
                                S.op("pe", "matmul", reads=[("hidT", f), "wd%d" % hh], writes=["psY%d" % k],
                                     signal=(f == NF - 1), out=psY[k][:], lhsT=hidT[:, f, j * 128:(j + 1) * 128],
                                     rhs=wd[hh][:, f, :], start=(f == 0), stop=(f == NF - 1))
                            if hh == 0:
                                S.op("act", "copy", reads=["psY%d" % k], writes=["yo%d" % ob],
                                     out=yo[ob][:, 0:512], in_=psY[k][:])
                            else:
                                S.op("dve", "tensor_copy", reads=["psY%d" % k], writes=["yo%d" % ob],
                                     out=yo[ob][:, 512:1024], in_=psY[k][:])
                        S.dma("pool", Y.ap()[R0 + j * 128:R0 + (j + 1) * 128, :], yo[ob][:], reads=["yo%d" % ob])
                    S.cond_end()
            S.barrier()
        with ExitStack() as st:
            cx = Ctx(nc, S, st, "m1c")
            NB3 = 3
            xs = [cx.sb("xs%d" % i, [128, D], F32) for i in range(NB3)]
            y1 = [cx.sb("y1_%d" % i, [128, D], F32) for i in range(NB3)]
            y2 = [cx.sb("y2_%d" % i, [128, D], F32) for i in range(NB3)]

            def fetch(i):
                k = i % NB3
                S.dma("sp", xs[k][:], xin[i * 128:(i + 1) * 128, :], writes=["xs%d" % k])
                for (j, yb, nm) in ((0, y1[k], "y1_%d" % k), (1, y2[k], "y2_%d" % k)):
                    S.dma("pool", yb[:], Y.ap(), reads=[("posb", i)], writes=[nm], meth="indirect_dma_start",
                          out_offset=None, in_offset=bass.IndirectOffsetOnAxis(posb[:, i, j:j + 1], 0),
                          bounds_check="BC", oob_is_err=False)

            fetch(0)
            if ntile > 1:
                fetch(1)
            for i in range(ntile):
                r0 = i * 128
                k = i % NB3
                S.op("dve", "scalar_tensor_tensor", reads=["y1_%d" % k, "xs%d" % k], writes=["xs%d" % k],
                     out=xs[k][:], in0=y1[k][:], scalar=gtb[:, i, 0:1], in1=xs[k][:], op0=ALU.mult, op1=ALU.add)
                S.op("dve", "scalar_tensor_tensor", reads=["y2_%d" % k, "xs%d" % k], writes=["xs%d" % k],
                     out=xs[k][:], in0=y2[k][:], scalar=gtb[:, i, 1:2], in1=xs[k][:], op0=ALU.mult, op1=ALU.add)
                if i + 2 < ntile:
                    fetch(i + 2)
                S.dma("sp", xo[r0:r0 + 128, :], xs[k][:], reads=["xs%d" % k])
            S.barrier()


WEIGHT_NAMES = [
    ("l0_attn_norm", [D]), ("l0_w_in", [D, L0_IN]), ("l0_a_q_norm", [64]), ("l0_a_k_norm", [64]),
    ("l0_a_sinks", [8]), ("l0_b_q_norm", [64]), ("l0_b_k_norm", [64]), ("l0_w_out", [D, D]),
    ("l0_ffn_norm", [D]), ("l0_w_gate", [D, DFF]), ("l0_w_up", [D, DFF]), ("l0_w_down", [DFF, D]),
    ("l1_attn_norm", [D]), ("l1_w_in", [D, L1_IN]), ("l1_q_a_norm", [384]), ("l1_w_uq", [384, 1536]),
    ("l1_kv_a_norm", [256]), ("l1_w_ukv", [256, 2048]), ("l1_c_q_norm", [96]), ("l1_c_k_norm", [96]),
    ("l1_w_out", [D, D]), ("l1_ffn_norm", [D]), ("l1_router", [D, NEXP]),
    ("l1_we_gate", [NEXP, D, DFE]), ("l1_we_up", [NEXP, D, DFE]), ("l1_we_down", [NEXP, DFE, D]),
]


def host_consts():
    p = np.arange(128)[:, None]
    j = np.arange(128)[None, :]
    NEG = -10000.0
    maskA = np.where(np.stack([(j < p), (j >= p)], axis=1), 0.0, NEG).astype(np.float32)
    mb = np.zeros((128, 16, 128), np.float32)
    for o in range(16):
        d = o * 128 + j - p
        c = ((d >= 0) & (d <= 128)).astype(np.float32) + ((d >= 0) & (d % 4 == 0) & (d <= 512)) + \
            ((d >= 0) & (d % 16 == 0) & (d <= 2048))
        with np.errstate(divide="ignore"):
            mb[:, 15 - o, :] = np.where(c > 0, np.log(np.maximum(c, 1.0)) / 0.125, NEG)
    inv_h = (10000.0 ** (-np.arange(0, 64, 2, dtype=np.float32) / 64)).astype(np.float32)
    inv_c = (10000.0 ** (-np.arange(0, 32, 2, dtype=np.float32) / 32)).astype(np.float32)
    return {
        "ident": np.eye(128, dtype=np.float32),
        "maskA": maskA,
        "maskB": mb,
        "inv_h": np.ascontiguousarray(np.broadcast_to(inv_h[None, :], (128, 32))),
        "inv_c": np.ascontiguousarray(np.broadcast_to(inv_c[None, :], (128, 16))),
        "triU": (p < j).astype(np.float32),
        "eoff": np.ascontiguousarray(np.broadcast_to((np.arange(8, dtype=np.float32) * 8192)[None, :], (128, 8))),
    }


def build_program(nseq, phases, ntiles=NT):
    nc = bass.Bass("TRN2", target_bir_lowering=False)
    T = {}
    rows = nseq * SEQ
    T["x"] = nc.dram_tensor("x", [rows, D], F32, kind="ExternalInput")
    T["pos_t"] = nc.dram_tensor("pos_t", [nseq, 128, NT], I32, kind="ExternalInput")
    for nm, shp in WEIGHT_NAMES:
        T[nm] = nc.dram_tensor(nm, shp, F32, kind="ExternalInput")
    for nm, arr in host_consts().items():
        T[nm] = nc.dram_tensor(nm, list(arr.shape), F32, kind="ExternalInput")
    T["out"] = nc.dram_tensor("out", [rows, D], F32, kind="ExternalOutput")
    nr = ntiles * 128 if ntiles < NT else rows
    with ExitStack() as st:
        S = Sched(nc, st)
        if "f0" in phases and "a0" in phases:
            for nm, shp in (("l0_w_gate", [D, DFF]), ("l0_w_up", [D, DFF]), ("l0_w_down", [DFF, D])):
                T[nm + "_bf"] = nc.dram_tensor(nm + "_bf", shp, BF16)
                wr = shp[0]
                for r0 in range(0, wr, 512):
                    rc = min(512, wr - r0)

                    def job(nm=nm, r0=r0, rc=rc, ncol=shp[1]):
                        src = T[nm].ap()[r0:r0 + rc, :].rearrange("r (a b) -> r a b", b=256)
                        dst = T[nm + "_bf"].ap()[r0:r0 + rc, :].rearrange("r (a b) -> r a b", b=256)
                        S.dma("pool", dst, src)
                    S.bg_jobs.append(job)
        if "m1" in phases:
            for nm, shp in (("l1_we_gate", [NEXP, D, DFE]), ("l1_we_up", [NEXP, D, DFE]),
                            ("l1_we_down", [NEXP, DFE, D])):
                T[nm + "_bf"] = nc.dram_tensor(nm + "_bf", shp, BF16)
            for e in range(NEXP):
                for nm, wrows in (("l1_we_gate", D), ("l1_we_up", D), ("l1_we_down", DFE)):
                    nchunk = 4
                    rc = wrows // nchunk
                    for ci in range(nchunk):
                        def job(nm=nm, e=e, r0=ci * rc, rc=rc):
                            src = T[nm].ap()[e, r0:r0 + rc, :].rearrange("r (a b) -> r a b", b=512)
                            dst = T[nm + "_bf"].ap()[e, r0:r0 + rc, :].rearrange("r (a b) -> r a b", b=512)
                            S.dma("pool", dst, src)
                        S.bg_jobs.append(job)
        cur = T["x"]
        for i, ph in enumerate(phases):
            dst = T["out"] if i == len(phases) - 1 else nc.dram_tensor("xr%d" % i, [rows, D], F32)
            if ph == "a0":
                phase_l0_attn(nc, S, T, cur, dst, nseq, ntiles)
            elif ph == "f0":
                phase_l0_ffn(nc, S, T, cur, dst, nr)
            elif ph == "a1":
                phase_l1_attn(nc, S, T, cur, dst, nseq, ntiles)
            elif ph == "m1":
                S.bg_step(len(S.bg_jobs))
                phase_l1_moe_sparse(nc, S, T, cur, dst, nr)
            elif ph == "m1d":
                phase_l1_moe(nc, S, T, cur, dst, nr)
            cur = dst
        S.finish()
        S.emit()
    return nc


PHASES = ["a0", "f0", "a1", "m1"]
N_CORES = 8
_PROG = {}


def kernel(**inputs):
    x = np.asarray(inputs["x"], dtype=np.float32)
    pos = np.asarray(inputs["positions"]).astype(np.int32)
    B = x.shape[0]
    nseq = B // N_CORES
    if nseq not in _PROG:
        _PROG[nseq] = build_program(nseq, PHASES)
    nc = _PROG[nseq]
    consts = host_consts()
    shared = {nm: np.ascontiguousarray(np.asarray(inputs[nm], dtype=np.float32)) for nm, _ in WEIGHT_NAMES}
    shared.update(consts)
    in_maps = []
    for c in range(N_CORES):
        m = dict(shared)
        m["x"] = np.ascontiguousarray(x[c * nseq:(c + 1) * nseq].reshape(nseq * SEQ, D))
        m["pos_t"] = np.ascontiguousarray(pos[c * nseq:(c + 1) * nseq].reshape(nseq, NT, 128).transpose(0, 2, 1))
        in_maps.append(m)
    res = run_bass_kernel_spmd(nc, in_maps, core_ids=list(range(N_CORES)))
    outs = [np.asarray(r["out"]).reshape(nseq, SEQ, D) for r in res.results]
    return np.concatenate(outs, axis=0).astype(np.float32)
```
